# Optimizing a Trainium2 kernel written in Bass

```python
import math
import jax, jax.numpy as jnp
from jax import lax
import numpy as np

D_MODEL = 4096
BATCH = 1
SEQ = 8192
DEPTH = 1

SSD_EXPAND = 2
SSD_D_INNER = SSD_EXPAND * D_MODEL
SSD_HEAD_DIM = 64
SSD_HEADS = SSD_D_INNER // SSD_HEAD_DIM
SSD_D_STATE = 128
SSD_GROUPS = 8
SSD_HEADS_PER_GROUP = SSD_HEADS // SSD_GROUPS
SSD_CONV = 5
SSD_CHUNK = 128
SSD_XBC = SSD_D_INNER + 2 * SSD_GROUPS * SSD_D_STATE
HG_HEAD_DIM = 128
HG_WIDTH = D_MODEL
HG_HEADS = HG_WIDTH // HG_HEAD_DIM
HG_CHUNK = 32
N_EXPERTS = 16
EC_CAPACITY_FACTOR = 2
D_FF_EXPERT = D_MODEL // 2
N_BRANCHES = 2
N_MOD = 6
RMS_EPS = 1e-6
IN_PROJ_SIZES = (SSD_D_INNER, SSD_XBC, SSD_HEADS, SSD_HEADS,
                 HG_WIDTH, HG_WIDTH, HG_WIDTH, HG_WIDTH, HG_WIDTH,
                 D_MODEL, D_MODEL)
D_IN_PROJ = sum(IN_PROJ_SIZES)

kernel_name = 'hybrid_ssd_hgrn2_ec_moe_block'


def rms_norm(x, w):
    xf = x.astype(jnp.float32)
    y = xf * lax.rsqrt(jnp.mean(xf * xf, axis=-1, keepdims=True) + RMS_EPS)
    return (y * w.astype(jnp.float32)).astype(x.dtype)


def split_in_proj(proj):
    parts, start = [], 0
    for size in IN_PROJ_SIZES:
        parts.append(proj[..., start:start + size])
        start += size
    return parts


def flip_seq(t):
    return jnp.flip(t, axis=1)


def segsum(a):
    T = a.shape[-1]
    rep = jnp.broadcast_to(a[..., None], a.shape + (T,))
    strict = jnp.tril(jnp.ones((T, T), dtype=bool), -1)
    cs = jnp.cumsum(jnp.where(strict, rep, 0.0), axis=-2)
    return jnp.where(jnp.tril(jnp.ones((T, T), dtype=bool)), cs, -jnp.inf)


def ssd_chunked(x, dt, a, b, c):
    Bsz, S = x.shape[:2]
    nc = S // SSD_CHUNK
    G, R, T = SSD_GROUPS, SSD_HEADS_PER_GROUP, SSD_CHUNK
    xdt = (x * dt[..., None]).reshape(Bsz, nc, T, G, R, SSD_HEAD_DIM)
    bc = b.reshape(Bsz, nc, T, G, SSD_D_STATE)
    cc = c.reshape(Bsz, nc, T, G, SSD_D_STATE)
    adt = jnp.moveaxis((dt * a).reshape(Bsz, nc, T, G, R), 2, -1)
    a_cs = jnp.cumsum(adt, axis=-1)
    decay = jnp.exp(segsum(adt))
    cb = jnp.einsum('bctgn,bcsgn->bcgts', cc, bc)
    y_diag = jnp.einsum('bcgts,bcgrts,bcsgrp->bctgrp', cb, decay, xdt)
    decay_states = jnp.exp(a_cs[..., -1:] - a_cs)
    states = jnp.einsum('bcsgn,bcgrs,bcsgrp->bcgrpn', bc, decay_states, xdt)
    chunk_log = jnp.moveaxis(a_cs[..., -1], 1, -1)
    chunk_log = jnp.pad(chunk_log, ((0, 0), (0, 0), (0, 0), (1, 0)))
    decay_chunk = jnp.exp(segsum(chunk_log))[..., :nc, :]
    states0 = jnp.concatenate([jnp.zeros_like(states[:, :1]), states], axis=1)
    prev_states = jnp.einsum('bgrzc,bcgrpn->bzgrpn', decay_chunk, states0)
    y_off = jnp.einsum('bctgn,bcgrpn,bcgrt->bctgrp', cc, prev_states, jnp.exp(a_cs))
    return (y_diag + y_off).reshape(Bsz, S, SSD_HEADS, SSD_HEAD_DIM)


def mamba2_branch(z, xbc, dt_f_raw, dt_b_raw, conv_w, conv_b, dt_bias_f, dt_bias_b,
                  a_log_f, a_log_b, d_skip, norm_w):
    dtype = z.dtype
    f32 = jnp.float32
    Bsz, S, _ = z.shape
    pad = SSD_CONV // 2
    xbc = lax.conv_general_dilated(xbc, conv_w[:, None, :].astype(xbc.dtype), window_strides=(1,),
                                   padding=[(pad, pad)], dimension_numbers=('NWC', 'WIO', 'NWC'),
                                   feature_group_count=SSD_XBC)
    xbc = jax.nn.silu((xbc + conv_b).astype(f32))
    nbc = SSD_GROUPS * SSD_D_STATE
    xs = xbc[..., :SSD_D_INNER].reshape(Bsz, S, SSD_HEADS, SSD_HEAD_DIM)
    bs = xbc[..., SSD_D_INNER:SSD_D_INNER + nbc].reshape(Bsz, S, SSD_GROUPS, SSD_D_STATE)
    cs = xbc[..., SSD_D_INNER + nbc:].reshape(Bsz, S, SSD_GROUPS, SSD_D_STATE)
    dt_f = jax.nn.softplus(dt_f_raw.astype(f32) + dt_bias_f.astype(f32))
    dt_b = jax.nn.softplus(dt_b_raw.astype(f32) + dt_bias_b.astype(f32))
    a_f = -jnp.exp(a_log_f.astype(f32))
    a_b = -jnp.exp(a_log_b.astype(f32))
    y_f = ssd_chunked(xs, dt_f, a_f, bs, cs)
    y_b = flip_seq(ssd_chunked(flip_seq(xs), flip_seq(dt_b), a_b, flip_seq(bs), flip_seq(cs)))
    y = y_f + y_b + d_skip.astype(f32)[:, None] * xs
    y = y.reshape(Bsz, S, SSD_D_INNER) * jax.nn.silu(z.astype(f32))
    yg = y.reshape(Bsz, S, SSD_GROUPS, SSD_D_INNER // SSD_GROUPS)
    yg = yg * lax.rsqrt(jnp.mean(yg * yg, axis=-1, keepdims=True) + RMS_EPS)
    return (yg.reshape(Bsz, S, SSD_D_INNER) * norm_w.astype(f32)).astype(dtype)


def hgrn2_chunked(q, k, v, log_f):
    Bsz, S = q.shape[:2]
    nc = S // HG_CHUNK
    T = HG_CHUNK

    def to_chunks(t):
        return jnp.moveaxis(t.reshape(Bsz, nc, T, HG_HEADS, HG_HEAD_DIM), 1, 0)

    causal_in_chunk = jnp.tril(jnp.ones((T, T), dtype=bool))[None, :, :, None, None]

    def step(state, inp):
        qc, kc, vc, gc = inp
        gcs = jnp.cumsum(gc, axis=1)
        o_inter = jnp.einsum('bthk,bhkv->bthv', qc * jnp.exp(gcs), state)
        rel = gcs[:, :, None] - gcs[:, None, :]
        rel = jnp.exp(jnp.where(causal_in_chunk, rel, -jnp.inf))
        scores = jnp.einsum('bthk,bshk,btshk->bhts', qc, kc, rel)
        o_intra = jnp.einsum('bhts,bshv->bthv', scores, vc)
        g_last = gcs[:, -1]
        k_dec = kc * jnp.exp(g_last[:, None] - gcs)
        state = state * jnp.exp(g_last)[..., None] + jnp.einsum('bshk,bshv->bhkv', k_dec, vc)
        return state, o_inter + o_intra

    state0 = jnp.zeros((Bsz, HG_HEADS, HG_HEAD_DIM, HG_HEAD_DIM), jnp.float32)
    _, o = lax.scan(step, state0, (to_chunks(q), to_chunks(k), to_chunks(v), to_chunks(log_f)))
    return jnp.moveaxis(o, 0, 1).reshape(Bsz, S, HG_WIDTH)


def hgrn2_branch(q, f_f_raw, f_b_raw, i, g, lb, norm_w):
    dtype = q.dtype
    f32 = jnp.float32
    Bsz, S, _ = q.shape
    shp = (Bsz, S, HG_HEADS, HG_HEAD_DIM)
    qf = q.astype(f32).reshape(shp)
    v = i.astype(f32).reshape(shp)

    def forget_terms(f_raw):
        fr = f_raw.astype(f32)
        log_f = jnp.log(lb + (1.0 - lb) * jax.nn.sigmoid(fr))
        k = (1.0 - lb) * jax.nn.sigmoid(-fr)
        return k.reshape(shp), log_f.reshape(shp)

    k_f, lf_f = forget_terms(f_f_raw)
    k_b, lf_b = forget_terms(f_b_raw)
    o_f = hgrn2_chunked(qf, k_f, v, lf_f)
    o_b = flip_seq(hgrn2_chunked(flip_seq(qf), flip_seq(k_b), flip_seq(v), flip_seq(lf_b)))
    o = o_f + o_b
    o = o * lax.rsqrt(jnp.mean(o * o, axis=-1, keepdims=True) + RMS_EPS) * norm_w.astype(f32)
    return (o * jax.nn.silu(g.astype(f32))).astype(dtype)


def token_mixer(h, w_in, conv_w, conv_b, dt_bias_f, dt_bias_b, a_log_f, a_log_b, d_skip,
                ssd_norm_w, lb, hg_norm_w, w_ssd_out, w_hg_out, w_mix_out):
    proj = jnp.einsum('bsd,de->bse', h, w_in)
    z, xbc, dtf, dtb, q, ff, fb, i, g, gate_a, gate_b = split_in_proj(proj)
    y_ssd = mamba2_branch(z, xbc, dtf, dtb, conv_w, conv_b, dt_bias_f, dt_bias_b,
                          a_log_f, a_log_b, d_skip, ssd_norm_w)
    y_hg = hgrn2_branch(q, ff, fb, i, g, lb, hg_norm_w)
    ya = jnp.einsum('bsi,id->bsd', y_ssd, w_ssd_out)
    yb = jnp.einsum('bsi,id->bsd', y_hg, w_hg_out)
    merged = jax.nn.sigmoid(gate_a) * ya + jax.nn.sigmoid(gate_b) * yb
    return jnp.einsum('bsd,de->bse', merged, w_mix_out)


def expert_choice_ffn(h, w_router, w_gate, w_up, w_down):
    Bsz, S, _ = h.shape
    cap = EC_CAPACITY_FACTOR * S // N_EXPERTS
    logits = jnp.einsum('bsd,de->bse', h, w_router).astype(jnp.float32)
    affinity = jax.nn.softmax(logits, axis=-1)
    gate_vals, tok_idx = lax.top_k(jnp.swapaxes(affinity, 1, 2), cap)
    bidx = jnp.arange(Bsz)[:, None, None]
    xg = h[bidx, tok_idx]
    hid = jax.nn.silu(jnp.einsum('becd,edf->becf', xg, w_gate)) * jnp.einsum('becd,edf->becf', xg, w_up)
    y = jnp.einsum('becf,efd->becd', hid, w_down) * gate_vals[..., None].astype(h.dtype)
    return jnp.zeros_like(h).at[bidx, tok_idx].add(y)


def setup_inputs(seed: int = 0) -> dict:
    key = jax.random.key(seed)
    ks = jax.random.split(key, 32)
    f32 = jnp.float32
    L = DEPTH

    def nrm(k, shape, scale):
        return jax.random.normal(k, shape, f32) * scale

    dt_f = jnp.exp(jax.random.uniform(ks[11], (L, SSD_HEADS), f32, math.log(1e-3), math.log(1e-1)))
    dt_b = jnp.exp(jax.random.uniform(ks[12], (L, SSD_HEADS), f32, math.log(1e-3), math.log(1e-1)))
    return {
        'x': nrm(ks[0], (BATCH, SEQ, D_MODEL), 1.0),
        'c': nrm(ks[1], (BATCH, D_MODEL), 1.0),
        'w_ada': nrm(ks[2], (L, D_MODEL, N_MOD * D_MODEL), 0.5 * D_MODEL ** -0.5),
        'b_ada': nrm(ks[3], (L, N_MOD * D_MODEL), 0.02),
        'norm_pre_mix': 1.0 + nrm(ks[4], (L, D_MODEL), 0.05),
        'norm_post_mix': 1.0 + nrm(ks[5], (L, D_MODEL), 0.05),
        'norm_pre_ffn': 1.0 + nrm(ks[6], (L, D_MODEL), 0.05),
        'norm_post_ffn': 1.0 + nrm(ks[7], (L, D_MODEL), 0.05),
        'w_in': nrm(ks[8], (L, D_MODEL, D_IN_PROJ), D_MODEL ** -0.5),
        'conv_w': nrm(ks[9], (L, SSD_CONV, SSD_XBC), SSD_CONV ** -0.5),
        'conv_b': nrm(ks[10], (L, SSD_XBC), 0.02),
        'dt_bias_fwd': dt_f + jnp.log(-jnp.expm1(-dt_f)),
        'dt_bias_bwd': dt_b + jnp.log(-jnp.expm1(-dt_b)),
        'a_log_fwd': jnp.log(jax.random.uniform(ks[13], (L, SSD_HEADS), f32, 1.0, 16.0)),
        'a_log_bwd': jnp.log(jax.random.uniform(ks[14], (L, SSD_HEADS), f32, 1.0, 16.0)),
        'd_skip': 1.0 + nrm(ks[15], (L, SSD_HEADS), 0.1),
        'ssd_norm_w': 1.0 + nrm(ks[16], (L, SSD_D_INNER), 0.05),
        'hg_lower_bound': nrm(ks[17], (DEPTH + 1, HG_WIDTH), 1.0),
        'hg_norm_w': 1.0 + nrm(ks[18], (L, HG_WIDTH), 0.05),
        'w_ssd_out': nrm(ks[19], (L, SSD_D_INNER, D_MODEL), SSD_D_INNER ** -0.5),
        'w_hg_out': nrm(ks[20], (L, HG_WIDTH, D_MODEL), HG_WIDTH ** -0.5),
        'w_mix_out': nrm(ks[21], (L, D_MODEL, D_MODEL), D_MODEL ** -0.5),
        'w_router': nrm(ks[22], (L, D_MODEL, N_EXPERTS), D_MODEL ** -0.5),
        'w_gate': nrm(ks[23], (L, N_EXPERTS, D_MODEL, D_FF_EXPERT), D_MODEL ** -0.5),
        'w_up': nrm(ks[24], (L, N_EXPERTS, D_MODEL, D_FF_EXPERT), D_MODEL ** -0.5),
        'w_down': nrm(ks[25], (L, N_EXPERTS, D_FF_EXPERT, D_MODEL), D_FF_EXPERT ** -0.5),
    }


def reference(x, c, w_ada, b_ada, norm_pre_mix, norm_post_mix, norm_pre_ffn, norm_post_ffn,
              w_in, conv_w, conv_b, dt_bias_fwd, dt_bias_bwd, a_log_fwd, a_log_bwd, d_skip,
              ssd_norm_w, hg_lower_bound, hg_norm_w, w_ssd_out, w_hg_out, w_mix_out,
              w_router, w_gate, w_up, w_down):
    lower_bounds = jnp.cumsum(jax.nn.softmax(hg_lower_bound.astype(jnp.float32), axis=0), axis=0)
    c_act = jax.nn.silu(c)
    for l in range(DEPTH):
        mod = jnp.einsum('bd,de->be', c_act, w_ada[l]) + b_ada[l]
        sh_m, sc_m, g_m, sh_f, sc_f, g_f = jnp.split(mod[:, None, :], N_MOD, axis=-1)
        h = rms_norm(x, norm_pre_mix[l]) * (1.0 + sc_m) + sh_m
        y = token_mixer(h, w_in[l], conv_w[l], conv_b[l], dt_bias_fwd[l], dt_bias_bwd[l],
                        a_log_fwd[l], a_log_bwd[l], d_skip[l], ssd_norm_w[l], lower_bounds[l],
                        hg_norm_w[l], w_ssd_out[l], w_hg_out[l], w_mix_out[l])
        x = x + g_m * rms_norm(y, norm_post_mix[l])
        h = rms_norm(x, norm_pre_ffn[l]) * (1.0 + sc_f) + sh_f
        y = expert_choice_ffn(h, w_router[l], w_gate[l], w_up[l], w_down[l])
        x = x + g_f * rms_norm(y, norm_post_ffn[l])
    return x
```

```python
import numpy as np
from contextlib import ExitStack
import concourse.bass as bass
import concourse.mybir as mybir

F32 = mybir.dt.float32
BF16 = mybir.dt.bfloat16
I32 = mybir.dt.int32
ACT = mybir.ActivationFunctionType
ALU = mybir.AluOpType
AX = mybir.AxisListType

NQ = 8


class Buf:
    __slots__ = ("t", "w_dma", "w_cmp", "r_dma", "r_cmp", "name")

    def __init__(self, t, name):
        self.t = t
        self.name = name
        self.w_dma = {}
        self.w_cmp = {}
        self.r_dma = {}
        self.r_cmp = {}

    def __getitem__(self, k):
        return self.t[k]


class Op:
    __slots__ = ("eng", "fn", "deps_c", "deps_d", "kind", "needed", "sem", "val", "dkey", "dval")


class K:
    def __init__(self, nc):
        self.nc = nc
        self.ops = []
        self.es = ExitStack()
        self.dma_cnt = {"sp": 0, "pool": 0, "act": 0}
        self.ncoll = 0
        self.uid = 0
        self.pes = None
        self.emitted = 0
        self.bar_idx = -1
        self.inited = False
        self.nwait = 0

    def sb(self, name, shape, dtype=F32):
        self.uid += 1
        es = self.pes if self.pes is not None else self.es
        t = es.enter_context(self.nc.sbuf_tensor(f"{name}_{self.uid}", list(shape), dtype))
        return Buf(t, name)

    def ps(self, name, shape, dtype=F32):
        self.uid += 1
        es = self.pes if self.pes is not None else self.es
        t = es.enter_context(self.nc.psum_tensor(f"{name}_{self.uid}", list(shape), dtype))
        return Buf(t, name)

    def dram(self, name, shape, dtype=F32):
        t = self.nc.dram_tensor(name, list(shape), dtype)
        return Buf(t, name)

    def ext(self, name, shape, dtype, kind):
        t = self.nc.dram_tensor(name, list(shape), dtype, kind=kind)
        return Buf(t, name)

    def _record(self, eng, fn, r, w, kind, acc):
        op = Op()
        op.eng = eng
        op.fn = fn
        op.kind = kind
        op.needed = False
        op.sem = None
        op.val = None
        dc = {}
        dd = {}

        def add_c(m):
            for e, i in m.items():
                if dc.get(e, -1) < i:
                    dc[e] = i

        def add_d(m):
            for k, v in m.items():
                if dd.get(k, 0) < v:
                    dd[k] = v

        for b in r:
            add_c(b.w_cmp)
            add_d(b.w_dma)
        for b in w:
            if not acc:
                add_c(b.w_cmp)
                add_d(b.w_dma)
                add_c(b.r_cmp)
                add_d(b.r_dma)
            else:
                add_c({e: i for e, i in b.w_cmp.items() if e != eng})
                add_d({kk: v for kk, v in b.w_dma.items() if kk[0] != eng})
                add_c({e: i for e, i in b.r_cmp.items() if e != eng})
                add_d({kk: v for kk, v in b.r_dma.items() if kk[0] != eng})
        idx = len(self.ops)
        op.dkey = None
        if kind == "dma":
            i = self.dma_cnt[eng]
            self.dma_cnt[eng] = i + 1
            op.dkey = (eng, i % NQ)
            op.dval = 16 * (i // NQ + 1)
        elif kind == "coll":
            op.dkey = ("coll", self.ncoll % NQ)
            op.dval = self.ncoll // NQ + 1
            self.ncoll += 1
        for b in r:
            if op.dkey is not None:
                if b.r_dma.get(op.dkey, 0) < op.dval:
                    b.r_dma[op.dkey] = op.dval
            else:
                b.r_cmp[eng] = idx
        for b in w:
            if not acc:
                b.w_cmp = {}
                b.w_dma = {}
                b.r_cmp = {}
                b.r_dma = {}
            if op.dkey is not None:
                if b.w_dma.get(op.dkey, 0) < op.dval:
                    b.w_dma[op.dkey] = op.dval
            else:
                b.w_cmp[eng] = idx
        op.deps_c = dc
        op.deps_d = dd
        self.ops.append(op)
        return op

    def op(self, eng, fn, r=(), w=(), acc=False):
        return self._record(eng, fn, r, w, "cmp", acc)

    def dma(self, q, out, in_, r=(), w=(), acc=False, **kw):
        nc = self.nc
        e = {"sp": nc.sync, "pool": nc.gpsimd, "act": nc.scalar}[q]
        return self._record(q, lambda: e.dma_start(out=out, in_=in_, **kw), r, w, "dma", acc)

    def allgather(self, src, dst):
        nc = self.nc
        import os
        if os.environ.get("NO_COLL"):
            rows = src.t.shape[0]
            for r0 in range(0, rows, 128):
                r1 = min(rows, r0 + 128)
                self.dma("sp", dst[r0:r1, :], src[r0:r1, :], r=[src], w=[dst], acc=True)
            return
        rows = src.t.shape[0]; cols = src.t.shape[1]
        isz = 2 if src.t.dtype == BF16 else 4
        cr = max(1, min(rows, (512 * 1024) // (cols * isz)))
        while rows % cr:
            cr -= 1
        self.uid += 1
        u = self.uid
        NR = 3
        ring = [(self.dram(f"agi{u}_{b}", [cr, cols], src.t.dtype), self.dram(f"ag2{u}_{b}", [2 * cr, cols], src.t.dtype),
                 self.dram(f"ag8{u}_{b}", [8 * cr, cols], src.t.dtype)) for b in range(NR)]
        g4 = [[0, 1, 2, 3], [4, 5, 6, 7]]
        g2 = [[0, 4], [1, 5], [2, 6], [3, 7]]
        def coll(groups, a, b_):
            return self._record(
                "pool",
                lambda: nc.gpsimd.collective_compute(
                    "AllGather", ALU.bypass, replica_groups=groups,
                    ins=[a.t.ap().opt()], outs=[b_.t.ap().opt()]),
                [a], [b_], "coll", False)
        starts = list(range(0, rows, cr))
        n = len(starts)
        for i in range(n + 1):
            if i < n:
                cin, c2, c8 = ring[i % NR]
                self.dma("sp", cin[:, :], src[starts[i]:starts[i] + cr, :], r=[src], w=[cin])
                coll(g2, cin, c2)
            if i >= 1:
                cin, c2, c8 = ring[(i - 1) % NR]
                r0 = starts[i - 1]
                coll(g4, c2, c8)
                for j in range(8):
                    core = (j // 2) + 4 * (j % 2)
                    self.dma("sp", dst[core * rows + r0: core * rows + r0 + cr, :], c8[j * cr:(j + 1) * cr, :], r=[c8], w=[dst], acc=True)

    def phase_begin(self):
        assert self.pes is None
        self.pes = ExitStack()

    def phase_end(self):
        self.barrier()
        self.flush()
        self.pes.close()
        self.pes = None

    def barrier(self):
        marks = {}
        for e in ("act", "dve", "pool"):
            op = Op()
            op.eng = e; op.fn = None; op.kind = "mark"; op.needed = True; op.sem = None; op.val = None
            op.deps_c = {}; op.deps_d = {}; op.dkey = None
            marks[e] = len(self.ops)
            self.ops.append(op)
        dd = {}
        for q, n in self.dma_cnt.items():
            for s_ in range(NQ):
                cnt = (n - s_ + NQ - 1) // NQ if n > s_ else 0
                if cnt > 0:
                    dd[(q, s_)] = 16 * cnt
        for s_ in range(NQ):
            cnt = (self.ncoll - s_ + NQ - 1) // NQ if self.ncoll > s_ else 0
            if cnt > 0:
                dd[("coll", s_)] = cnt
        for e in ("pe", "act", "dve", "pool", "sp"):
            op = Op()
            op.eng = e; op.fn = None; op.kind = "barwait"; op.needed = False; op.sem = None; op.val = None
            op.deps_c = dict(marks); op.deps_d = dict(dd); op.dkey = None
            self.ops.append(op)
        self.bar_idx = len(self.ops)

    def _init_emit(self):
        nc = self.nc
        self.engs = {"pe": nc.tensor, "act": nc.scalar, "dve": nc.vector, "pool": nc.gpsimd, "sp": nc.sync}
        self.csem = {e: self.es.enter_context(nc.semaphore(f"c_{e}")) for e in ("pe", "act", "dve", "pool")}
        self.ccnt = {e: 0 for e in self.csem}
        self.dsem = {}
        for q in ("sp", "pool", "act"):
            for s_ in range(NQ):
                self.dsem[(q, s_)] = self.es.enter_context(nc.semaphore(f"d_{q}{s_}"))
        for s_ in range(NQ):
            self.dsem[("coll", s_)] = self.es.enter_context(nc.semaphore(f"cc{s_}"))
        self.seen = {e: {} for e in self.engs}
        self.marktile = {e: self.es.enter_context(nc.sbuf_tensor(f"mark_{e}", [1, 8], F32)) for e in ("act", "dve", "pool")}
        self.markps = self.es.enter_context(nc.sbuf_tensor("mark_pe_in", [1, 8], BF16))
        nc.vector.memset(self.marktile["act"][:], 0.0)
        self.inited = True

    def flush(self):
        nc = self.nc
        if not self.inited:
            self._init_emit()
        ops = self.ops
        start = self.emitted
        for o in ops[start:]:
            for e, i in o.deps_c.items():
                ops[i].needed = True
        engs, csem, dsem, ccnt = self.engs, self.csem, self.dsem, self.ccnt
        for idx in range(start, len(ops)):
            o = ops[idx]
            e = o.eng
            h = engs[e]
            sn = self.seen[e]
            for de, di in o.deps_c.items():
                d = ops[di]
                if de == "pe" and e == "pe":
                    continue
                if d.val is None:
                    continue
                key = ("c", de)
                if sn.get(key, 0) >= d.val:
                    continue
                h.wait_ge(csem[de], d.val)
                self.nwait += 1
                sn[key] = d.val
            for dk, dv in o.deps_d.items():
                if sn.get(dk, 0) >= dv:
                    continue
                if dk not in dsem:
                    dsem[dk] = self.es.enter_context(nc.semaphore(f"cc{dk[1]}"))
                h.wait_ge(dsem[dk], dv)
                self.nwait += 1
                sn[dk] = dv
            if o.kind == "dma":
                prev = o.dval - 16
                if prev > 0 and sn.get(o.dkey, 0) < prev:
                    h.wait_ge(dsem[o.dkey], prev)
                    self.nwait += 1
                    sn[o.dkey] = prev
                o.fn().then_inc(dsem[o.dkey], 16)
            elif o.kind == "coll":
                prev = o.dval - 1
                if prev > 0 and sn.get(o.dkey, 0) < prev:
                    h.wait_ge(dsem[o.dkey], prev)
                    self.nwait += 1
                    sn[o.dkey] = prev
                o.fn().then_inc(dsem[o.dkey])
            elif o.kind == "barwait":
                pass
            else:
                if o.kind == "mark":
                    if e == "pe":
                        ins = nc.tensor.nop()
                    elif e == "act":
                        ins = nc.scalar.copy(out=self.marktile["act"][:, 0:4], in_=self.marktile["act"][:, 4:8])
                    elif e == "dve":
                        ins = nc.vector.memset(self.marktile["dve"][:], 0.0)
                    else:
                        ins = nc.gpsimd.memset(self.marktile["pool"][:], 0.0)
                else:
                    ins = o.fn()
                if o.needed:
                    ccnt[e] += 1
                    ins.then_inc(csem[e], 1)
                    o.val = ccnt[e]
            o.fn = None
        self.emitted = len(ops)

    def finish(self):
        assert self.pes is None
        self.barrier()
        self.flush()
        self.stats = dict(nops=len(self.ops), nwait=self.nwait)
        self.es.close()


class Cfg:
    def __init__(self, D=4096, S=8192):
        self.D = D; self.S = S
        self.KT = D // 128
        self.DC = D // 8; self.DCT = self.DC // 128
        self.DI = 2 * D; self.DIc = self.DI // 8; self.XT = self.DIc // 128
        self.P = 64; self.N = 128; self.R = self.DIc // 64
        self.HHc = D // 128 // 8; self.HW = self.HHc * 128
        self.E = 16; self.cap = 2 * S // 16; self.F = D // 2; self.FT = self.F // 128
        self.NT = S // 128
        c = self
        o = 0
        c.oz = o; o += c.DIc
        c.ox = o; o += c.DIc
        c.oB = o; o += c.N
        c.oC = o; o += c.N
        c.oq = o; o += c.HW
        c.off = o; o += c.HW
        c.ofb = o; o += c.HW
        c.oi = o; o += c.HW
        c.og = o; o += c.HW
        c.oga = o; o += c.DC
        c.ogb = o; o += c.DC
        c.odt = o; o += 2 * c.R
        c.NCOL = o
        c.CT3 = (c.DIc + 2 * c.N) // 128


def _eng(nc, e):
    return {"dve": nc.vector, "pool": nc.gpsimd}[e]


class Prog:
    def __init__(self, cfg, debug=()):
        self.cfg = cfg
        self.debug = set(debug)
        self.nc = bass.Bass("TRN2", target_bir_lowering=False)
        self.k = K(self.nc)
        self.k._init_emit()

    def TT(self, e, out, in0, in1, op, r, w, acc=False):
        en = _eng(self.nc, e)
        return self.k.op(e, lambda: en.tensor_tensor(out=out, in0=in0, in1=in1, op=op), r, w, acc)

    def TS(self, e, out, in0, s1, s2, op0, op1=None, r=(), w=(), acc=False, accum_out=None):
        en = _eng(self.nc, e)
        if op1 is None:
            return self.k.op(e, lambda: en.tensor_scalar(out=out, in0=in0, scalar1=s1, scalar2=None, op0=op0), r, w, acc)
        if accum_out is not None:
            return self.k.op(e, lambda: en.tensor_scalar(out=out, in0=in0, scalar1=s1, scalar2=s2, op0=op0, op1=op1, accum_out=accum_out), r, w, acc)
        return self.k.op(e, lambda: en.tensor_scalar(out=out, in0=in0, scalar1=s1, scalar2=s2, op0=op0, op1=op1), r, w, acc)

    def STT(self, out, in0, scalar, in1, op0, op1, r, w, acc=False):
        nc = self.nc
        return self.k.op("dve", lambda: nc.vector.scalar_tensor_tensor(out=out, in0=in0, scalar=scalar, in1=in1, op0=op0, op1=op1), r, w, acc)

    def AC(self, out, in_, func, r, w, bias=None, scale=None, acc=False, accum_out=None):
        nc = self.nc
        kw = {}
        if bias is not None:
            kw["bias"] = bias
        if scale is not None:
            kw["scale"] = scale
        if accum_out is not None:
            kw["accum_out"] = accum_out
        return self.k.op("act", lambda: nc.scalar.activation(out=out, in_=in_, func=func, **kw), r, w, acc)

    def MM(self, ps, lhsT, rhs, start, stop, r, w, acc=None):
        nc = self.nc
        if acc is None:
            acc = not start
        return self.k.op("pe", lambda: nc.tensor.matmul(ps, lhsT=lhsT, rhs=rhs, start=start, stop=stop), r, w, acc)

    def TR(self, ps, in_, ident, r, w, acc=False):
        nc = self.nc
        return self.k.op("pe", lambda: nc.tensor.transpose(ps, in_, ident), r, w, acc)

    def MEMSET(self, e, ap, val, w, acc=False):
        en = _eng(self.nc, e)
        return self.k.op(e, lambda: en.memset(ap, val), (), w, acc)

    def dbg(self, name, src_buf, shape, dtype=F32):
        if name not in self.debug:
            return
        o = self.k.ext("dbg_" + name, shape, dtype, "ExternalOutput")
        rows = shape[0]
        for r0 in range(0, rows, 128):
            r1 = min(rows, r0 + 128)
            self.k.dma("sp", o[r0:r1, :], src_buf[r0:r1, :], r=[src_buf], w=[o], acc=True)

    def consts(self):
        k, nc = self.k, self.nc
        self.ones_f = k.sb("ones_f", [128, 128]); self.ident_f = k.sb("ident_f", [128, 128])
        self.ones_b = k.sb("ones_b", [128, 128], BF16); self.ident_b = k.sb("ident_b", [128, 128], BF16)
        self.eps = k.sb("eps", [128, 1]); self.onec = k.sb("onec", [128, 1])
        self.maskF = k.sb("maskF", [128, 128]); self.maskB = k.sb("maskB", [128, 128])
        self.MEMSET("pool", self.ones_f[:], 1.0, [self.ones_f])
        self.MEMSET("pool", self.ones_b[:], 1.0, [self.ones_b])
        self.MEMSET("pool", self.eps[:], 1e-6, [self.eps])
        self.MEMSET("pool", self.onec[:], 1.0, [self.onec])
        of, idf, mF, mB = self.ones_f, self.ident_f, self.maskF, self.maskB
        k.op("pool", lambda: nc.gpsimd.affine_select(out=idf[:], in_=of[:], pattern=[[-1, 128]], compare_op=ALU.is_equal, fill=0.0, base=0, channel_multiplier=1), [of], [idf])
        k.op("pool", lambda: nc.gpsimd.affine_select(out=mF[:], in_=of[:], pattern=[[1, 128]], compare_op=ALU.is_ge, fill=0.0, base=0, channel_multiplier=-1), [of], [mF])
        k.op("pool", lambda: nc.gpsimd.affine_select(out=mB[:], in_=of[:], pattern=[[-1, 128]], compare_op=ALU.is_ge, fill=0.0, base=0, channel_multiplier=1), [of], [mB])
        ib = self.ident_b
        k.op("pool", lambda: nc.gpsimd.tensor_copy(out=ib[:], in_=idf[:]), [idf], [ib])

    def gemm(self, segs, C, T, epi, TB=512, t0=0, tag="g", CG=512):
        k = self.k
        nseg = len(segs)
        wr = [[k.sb(f"{tag}w{si}_{b}", [128, segs[si][2], CG], BF16) for b in range(2)] for si in range(nseg)]
        ar = [[k.sb(f"{tag}a{si}_{b}", [128, segs[si][2], TB], BF16) for b in range(2)] for si in range(nseg)]
        psr = [k.ps(f"{tag}ps{b}", [128, TB]) for b in range(3)]
        total = sum(s[2] for s in segs)
        na = 0; npz = 0
        for cg in range((C + CG - 1) // CG):
            c0 = cg * CG; cw = min(CG, C - c0)
            wbs = []
            for si, (act, kt0, nkt, wfn, wbuf) in enumerate(segs):
                wb = wr[si][cg % 2]
                k.dma("pool", wb[:, :, :cw], wfn(c0, cw), r=[wbuf], w=[wb])
                wbs.append(wb)
            for tb in range(T // TB):
                abs_ = []
                for si, (act, kt0, nkt, wfn, wbuf) in enumerate(segs):
                    ab = ar[si][na % 2]
                    src = act.t.ap().rearrange("(kt p) s -> p kt s", p=128)[:, kt0:kt0 + nkt, t0 + tb * TB: t0 + (tb + 1) * TB]
                    k.dma("sp", ab[:], src, r=[act], w=[ab])
                    abs_.append(ab)
                na += 1
                for ct in range((cw + 127) // 128):
                    m = min(128, cw - ct * 128)
                    ps = psr[npz % 3]; npz += 1
                    i = 0
                    for si, (act, kt0, nkt, wfn, wbuf) in enumerate(segs):
                        for kt in range(nkt):
                            self.MM(ps[:m, :], wbs[si][:, kt, ct * 128: ct * 128 + m], abs_[si][:, kt, :],
                                    i == 0, i == total - 1, [wbs[si], abs_[si]], [ps])
                            i += 1
                    epi(c0 + ct * 128, m, tb, ps)

    def ag(self, src, dst):
        self.k.allgather(src, dst)

    def declare_inputs(self):
        c, k = self.cfg, self.k
        I = {}
        def inp(name, shape, dt=F32):
            I[name] = k.ext(name, shape, dt, "ExternalInput")
        inp("xT", [c.DC, c.S]); inp("c_l", [128, c.KT]); inp("w_ada", [c.D, c.D]); inp("b_ada_l", [128, c.KT])
        inp("npm_l", [128, c.KT]); inp("npf_l", [128, c.KT]); inp("npom_l", [128, c.DCT]); inp("npof_l", [128, c.DCT])
        inp("w_in", [c.D, c.NCOL]); inp("convw_l", [128, c.CT3, 5]); inp("convb_l", [128, c.CT3])
        inp("par8", [8 * c.R, 8]); inp("par2", [2 * c.R, 2 + 2 * c.R]); inp("par2b", [2 * c.R, 2])
        inp("dskip_bc", [128, c.DIc]); inp("ssdw_bc", [128, c.DIc])
        inp("lb_l", [128, c.HHc, 2]); inp("hgw_l", [128, c.HHc])
        inp("w_so", [c.DI, c.DC]); inp("w_ho", [c.D, c.DC]); inp("w_mo", [c.D, c.DC])
        inp("w_r_l", [128, c.KT, 16]); inp("w_g", [2 * c.D, c.F]); inp("w_u", [2 * c.D, c.F]); inp("w_d", [16 * c.F, c.DC])
        inp("esel", [16, 2]); inp("onesel", [16, 16 * 128]); inp("cidx", [128, max(1, c.cap // 128)])
        inp("rowmask", [128, 4]); inp("bmask", [128, 256])
        self.I = I
        self.outT = k.ext("outT", [c.DC, c.S], F32, "ExternalOutput")

    def wview(self, wbuf, kt0, nkt, r0=0):
        def fn(c0, cw):
            return wbuf.t.ap()[r0:, :].rearrange("(kt p) c -> p kt c", p=128)[:, kt0:kt0 + nkt, c0:c0 + cw]
        return fn

    def phase0(self):
        c, k, nc, I = self.cfg, self.k, self.nc, self.I
        KT = c.KT
        self.xTb = k.dram("xTb", [c.DC, c.S]); self.xTf = k.dram("xTf", [c.D, c.S])
        for r0 in range(0, c.DC, 128):
            k.dma("sp", self.xTb[r0:r0 + 128, :], I["xT"][r0:r0 + 128, :], r=[I["xT"]], w=[self.xTb], acc=True)
        self.ag(self.xTb, self.xTf)
        self.A_m = k.sb("A_m", [128, KT]); self.B_m = k.sb("B_m", [128, KT])
        self.A_f = k.sb("A_f", [128, KT]); self.B_f = k.sb("B_f", [128, KT])
        self.Gm = k.sb("Gm", [128, c.DCT]); self.Gf = k.sb("Gf", [128, c.DCT])
        k.phase_begin()
        nj = 6 * KT // 8
        cl = k.sb("cl", [128, KT]); cact = k.sb("cact", [128, KT]); bl = k.sb("bl", [128, KT]); modl = k.sb("modl", [128, KT])
        k.dma("sp", cl[:], I["c_l"][:, :], r=[I["c_l"]], w=[cl])
        k.dma("sp", bl[:], I["b_ada_l"][:, :], r=[I["b_ada_l"]], w=[bl])
        self.AC(cact[:], cl[:], ACT.Silu, [cl], [cact])
        CGW = 512 if c.D >= 512 else c.D
        war = [k.sb(f"wa{b}", [128, KT, CGW]) for b in range(2)]
        psmod = k.ps("psmod", [128, KT])
        wv = I["w_ada"].t.ap().rearrange("(kt p) c -> p kt c", p=128)
        first = True
        for cg in range(c.D // CGW):
            wb = war[cg % 2]
            k.dma("sp", wb[:], wv[:, :, cg * CGW:(cg + 1) * CGW], r=[I["w_ada"]], w=[wb])
            for j in range(CGW // 128):
                col = cg * (CGW // 128) + j
                for kt in range(KT):
                    self.MM(psmod[:, col:col + 1], wb[:, kt, j * 128:(j + 1) * 128], cact[:, kt:kt + 1],
                            kt == 0, kt == KT - 1, [wb, cact], [psmod], acc=not first)
                    first = False
        self.TT("dve", modl[:], psmod[:], bl[:], ALU.add, [psmod, bl], [modl])
        modb = k.dram("modb", [nj, 128]); modg = k.dram("modg", [8 * nj, 128])
        pst = k.ps("pst", [128, 128])
        tsb = k.sb("tsb", [128, 128])
        self.TR(pst[:nj, :], modl[:, 0:nj], self.ident_f[:], [modl, self.ident_f], [pst])
        self.AC(tsb[:nj, :], pst[:nj, :], ACT.Copy, [pst], [tsb])
        k.dma("sp", modb[:, :], tsb[:nj, :], r=[tsb], w=[modb])
        self.ag(modb, modg)
        mod = k.sb("mod", [128, 6 * KT])
        half = 3 * KT
        for hh in range(2):
            gsb = k.sb(f"gsb{hh}", [128, 128])
            k.dma("sp", gsb[:half, :], modg[hh * half:(hh + 1) * half, :], r=[modg], w=[gsb])
            pst2 = k.ps(f"pst2{hh}", [128, 128])
            self.TR(pst2[:, :half], gsb[:half, :], self.ident_f[:half, :half], [gsb, self.ident_f], [pst2])
            self.AC(mod[:, hh * half:(hh + 1) * half], pst2[:, :half], ACT.Copy, [pst2], [mod], acc=(hh == 1))
        nm = k.sb("nm", [128, 2 * KT + 2 * c.DCT])
        k.dma("sp", nm[:, 0:KT], I["npm_l"][:, :], r=[I["npm_l"]], w=[nm])
        k.dma("sp", nm[:, KT:2 * KT], I["npf_l"][:, :], r=[I["npf_l"]], w=[nm], acc=True)
        k.dma("sp", nm[:, 2 * KT:2 * KT + c.DCT], I["npom_l"][:, :], r=[I["npom_l"]], w=[nm], acc=True)
        k.dma("sp", nm[:, 2 * KT + c.DCT:], I["npof_l"][:, :], r=[I["npof_l"]], w=[nm], acc=True)
        self.STT(self.A_m[:], mod[:, KT:2 * KT], 1.0, nm[:, 0:KT], ALU.add, ALU.mult, [mod, nm], [self.A_m])
        self.k.op("dve", (lambda a=self.B_m, m=mod: nc.vector.tensor_copy(out=a[:], in_=m[:, 0:KT])), [mod], [self.B_m])
        self.STT(self.A_f[:], mod[:, 4 * KT:5 * KT], 1.0, nm[:, KT:2 * KT], ALU.add, ALU.mult, [mod, nm], [self.A_f])
        self.k.op("dve", (lambda a=self.B_f, m=mod: nc.vector.tensor_copy(out=a[:], in_=m[:, 3 * KT:4 * KT])), [mod], [self.B_f])
        self.TT("dve", self.Gm[:], modl[:, nj:nj + c.DCT], nm[:, 2 * KT:2 * KT + c.DCT], ALU.mult, [modl, nm], [self.Gm])
        self.TT("dve", self.Gf[:], modl[:, nj + c.DCT:nj + 2 * c.DCT], nm[:, 2 * KT + c.DCT:], ALU.mult, [modl, nm], [self.Gf])
        k.phase_end()

    def norm_phase(self, src, A, B, dst, router=False):
        c, k, nc, I = self.cfg, self.k, self.nc, self.I
        KT = c.KT; TBn = 256
        k.phase_begin()
        xr = [k.sb(f"nx{b}", [128, KT, TBn]) for b in range(2)]
        sq = [k.sb(f"nsq{b}", [128, KT, TBn]) for b in range(1)]
        hb = [k.sb(f"nhb{b}", [128, KT, TBn], BF16) for b in range(2)]
        rs = [k.sb(f"nrs{b}", [128, TBn]) for b in range(2)]
        pss = [k.ps(f"nps{b}", [128, TBn]) for b in range(2)]
        sv = src.t.ap().rearrange("(kt p) s -> p kt s", p=128)
        dv = dst.t.ap().rearrange("(kt p) s -> p kt s", p=128)
        if router:
            wr = k.sb("wr", [128, KT, 16])
            k.dma("sp", wr[:], I["w_r_l"][:, :, :], r=[I["w_r_l"]], w=[wr])
            psl = [k.ps(f"psl{b}", [16, TBn]) for b in range(2)]
            pstr = [k.ps(f"pstr{b}", [128, 512]) for b in range(2)]
            htk = [k.sb(f"htk{b}", [128, c.D], BF16) for b in range(2)]
            lgt = [k.sb(f"lgt{b}", [16, TBn]) for b in range(2)]
        A3 = A[:, :].unsqueeze(2).broadcast_to([128, KT, TBn])
        B3 = B[:, :].unsqueeze(2).broadcast_to([128, KT, TBn])
        for tb in range(c.S // TBn):
            x = xr[tb % 2]; s = sq[0]; h = hb[tb % 2]; r_ = rs[tb % 2]; ps = pss[tb % 2]
            k.dma("sp", x[:], sv[:, :, tb * TBn:(tb + 1) * TBn], r=[src], w=[x])
            self.AC(s[:].rearrange("p a b -> p (a b)"), x[:].rearrange("p a b -> p (a b)"), ACT.Square, [x], [s])
            for kt in range(KT):
                self.MM(ps[:], self.ones_f[:], s[:, kt, :], kt == 0, kt == KT - 1, [self.ones_f, s], [ps])
            self.AC(r_[:], ps[:], ACT.Sqrt, [ps, self.eps], [r_], bias=self.eps[:], scale=1.0 / c.D)
            nc_ = nc
            k.op("dve", (lambda r_=r_: nc_.vector.reciprocal(out=r_[:], in_=r_[:])), [r_], [r_])
            self.TT("dve", s[:], x[:], r_[:].unsqueeze(1).broadcast_to([128, KT, TBn]), ALU.mult, [x, r_], [s])
            self.TT("pool", s[:], s[:], A3, ALU.mult, [s, A], [s])
            if not router:
                self.TT("dve", h[:], s[:], B3, ALU.add, [s, B], [h])
            else:
                self.TT("dve", s[:], s[:], B3, ALU.add, [s, B], [s])
                self.AC(h[:].rearrange("p a b -> p (a b)"), s[:].rearrange("p a b -> p (a b)"), ACT.Copy, [s], [h])
                pl = psl[tb % 2]
                for kt in range(KT):
                    self.MM(pl[:], wr[:, kt, :], s[:, kt, :], kt == 0, kt == KT - 1, [wr, s], [pl])
                lt = lgt[tb % 2]
                self.TS("dve", lt[:], pl[:], 1.0, None, ALU.mult, None, [pl], [lt])
                k.dma("sp", self.logits[:, tb * TBn:(tb + 1) * TBn], lt[:], r=[lt], w=[self.logits], acc=True)
                for th in range(TBn // 128):
                    ht = htk[(tb * 2 + th) % 2]
                    for g4 in range(KT // 4):
                        pt = pstr[g4 % 2]
                        for q in range(4):
                            kt = g4 * 4 + q
                            self.MM(pt[:, q * 128:(q + 1) * 128], h[:, kt, th * 128:(th + 1) * 128], self.ident_b[:], True, True, [h, self.ident_b], [pt], acc=(q > 0))
                        if g4 % 2 == 0:
                            self.AC(ht[:, g4 * 512:(g4 + 1) * 512], pt[:], ACT.Copy, [pt], [ht], acc=(g4 > 0))
                        else:
                            k.op("pool" if False else "dve", (lambda ht=ht, pt=pt, g4=g4: nc_.vector.tensor_copy(out=ht[:, g4 * 512:(g4 + 1) * 512], in_=pt[:])), [pt], [ht], acc=True)
                    t_ = tb * (TBn // 128) + th
                    k.dma("sp", self.h2tok[t_ * 128:(t_ + 1) * 128, :], ht[:], r=[ht], w=[self.h2tok], acc=True)
            k.dma("sp", dv[:, :, tb * TBn:(tb + 1) * TBn], h[:], r=[h], w=[dst], acc=True)
        k.phase_end()

    def phase2(self):
        c, k, nc, I = self.cfg, self.k, self.nc, self.I
        self.projT = k.dram("projT", [c.NCOL, c.S])
        k.phase_begin()
        ot = [k.sb(f"po{b}", [128, 512]) for b in range(3)]
        cnt = [0]
        def func_for(c0):
            if c0 < c.ox: return ACT.Silu
            if c.og <= c0 < c.oga: return ACT.Silu
            if c.oga <= c0 < c.odt: return ACT.Sigmoid
            return ACT.Copy
        def epi(c0, m, tb, ps):
            o = ot[cnt[0] % 3]; cnt[0] += 1
            self.AC(o[:m, :], ps[:m, :], func_for(c0), [ps], [o])
            k.dma("sp", self.projT[c0:c0 + m, tb * 512:(tb + 1) * 512], o[:m, :], r=[o], w=[self.projT], acc=True)
        self.gemm([(self.hT, 0, c.KT, self.wview(I["w_in"], 0, c.KT), I["w_in"])], c.NCOL, c.S, epi, TB=512, tag="ip", CG=1024)
        k.phase_end()

    def phase3_prep(self):
        c, k, nc, I = self.cfg, self.k, self.nc, self.I
        R = c.R; PR = 8 * R; S = c.S; NC = c.NT
        self.Qd = k.dram("Qd", [PR, S]); self.etd = k.dram("etd", [PR, NC])
        self.xcT = k.dram("xcT", [c.DIc, S]); self.BCb = k.dram("BCb", [2 * c.N, S], BF16)
        k.phase_begin()
        par = k.sb("par", [PR, 8]); k.dma("sp", par[:], I["par8"][:, :], r=[I["par8"]], w=[par])
        b1 = k.sb("b1", [PR, S]); b2 = k.sb("b2", [PR, S]); b3 = k.sb("b3", [PR, S]); b4 = k.sb("b4", [PR, S]); b5 = k.sb("b5", [PR, S])
        rm = k.sb("rm128", [PR, S], BF16)
        aexp = k.sb("aexp", [PR, 1]); totc = k.sb("totc", [PR, NC]); etot = k.sb("etot", [PR, NC])
        for blk in range(4):
            k.dma("sp", b1[blk * 2 * R:(blk + 1) * 2 * R, :], self.projT[c.odt:c.odt + 2 * R, :], r=[self.projT], w=[b1], acc=(blk > 0))
        self.MEMSET("pool", rm[:], 1.0, [rm])
        self.MEMSET("pool", rm[:].rearrange("p (c t) -> p c t", t=128)[:, :, 0:1], 0.0, [rm])
        self.AC(aexp[:], par[:, 1:2], ACT.Exp, [par], [aexp])
        self.AC(b1[:], b1[:], ACT.Exp, [b1, par], [b1], bias=par[:, 0:1])
        self.AC(b1[:], b1[:], ACT.Ln, [b1, self.onec], [b1], bias=self.onec[:PR, :])
        self.TS("dve", b2[:], b1[:], aexp[:, 0:1], -1.0, ALU.mult, ALU.mult, [b1, aexp], [b2])
        k.op("dve", lambda: nc.vector.tensor_tensor_scan(out=b3[:], data0=rm[:], data1=b2[:], initial=0.0, op0=ALU.mult, op1=ALU.add), [rm, b2], [b3])
        cs3 = b3[:].rearrange("p (c t) -> p c t", t=128)
        k.op("dve", lambda: nc.vector.tensor_copy(out=totc[:].unsqueeze(2), in_=cs3[:, :, 127:128]), [b3], [totc])
        tot3 = totc[:].unsqueeze(2).broadcast_to([PR, NC, 128])
        v3 = lambda b: b[:].rearrange("p (c t) -> p c t", t=128)
        self.TT("dve", b4[:], b2[:], b3[:], ALU.subtract, [b2, b3], [b4])
        self.TT("dve", v3(b4), v3(b4), tot3, ALU.add, [b4, totc], [b4])
        self.TS("dve", b5[:], b3[:], par[:, 6:7], None, ALU.mult, None, [b3, par], [b5])
        self.STT(b5[:], b4[:], par[:, 7:8], b5[:], ALU.mult, ALU.add, [b4, par, b5], [b5])
        self.TT("dve", v3(b4), tot3, v3(b3), ALU.subtract, [totc, b3], [b4])
        self.TS("dve", b4[:], b4[:], par[:, 6:7], None, ALU.mult, None, [b4, par], [b4])
        self.TT("dve", b2[:], b3[:], b2[:], ALU.subtract, [b3, b2], [b2])
        self.STT(b4[:], b2[:], par[:, 7:8], b4[:], ALU.mult, ALU.add, [b2, par, b4], [b4])
        self.AC(b4[:], b4[:], ACT.Exp, [b4], [b4])
        self.AC(b3[:], b5[:], ACT.Exp, [b5], [b3])
        self.TS("dve", b1[:], b1[:], par[:, 2:3], None, ALU.mult, None, [b1, par], [b1])
        self.STT(b1[:], b5[:], par[:, 3:4], b1[:], ALU.mult, ALU.add, [b5, par, b1], [b1])
        self.STT(b1[:], b4[:], par[:, 4:5], b1[:], ALU.mult, ALU.add, [b4, par, b1], [b1])
        self.STT(b1[:], b3[:], par[:, 5:6], b1[:], ALU.mult, ALU.add, [b3, par, b1], [b1])
        self.AC(etot[:], totc[:], ACT.Exp, [totc], [etot])
        k.dma("sp", self.Qd[:, :], b1[:], r=[b1], w=[self.Qd])
        k.dma("sp", self.etd[:, :], etot[:], r=[etot], w=[self.etd])
        k.phase_end()
        k.phase_begin()
        cw = k.sb("cw", [128, c.CT3, 5]); cb = k.sb("cb", [128, c.CT3])
        k.dma("sp", cw[:], I["convw_l"][:, :, :], r=[I["convw_l"]], w=[cw])
        k.dma("sp", cb[:], I["convb_l"][:, :], r=[I["convb_l"]], w=[cb])
        xin = [k.sb(f"xin{b}", [128, S + 4]) for b in range(2)]
        acc_ = [k.sb(f"cacc{b}", [128, S]) for b in range(2)]
        ob = [k.sb(f"cob{b}", [128, S], BF16) for b in range(1)]
        for ci in range(c.CT3):
            xi = xin[ci % 2]; a = acc_[ci % 2]
            self.MEMSET("pool", xi[:, 0:2], 0.0, [xi])
            self.MEMSET("pool", xi[:, S + 2:S + 4], 0.0, [xi], acc=True)
            k.dma("sp", xi[:, 2:S + 2], self.projT[c.ox + ci * 128: c.ox + (ci + 1) * 128, :], r=[self.projT], w=[xi], acc=True)
            self.TS("dve", a[:], xi[:, 0:S], cw[:, ci, 0:1], None, ALU.mult, None, [xi, cw], [a])
            for j in range(1, 5):
                self.STT(a[:], xi[:, j:S + j], cw[:, ci, j:j + 1], a[:], ALU.mult, ALU.add, [xi, cw, a], [a])
            if ci < c.XT:
                self.AC(a[:], a[:], ACT.Silu, [a, cb], [a], bias=cb[:, ci:ci + 1])
                k.dma("sp", self.xcT[ci * 128:(ci + 1) * 128, :], a[:], r=[a], w=[self.xcT], acc=True)
            else:
                o = ob[0]
                self.AC(o[:], a[:], ACT.Silu, [a, cb], [o], bias=cb[:, ci:ci + 1])
                j0 = (ci - c.XT) * 128
                k.dma("sp", self.BCb[j0:j0 + 128, :], o[:], r=[o], w=[self.BCb], acc=True)
        k.phase_end()

    def ssd_sweep(self, d):
        c, k, nc, I = self.cfg, self.k, self.nc, self.I
        R = c.R; PR = 8 * R; S = c.S; NC = c.NT; DIc = c.DIc; XT = c.XT
        HWD = min(512, DIc); H = DIc // HWD; HPH = HWD // 64; TPH = HWD // 128; G4 = HPH // 4
        if d == 0:
            self.yf = k.dram("yf", [S, DIc])
        else:
            self.yssd_b = k.dram("yssd_b", [DIc, S], BF16)
        k.phase_begin()
        idf = self.ident_f
        et = k.sb("et", [R, NC]); k.dma("sp", et[:], self.etd[d * R:(d + 1) * R, :], r=[self.etd], w=[et])
        diagE = k.sb("diagE", [R, NC, R])
        self.TT("dve", diagE[:], idf[0:R, 0:R].unsqueeze(1).broadcast_to([R, NC, R]), et[:].unsqueeze(2).broadcast_to([R, NC, R]), ALU.mult, [idf, et], [diagE])
        edec = k.sb("edec", [128, NC, R])
        negI = k.sb("negI", [R, R, 128])
        self.TS("dve", negI[:], idf[0:R, 0:R].unsqueeze(2).broadcast_to([R, R, 128]), -1.0, None, ALU.mult, None, [idf], [negI])
        xtp = k.ps("xtp", [128, 512]); ydg = k.ps("ydg", [128, 512]); yof = k.ps("yof", [128, 512]); stp = k.ps("stp", [128, 512])
        Dp = [k.ps(f"Dp{b}", [128, 512]) for b in range(2)]
        misc = k.ps("misc", [128, 512]); tb16 = k.ps("tb16", [128, 384])
        cbT = qtp = edp = misc
        btp = ybt = tb16
        dE = diagE[:].rearrange("p a b -> p (a b)"); eD = edec[:].rearrange("p a b -> p (a b)")
        ncol = NC * R
        for b0 in range(0, ncol, 256):
            w_ = min(256, ncol - b0)
            self.MM(edp[:, 256:256 + w_], self.ones_f[0:R, :], dE[:, b0:b0 + w_], True, True, [self.ones_f, diagE], [edp])
            self.AC(eD[:, b0:b0 + w_], edp[:, 256:256 + w_], ACT.Copy, [edp], [edec], acc=(b0 > 0))
        mask = self.maskF if d == 0 else self.maskB
        st32 = k.sb("st32", [128, DIc]); stb = k.sb("stb", [128, DIc], BF16)
        self.MEMSET("dve", st32[:], 0.0, [st32]); self.MEMSET("dve", stb[:], 0.0, [stb])
        dsk = k.sb("dsk", [128, DIc]); snw = k.sb("snw", [128, DIc])
        if d == 1:
            k.dma("sp", dsk[:], I["dskip_bc"][:, :], r=[I["dskip_bc"]], w=[dsk])
            k.dma("sp", snw[:], I["ssdw_bc"][:, :], r=[I["ssdw_bc"]], w=[snw])
        NB = 2
        xch = [k.sb(f"xch{b}", [128, XT, 128]) for b in range(NB)]
        Bc = [k.sb(f"Bc{b}", [128, 128], BF16) for b in range(NB)]
        Cc = [k.sb(f"Cc{b}", [128, 128], BF16) for b in range(NB)]
        Qc = [k.sb(f"Qc{b}", [PR, 128]) for b in range(NB)]
        C0 = [k.sb(f"C0{b}", [R, 128]) for b in range(NB)]
        qtok = [k.sb(f"qtok{b}", [128, PR]) for b in range(NB)]
        dtds = [k.sb(f"dtds{b}", [128, R]) for b in range(NB)]
        Btok = [k.sb(f"Btok{b}", [128, 128], BF16) for b in range(NB)]
        cbm = [k.sb(f"cbm{b}", [128, 128]) for b in range(NB)]
        BD = [k.sb(f"BD{b}", [R, R, 128]) for b in range(NB)]
        xdt = [k.sb(f"xdt{b}", [128, HWD], BF16) for b in range(NB)]
        xds = [k.sb(f"xds{b}", [128, HWD], BF16) for b in range(NB)]
        Dm = [k.sb(f"Dm{b}", [128, 512]) for b in range(NB)]
        Mt = [k.sb(f"Mt{b}", [128, 4, 128], BF16) for b in range(NB)]
        ydir = [k.sb(f"ydir{b}", [128, DIc]) for b in range(NB)]
        if d == 1:
            yfc = [k.sb(f"yfc{b}", [128, DIc]) for b in range(NB)]
            zc = [k.sb(f"zc{b}", [128, XT, 128]) for b in range(NB)]
            ssq = [k.sb(f"ssq{b}", [128, 4]) for b in range(NB)]
            junk = k.sb("junk", [128, HWD])
            yn = [k.sb(f"yn{b}", [128, DIc], BF16) for b in range(NB)]
            ysb = [k.sb(f"ysb{b}", [128, XT, 128], BF16) for b in range(NB)]
        xv = self.xcT.t.ap().rearrange("(j p) s -> p j s", p=128)
        zv = self.projT.t.ap()[c.oz:c.oz + DIc, :].rearrange("(j p) s -> p j s", p=128)
        n4 = 0
        import os
        cut = int(os.environ.get("SSD_CUT", "9"))
        for ci in range(NC if cut > 0 else 0):
            ch = ci if d == 0 else NC - 1 - ci
            b = ci % NB
            sl = slice(ch * 128, (ch + 1) * 128)
            k.dma("sp", xch[b][:], xv[:, :, sl], r=[self.xcT], w=[xch[b]])
            k.dma("sp", Bc[b][:], self.BCb[0:128, sl], r=[self.BCb], w=[Bc[b]])
            k.dma("sp", Cc[b][:], self.BCb[128:256, sl], r=[self.BCb], w=[Cc[b]])
            k.dma("sp", Qc[b][:], self.Qd[:, sl], r=[self.Qd], w=[Qc[b]])
            k.dma("sp", C0[b][:], self.Qd[2 * R + d * R: 2 * R + (d + 1) * R, sl], r=[self.Qd], w=[C0[b]])
            if d == 1:
                k.dma("sp", yfc[b][:], self.yf[sl, :], r=[self.yf], w=[yfc[b]])
                k.dma("sp", zc[b][:], zv[:, :, sl], r=[self.projT], w=[zc[b]])
            if cut < 2:
                continue
            skip = os.environ.get("SSD_SKIP", "")
            if "qtp" not in skip:
                self.TR(qtp[:, 128:128 + PR], Qc[b][:, :], idf[0:PR, 0:PR], [Qc[b], idf], [qtp])
                self.AC(qtok[b][:], qtp[:, 128:128 + PR], ACT.Copy, [qtp], [qtok[b]])
            qt = qtok[b]
            if "dtds" not in skip:
              self.TT("dve", dtds[b][:], qt[:, d * R:(d + 1) * R], qt[:, 4 * R + d * R:4 * R + (d + 1) * R], ALU.mult, [qt], [dtds[b]])
            skip = os.environ.get("SSD_SKIP", "")
            if "btp" not in skip:
                self.MM(btp[:, 0:128], Bc[b][:], self.ident_b[:], True, True, [Bc[b], self.ident_b], [btp])
                self.AC(Btok[b][:], btp[:, 0:128], ACT.Copy, [btp], [Btok[b]])
            if "cbt" not in skip:
                self.MM(cbT[:, 0:128], Bc[b][:], Cc[b][:], True, True, [Bc[b], Cc[b]], [cbT])
                self.TT("dve", cbm[b][:], cbT[:, 0:128], mask[:], ALU.mult, [cbT, mask], [cbm[b]])
            if "bd" not in skip:
              self.TT("dve", BD[b][:], idf[0:R, 0:R].unsqueeze(2).broadcast_to([R, R, 128]), C0[b][:].unsqueeze(1).broadcast_to([R, R, 128]), ALU.mult, [idf, C0[b]], [BD[b]])
            if d == 1:
                self.MEMSET("dve", ssq[b][:], 0.0, [ssq[b]])
            if cut < 3:
                continue
            for hf in range(H):
                hs = slice(hf * HWD, (hf + 1) * HWD)
                for j in range(TPH):
                    self.TR(xtp[:, j * 128:(j + 1) * 128], xch[b][:, hf * TPH + j, :], idf[:], [xch[b], idf], [xtp], acc=(j > 0))
                r0 = hf * HPH
                x3 = xtp[:, 0:HWD].rearrange("p (r q) -> p r q", q=64)
                dt_b = qt[:, d * R + r0: d * R + r0 + HPH].unsqueeze(2).broadcast_to([128, HPH, 64])
                dd_b = dtds[b][:, r0:r0 + HPH].unsqueeze(2).broadcast_to([128, HPH, 64])
                E_b = qt[:, 6 * R + d * R + r0: 6 * R + d * R + r0 + HPH].unsqueeze(2).broadcast_to([128, HPH, 64])
                self.TT("dve", xdt[b][:].rearrange("p (r q) -> p r q", q=64), x3, dt_b, ALU.mult, [xtp, qt], [xdt[b]])
                self.TT("dve", xds[b][:].rearrange("p (r q) -> p r q", q=64), x3, dd_b, ALU.mult, [xtp, dtds[b]], [xds[b]])
                yd = ydir[b]
                if d == 1:
                    self.TT("dve", yd[:, hs], xtp[:, 0:HWD], dsk[:, hs], ALU.mult, [xtp, dsk], [yd], acc=(hf > 0))
                for g4 in range(G4 if cut > 3 else 0):
                    dp = Dp[n4 % 2]; dm = Dm[n4 % 2]; mt = Mt[n4 % 2]; n4 += 1
                    h0 = r0 + g4 * 4
                    self.MM(dp[:], self.ones_f[0:R, :], BD[b][:, h0:h0 + 4, :].rearrange("p a b -> p (a b)"), True, False, [self.ones_f, BD[b]], [dp], acc=False)
                    self.MM(dp[:], C0[b][:, :], negI[:, h0:h0 + 4, :].rearrange("p a b -> p (a b)"), False, True, [C0[b], negI], [dp], acc=True)
                    self.TS("dve", dm[:], dp[:], 0.0, None, ALU.min, None, [dp], [dm])
                    self.AC(dm[:], dm[:], ACT.Exp, [dm], [dm])
                    self.TT("dve", mt[:], dm[:].rearrange("p (a b) -> p a b", b=128), cbm[b][:].unsqueeze(1).broadcast_to([128, 4, 128]), ALU.mult, [dm, cbm[b]], [mt])
                    for h in range(4):
                        hl = g4 * 4 + h
                        self.MM(ydg[:, hl * 64:(hl + 1) * 64], mt[:, h, :], xdt[b][:, hl * 64:(hl + 1) * 64], True, True, [mt, xdt[b]], [ydg], acc=(hl > 0))
                if cut < 5:
                    continue
                self.MM(yof[:, 0:HWD], Cc[b][:], stb[:, hs], True, True, [Cc[b], stb], [yof])
                if d == 0:
                    self.TT("dve", yd[:, hs].rearrange("p (r q) -> p r q", q=64), yof[:, 0:HWD].rearrange("p (r q) -> p r q", q=64), E_b, ALU.mult, [yof, qt], [yd], acc=(hf > 0))
                else:
                    tmp = Dm[n4 % 2]
                    self.TT("dve", tmp[:, 0:HWD].rearrange("p (r q) -> p r q", q=64), yof[:, 0:HWD].rearrange("p (r q) -> p r q", q=64), E_b, ALU.mult, [yof, qt], [tmp])
                    self.TT("dve", yd[:, hs], yd[:, hs], tmp[:, 0:HWD], ALU.add, [yd, tmp], [yd], acc=True)
                self.TT("dve", yd[:, hs], yd[:, hs], ydg[:, 0:HWD], ALU.add, [yd, ydg], [yd], acc=True)
                self.MM(stp[:, 0:HWD], Btok[b][:], xds[b][:], True, True, [Btok[b], xds[b]], [stp])
                e_b = edec[:, ch, r0:r0 + HPH].unsqueeze(2).broadcast_to([128, HPH, 64])
                self.TT("dve", st32[:, hs].rearrange("p (r q) -> p r q", q=64), st32[:, hs].rearrange("p (r q) -> p r q", q=64), e_b, ALU.mult, [st32, edec], [st32], acc=(hf > 0))
                self.TT("dve", st32[:, hs], st32[:, hs], stp[:, 0:HWD], ALU.add, [st32, stp], [st32], acc=True)
                self.AC(stb[:, hs], st32[:, hs], ACT.Copy, [st32], [stb], acc=(hf > 0))
                if d == 1:
                    self.TT("dve", yd[:, hs], yd[:, hs], yfc[b][:, hs], ALU.add, [yd, yfc[b]], [yd], acc=True)
                    for j in range(TPH):
                        self.TR(xtp[:, j * 128:(j + 1) * 128], zc[b][:, hf * TPH + j, :], idf[:], [zc[b], idf], [xtp], acc=(j > 0))
                    self.TT("dve", yd[:, hs], yd[:, hs], xtp[:, 0:HWD], ALU.mult, [yd, xtp], [yd], acc=True)
                    self.AC(junk[:], yd[:, hs], ACT.Square, [yd], [junk, ssq[b]], accum_out=ssq[b][:, hf:hf + 1])
            if cut < 6:
                continue
            if d == 0:
                k.dma("sp", self.yf[sl, :], ydir[b][:], r=[ydir[b]], w=[self.yf], acc=True)
            else:
                sq_ = ssq[b]
                if H > 1:
                    self.TT("dve", sq_[:, 0:1], sq_[:, 0:1], sq_[:, 1:2], ALU.add, [sq_], [sq_])
                self.AC(sq_[:, 2:3], sq_[:, 0:1], ACT.Sqrt, [sq_, self.eps], [sq_], bias=self.eps[:], scale=1.0 / DIc)
                k.op("dve", (lambda sq_=sq_: nc.vector.reciprocal(out=sq_[:, 3:4], in_=sq_[:, 2:3])), [sq_], [sq_])
                self.STT(yn[b][:], ydir[b][:], sq_[:, 3:4], snw[:], ALU.mult, ALU.mult, [ydir[b], sq_, snw], [yn[b]])
                for j in range(XT):
                    self.MM(ybt[:, 128 + (j % 2) * 128: 256 + (j % 2) * 128], yn[b][:, j * 128:(j + 1) * 128], self.ident_b[:], True, True, [yn[b], self.ident_b], [ybt])
                    self.AC(ysb[b][:, j, :], ybt[:, 128 + (j % 2) * 128: 256 + (j % 2) * 128], ACT.Copy, [ybt], [ysb[b]], acc=(j > 0))
                k.dma("sp", self.yssd_b.t.ap().rearrange("(j p) s -> p j s", p=128)[:, :, sl], ysb[b][:], r=[ysb[b]], w=[self.yssd_b], acc=True)
        k.phase_end()

    def phase4(self):
        c, k, nc, I = self.cfg, self.k, self.nc, self.I
        S = c.S; NT = c.NT; HHc = c.HHc; HW = c.HW
        NC32 = S // 32
        self.ofT = k.dram("ofT", [HW, S]); self.obT = k.dram("obT", [HW, S])
        k.phase_begin()
        idf, idb = self.ident_f, self.ident_b
        lbt = k.sb("lbt", [128, HHc, 2]); k.dma("sp", lbt[:], I["lb_l"][:, :, :], r=[I["lb_l"]], w=[lbt])
        lb = k.sb("lb", [128, HHc]); oml = k.sb("oml", [128, HHc])
        self.TT("dve", lb[:], lbt[:, :, 0], lbt[:, :, 1], ALU.subtract, [lbt], [lb])
        self.AC(lb[:], lb[:], ACT.Sigmoid, [lb], [lb])
        self.TS("dve", oml[:], lb[:], -1.0, 1.0, ALU.mult, ALU.add, [lb], [oml])
        rmask = k.sb("rmask", [128, 4]); k.dma("sp", rmask[:], I["rowmask"][:, :], r=[I["rowmask"]], w=[rmask])
        bm = k.sb("bm", [128, 256]); k.dma("sp", bm[:], I["bmask"][:, :], r=[I["bmask"]], w=[bm])
        PB = min(2048, S)
        rm = k.sb("rm32", [128, PB], BF16)
        self.MEMSET("pool", rm[:], 1.0, [rm])
        self.MEMSET("pool", rm[:].rearrange("p (c t) -> p c t", t=32)[:, :, 0:1], 0.0, [rm])
        t1 = k.sb("t1", [128, PB]); t2 = k.sb("t2", [128, PB]); t3 = k.sb("t3", [128, PB]); t4 = k.sb("t4", [128, PB])
        totc = k.sb("h_totc", [128, PB // 32])
        qt = [k.sb(f"qt{d}", [128, S], BF16) for d in range(2)]
        kt_ = [k.sb(f"kt{d}", [128, S], BF16) for d in range(2)]
        kd = [k.sb(f"kd{d}", [128, S], BF16) for d in range(2)]
        egl = [k.sb(f"egl{d}", [128, NC32]) for d in range(2)]
        vtok = k.sb("vtok", [128, NT, 128], BF16)
        pw = [k.ps(f"pw{d}", [128, 512]) for d in range(2)]
        pk = [k.ps(f"pk{d}", [128, 128]) for d in range(2)]
        pv = k.ps("pv", [128, 128])
        S32 = [k.sb(f"S32_{d}", [128, 128]) for d in range(2)]
        Sb = [[k.sb(f"Sb{d}_{b}", [128, 128], BF16) for b in range(2)] for d in range(2)]
        scm = [[k.sb(f"scm{d}_{b}", [128, 128], BF16) for b in range(2)] for d in range(2)]
        kdtok = [[k.sb(f"kdtok{d}_{b}", [128, 128], BF16) for b in range(2)] for d in range(2)]
        kdm = [[k.sb(f"kdm{d}_{b}", [128, 4, 128], BF16) for b in range(2)] for d in range(2)]
        oin = [[k.sb(f"oin{d}_{b}", [128, 128]) for b in range(2)] for d in range(2)]
        otl = [[k.sb(f"otl{d}_{b}", [128, 128]) for b in range(2)] for d in range(2)]
        vld = [k.sb(f"vld{b}", [128, 128]) for b in range(2)]
        for h in range(HHc):
            hr = h * 128
            for ti in range(NT):
                vb = vld[ti % 2]
                k.dma("sp", vb[:], self.projT[c.oi + hr: c.oi + hr + 128, ti * 128:(ti + 1) * 128], r=[self.projT], w=[vb])
                self.TR(pv[:], vb[:], idf[:], [vb, idf], [pv])
                self.AC(vtok[:, ti, :], pv[:], ACT.Copy, [pv], [vtok], acc=(ti > 0))
            for d in range(2):
                fo = c.off if d == 0 else c.ofb
                for pb in range(S // PB):
                    ps_ = slice(pb * PB, (pb + 1) * PB)
                    k.dma("sp", t1[:], self.projT[fo + hr: fo + hr + 128, ps_], r=[self.projT], w=[t1])
                    k.dma("sp", t4[:], self.projT[c.oq + hr: c.oq + hr + 128, ps_], r=[self.projT], w=[t4])
                    self.AC(t1[:], t1[:], ACT.Sigmoid, [t1], [t1])
                    self.TS("dve", t1[:], t1[:], oml[:, h:h + 1], lb[:, h:h + 1], ALU.mult, ALU.add, [t1, oml, lb], [t1])
                    self.AC(t2[:], t1[:], ACT.Ln, [t1], [t2])
                    self.TS("dve", t1[:], t1[:], -1.0, 1.0, ALU.mult, ALU.add, [t1], [t1])
                    k.op("dve", (lambda: nc.vector.tensor_tensor_scan(out=t3[:], data0=rm[:], data1=t2[:], initial=0.0, op0=ALU.mult, op1=ALU.add)), [rm, t2], [t3])
                    g3 = t3[:].rearrange("p (c t) -> p c t", t=32)
                    if d == 1:
                        k.op("dve", (lambda g3=g3: nc.vector.tensor_copy(out=totc[:].unsqueeze(2), in_=g3[:, :, 31:32])), [t3], [totc])
                        self.TT("dve", t3[:], t2[:], t3[:], ALU.subtract, [t2, t3], [t3])
                        self.TT("dve", g3, g3, totc[:].unsqueeze(2).broadcast_to([128, PB // 32, 32]), ALU.add, [t3, totc], [t3])
                    self.AC(t2[:], t3[:], ACT.Exp, [t3], [t2])
                    e3 = t2[:].rearrange("p (c t) -> p c t", t=32)
                    pos = 31 if d == 0 else 0
                    eg_dst = egl[d][:, pb * (PB // 32):(pb + 1) * (PB // 32)]
                    k.op("dve", (lambda e3=e3, eg_dst=eg_dst, pos=pos: nc.vector.tensor_copy(out=eg_dst.unsqueeze(2), in_=e3[:, :, pos:pos + 1])), [t2], [egl[d]], acc=(pb > 0))
                    self.TT("dve", qt[d][:, ps_], t4[:], t2[:], ALU.mult, [t4, t2], [qt[d]], acc=(pb > 0))
                    self.AC(t2[:], t3[:], ACT.Exp, [t3], [t2], scale=-1.0)
                    self.TT("dve", t1[:], t1[:], t2[:], ALU.mult, [t1, t2], [t1])
                    self.AC(kt_[d][:, ps_], t1[:], ACT.Copy, [t1], [kt_[d]], acc=(pb > 0))
                    self.TT("dve", kd[d][:, ps_].rearrange("p (c t) -> p c t", t=32), t1[:].rearrange("p (c t) -> p c t", t=32),
                            eg_dst.unsqueeze(2).broadcast_to([128, PB // 32, 32]), ALU.mult, [t1, egl[d]], [kd[d]], acc=(pb > 0))
                self.MEMSET("dve", S32[d][:], 0.0, [S32[d]])
                self.MEMSET("dve", Sb[d][0][:], 0.0, [Sb[d][0]])
            nS = [0, 0]
            for i in range(NT):
                for d in range(2):
                    ti = i if d == 0 else NT - 1 - i
                    tsl = slice(ti * 128, (ti + 1) * 128)
                    b = i % 2
                    P = pw[d]
                    scT = Buf(P.t, "scT"); oia = Buf(P.t, "oia"); oie = Buf(P.t, "oie"); stp = Buf(P.t, "stp")
                    self.MM(scT[:, 0:128], kt_[d][:, tsl], qt[d][:, tsl], True, True, [kt_[d], qt[d]], [P])
                    self.TT("dve", scm[d][b][:], P[:, 0:128], bm[:, d * 128:(d + 1) * 128], ALU.mult, [P, bm], [scm[d][b]])
                    self.MM(pk[d][:], kd[d][:, tsl], idb[:], True, True, [kd[d], idb], [pk[d]])
                    self.AC(kdtok[d][b][:], pk[d][:], ACT.Copy, [pk[d]], [kdtok[d][b]])
                    for j in range(4):
                        self.TS("dve", kdm[d][b][:, j, :], kdtok[d][b][:], rmask[:, j:j + 1], None, ALU.mult, None, [kdtok[d][b], rmask], [kdm[d][b]], acc=(j > 0))
                    self.MM(P[:, 128:256], vtok[:, ti, :], scm[d][b][:], True, True, [vtok, scm[d][b]], [P])
                    self.AC(oin[d][b][:], P[:, 128:256], ACT.Copy, [P], [oin[d][b]])
                    for jj in range(4):
                        j = jj if d == 0 else 3 - jj
                        cs_ = slice(ti * 128 + j * 32, ti * 128 + (j + 1) * 32)
                        sb_cur = Sb[d][nS[d] % 2]; sb_nxt = Sb[d][(nS[d] + 1) % 2]; nS[d] += 1
                        self.MM(P[:, 256 + j * 32:256 + (j + 1) * 32], sb_cur[:], qt[d][:, cs_], True, True, [sb_cur, qt[d]], [P])
                        self.MM(P[:, 384:512], kdm[d][b][:, j, :], vtok[:, ti, :], True, True, [kdm[d][b], vtok], [P])
                        cidx = ti * 4 + j
                        self.STT(S32[d][:], S32[d][:], egl[d][:, cidx:cidx + 1], P[:, 384:512], ALU.mult, ALU.add, [S32[d], egl[d], P], [S32[d]])
                        self.AC(sb_nxt[:], S32[d][:], ACT.Copy, [S32[d]], [sb_nxt])
                    self.TT("dve", otl[d][b][:], oin[d][b][:], P[:, 256:384], ALU.add, [oin[d][b], P], [otl[d][b]])
                    dst = self.ofT if d == 0 else self.obT
                    k.dma("sp", dst[hr:hr + 128, tsl], otl[d][b][:], r=[otl[d][b]], w=[dst], acc=True)
        k.phase_end()

    def phase4b(self):
        c, k, nc, I = self.cfg, self.k, self.nc, self.I
        S = c.S; HHc = c.HHc; HW = c.HW; TB = 512
        self.oT = k.dram("oT", [HW, S]); ssqb = k.dram("hssq_b", [1, S]); ssqg = k.dram("hssq_g", [8, S])
        self.yhg_b = k.dram("yhg_b", [HW, S], BF16)
        k.phase_begin()
        a = [k.sb(f"ha{b}", [128, HHc, TB]) for b in range(2)]
        bb = [k.sb(f"hb{b}", [128, HHc, TB]) for b in range(2)]
        sq = k.sb("hsq", [128, HHc, TB])
        ps = [k.ps(f"hps{b}", [128, TB]) for b in range(2)]
        row = [k.sb(f"hrow{b}", [1, TB]) for b in range(2)]
        ov = lambda t: t.t.ap().rearrange("(h p) s -> p h s", p=128)
        for tb in range(S // TB):
            ts_ = slice(tb * TB, (tb + 1) * TB); i = tb % 2
            k.dma("sp", a[i][:], ov(self.ofT)[:, :, ts_], r=[self.ofT], w=[a[i]])
            k.dma("sp", bb[i][:], ov(self.obT)[:, :, ts_], r=[self.obT], w=[bb[i]])
            self.TT("dve", a[i][:], a[i][:], bb[i][:], ALU.add, [a[i], bb[i]], [a[i]])
            self.AC(sq[:].rearrange("p a b -> p (a b)"), a[i][:].rearrange("p a b -> p (a b)"), ACT.Square, [a[i]], [sq])
            for h in range(HHc):
                self.MM(ps[i][:], self.ones_f[:], sq[:, h, :], h == 0, h == HHc - 1, [self.ones_f, sq], [ps[i]])
            self.AC(row[i][:], ps[i][0:1, :], ACT.Copy, [ps[i]], [row[i]])
            k.dma("sp", ssqb[0:1, ts_], row[i][:], r=[row[i]], w=[ssqb], acc=True)
            k.dma("sp", ov(self.oT)[:, :, ts_], a[i][:], r=[a[i]], w=[self.oT], acc=True)
        k.phase_end()
        self.ag(ssqb, ssqg)
        k.phase_begin()
        hgw = k.sb("hgw", [128, HHc]); k.dma("sp", hgw[:], I["hgw_l"][:, :], r=[I["hgw_l"]], w=[hgw])
        a = [k.sb(f"ha{b}", [128, HHc, TB]) for b in range(2)]
        g = [k.sb(f"hg{b}", [128, HHc, TB]) for b in range(2)]
        s8 = [k.sb(f"s8{b}", [8, TB]) for b in range(2)]
        rs = [k.sb(f"hrs{b}", [128, TB]) for b in range(2)]
        yo = [k.sb(f"hyo{b}", [128, HHc, TB], BF16) for b in range(2)]
        ps = [k.ps(f"hps{b}", [128, TB]) for b in range(2)]
        gv = self.projT.t.ap()[c.og:c.og + HW, :].rearrange("(h p) s -> p h s", p=128)
        for tb in range(S // TB):
            ts_ = slice(tb * TB, (tb + 1) * TB); i = tb % 2
            k.dma("sp", a[i][:], ov(self.oT)[:, :, ts_], r=[self.oT], w=[a[i]])
            k.dma("sp", g[i][:], gv[:, :, ts_], r=[self.projT], w=[g[i]])
            k.dma("sp", s8[i][:], ssqg[:, ts_], r=[ssqg], w=[s8[i]])
            self.MM(ps[i][:], self.ones_f[0:8, :], s8[i][:], True, True, [self.ones_f, s8[i]], [ps[i]])
            self.AC(rs[i][:], ps[i][:], ACT.Sqrt, [ps[i], self.eps], [rs[i]], bias=self.eps[:], scale=1.0 / c.D)
            k.op("dve", (lambda r_=rs[i]: nc.vector.reciprocal(out=r_[:], in_=r_[:])), [rs[i]], [rs[i]])
            self.TT("dve", a[i][:], a[i][:], rs[i][:].unsqueeze(1).broadcast_to([128, HHc, TB]), ALU.mult, [a[i], rs[i]], [a[i]])
            self.TT("pool", a[i][:], a[i][:], hgw[:, :].unsqueeze(2).broadcast_to([128, HHc, TB]), ALU.mult, [a[i], hgw], [a[i]])
            self.TT("dve", yo[i][:], a[i][:], g[i][:], ALU.mult, [a[i], g[i]], [yo[i]])
            k.dma("sp", ov(self.yhg_b)[:, :, ts_], yo[i][:], r=[yo[i]], w=[self.yhg_b], acc=True)
        k.phase_end()

    def phase5(self):
        c, k, nc, I = self.cfg, self.k, self.nc, self.I
        S = c.S; DC = c.DC; TB = 256
        self.yssdT = k.dram("yssdT", [c.DI, S], BF16); self.yhgT = k.dram("yhgT", [c.D, S], BF16)
        self.ag(self.yssd_b, self.yssdT); self.ag(self.yhg_b, self.yhgT)
        t1d = k.dram("t1d", [DC, S]); self.merged_b = k.dram("merged_b", [DC, S], BF16)
        self.mergedT = k.dram("mergedT", [c.D, S], BF16)
        k.phase_begin()
        sg = [k.sb(f"sg{b}", [128, TB]) for b in range(3)]; o = [k.sb(f"o5{b}", [128, TB]) for b in range(3)]
        cnt = [0]
        def epi_a(c0, m, tb, ps):
            i = cnt[0] % 3; cnt[0] += 1
            ts_ = slice(tb * TB, (tb + 1) * TB)
            k.dma("sp", sg[i][:m, :], self.projT[c.oga + c0: c.oga + c0 + m, ts_], r=[self.projT], w=[sg[i]])
            self.TT("dve", o[i][:m, :], ps[:m, :], sg[i][:m, :], ALU.mult, [ps, sg[i]], [o[i]])
            k.dma("sp", t1d[c0:c0 + m, ts_], o[i][:m, :], r=[o[i]], w=[t1d], acc=True)
        nk = c.DI // 128
        segs = []
        for s0 in range(0, nk, 32):
            n_ = min(32, nk - s0)
            segs.append((self.yssdT, s0, n_, self.wview(I["w_so"], s0, n_), I["w_so"]))
        self.gemm(segs, DC, S, epi_a, TB=TB, tag="ya")
        k.phase_end()
        k.phase_begin()
        sg = [k.sb(f"sg{b}", [128, TB]) for b in range(3)]; t1 = [k.sb(f"t1{b}", [128, TB]) for b in range(3)]
        o = [k.sb(f"o5{b}", [128, TB]) for b in range(3)]; ob = [k.sb(f"ob5{b}", [128, TB], BF16) for b in range(3)]
        cnt = [0]
        def epi_b(c0, m, tb, ps):
            i = cnt[0] % 3; cnt[0] += 1
            ts_ = slice(tb * TB, (tb + 1) * TB)
            k.dma("sp", sg[i][:m, :], self.projT[c.ogb + c0: c.ogb + c0 + m, ts_], r=[self.projT], w=[sg[i]])
            k.dma("sp", t1[i][:m, :], t1d[c0:c0 + m, ts_], r=[t1d], w=[t1[i]])
            self.TT("dve", o[i][:m, :], ps[:m, :], sg[i][:m, :], ALU.mult, [ps, sg[i]], [o[i]])
            self.TT("dve", ob[i][:m, :], o[i][:m, :], t1[i][:m, :], ALU.add, [o[i], t1[i]], [ob[i]])
            k.dma("sp", self.merged_b[c0:c0 + m, ts_], ob[i][:m, :], r=[ob[i]], w=[self.merged_b], acc=True)
        self.gemm([(self.yhgT, 0, c.KT, self.wview(I["w_ho"], 0, c.KT), I["w_ho"])], DC, S, epi_b, TB=TB, tag="yb")
        k.phase_end()
        self.ag(self.merged_b, self.mergedT)
        self.ymixT = k.dram("ymixT", [DC, S])
        self.resid_gemm(self.mergedT, c.KT, self.wview(I["w_mo"], 0, c.KT), I["w_mo"], self.ymixT, "mx")
        self.x1T_b = k.dram("x1T_b", [DC, S]); self.x1Tf = k.dram("x1Tf", [c.D, S])
        self.resid_pass(self.ymixT, self.I["xT"], self.Gm, self.x1T_b, "mx")
        self.ag(self.x1T_b, self.x1Tf)

    def resid_gemm(self, actT, nkt, wfn, wbuf, ydst, tag):
        c, k, nc = self.cfg, self.k, self.nc
        S = c.S; DC = c.DC; TB = 512
        ssqb = k.dram(f"{tag}_ssqb", [1, S]); ssqg = k.dram(f"{tag}_ssqg", [8, S])
        k.phase_begin()
        o = [k.sb(f"ro{b}", [128, TB]) for b in range(3)]; sq = [k.sb(f"rsq{b}", [128, TB]) for b in range(2)]
        pss = k.ps("rpss", [128, TB]); row = [k.sb(f"rrow{b}", [1, TB]) for b in range(2)]
        cnt = [0]
        def epi(c0, m, tb, ps):
            i = cnt[0] % 3; j = cnt[0] % 2; cnt[0] += 1
            ts_ = slice(tb * TB, (tb + 1) * TB)
            ct = c0 // 128
            self.AC(o[i][:m, :], ps[:m, :], ACT.Copy, [ps], [o[i]])
            self.AC(sq[j][:m, :], ps[:m, :], ACT.Square, [ps], [sq[j]])
            k.dma("sp", ydst[c0:c0 + m, ts_], o[i][:m, :], r=[o[i]], w=[ydst], acc=True)
            self.MM(pss[:], self.ones_f[:m, :], sq[j][:m, :], ct == 0, ct == c.DCT - 1, [self.ones_f, sq[j]], [pss])
            if ct == c.DCT - 1:
                self.AC(row[tb % 2][:], pss[0:1, :], ACT.Copy, [pss], [row[tb % 2]])
                k.dma("sp", ssqb[0:1, ts_], row[tb % 2][:], r=[row[tb % 2]], w=[ssqb], acc=True)
        self.gemm([(actT, 0, nkt, wfn, wbuf)], DC, S, epi, TB=TB, tag=tag)
        k.phase_end()
        self.ag(ssqb, ssqg)
        if not hasattr(self, "ssq8"):
            self.ssq8 = {}
        self.ssq8[tag] = ssqg

    def resid_pass(self, yT, srcT, G, dst, tag):
        c, k, nc = self.cfg, self.k, self.nc
        S = c.S; DCT = c.DCT; TB = 512
        ssqg = self.ssq8[tag]
        k.phase_begin()
        y = [k.sb(f"py{b}", [128, DCT, TB]) for b in range(2)]; x = [k.sb(f"px{b}", [128, DCT, TB]) for b in range(2)]
        s8 = [k.sb(f"ps8{b}", [8, TB]) for b in range(2)]; rs = [k.sb(f"prs{b}", [128, TB]) for b in range(2)]
        ps = [k.ps(f"pps{b}", [128, TB]) for b in range(2)]
        v = lambda t: t.t.ap().rearrange("(j p) s -> p j s", p=128)
        for tb in range(S // TB):
            ts_ = slice(tb * TB, (tb + 1) * TB); i = tb % 2
            k.dma("sp", y[i][:], v(yT)[:, :, ts_], r=[yT], w=[y[i]])
            k.dma("sp", x[i][:], v(srcT)[:, :, ts_], r=[srcT], w=[x[i]])
            k.dma("sp", s8[i][:], ssqg[:, ts_], r=[ssqg], w=[s8[i]])
            self.MM(ps[i][:], self.ones_f[0:8, :], s8[i][:], True, True, [self.ones_f, s8[i]], [ps[i]])
            self.AC(rs[i][:], ps[i][:], ACT.Sqrt, [ps[i], self.eps], [rs[i]], bias=self.eps[:], scale=1.0 / c.D)
            k.op("dve", (lambda r_=rs[i]: nc.vector.reciprocal(out=r_[:], in_=r_[:])), [rs[i]], [rs[i]])
            self.TT("dve", y[i][:], y[i][:], rs[i][:].unsqueeze(1).broadcast_to([128, DCT, TB]), ALU.mult, [y[i], rs[i]], [y[i]])
            self.TT("pool", y[i][:], y[i][:], G[:, :].unsqueeze(2).broadcast_to([128, DCT, TB]), ALU.mult, [y[i], G], [y[i]])
            self.TT("dve", x[i][:], x[i][:], y[i][:], ALU.add, [x[i], y[i]], [x[i]])
            k.dma("sp", v(dst)[:, :, ts_], x[i][:], r=[x[i]], w=[dst], acc=True)
        k.phase_end()

    def phase7(self):
        c, k, nc, I = self.cfg, self.k, self.nc, self.I
        S = c.S; NT = c.NT; cap = c.cap; D = c.D; F = c.F; KT = c.KT; FT = c.FT; DC = c.DC; DCT = c.DCT
        self.posd = k.dram("posd", [16, S]); self.gmd = k.dram("gmd", [16, S])
        ploc = k.sb("ploc", [128, NT, 2])
        k.phase_begin()
        lg = k.sb("lg", [16, S]); k.dma("sp", lg[:], self.logits[:, :], r=[self.logits], w=[lg])
        aff = lg; junk = k.sb("mjunk", [16, S]); cum = k.sb("cum", [16, S]); onesr = k.sb("onesr", [16, S], BF16)
        self.posm = cum; self.gm = aff
        pst = k.ps("mps", [16, 512]); rcp = k.sb("rcp", [16, 512])
        self.AC(aff[:], lg[:], ACT.Exp, [lg], [aff])
        for tb in range(S // 512):
            ts_ = slice(tb * 512, (tb + 1) * 512)
            self.MM(pst[:], self.ones_f[0:16, 0:16], aff[:, ts_], True, True, [self.ones_f, aff], [pst])
            k.op("dve", (lambda ts_=ts_: nc.vector.reciprocal(out=rcp[:], in_=pst[:])), [pst], [rcp])
            self.TT("dve", aff[:, ts_], aff[:, ts_], rcp[:], ALU.mult, [aff, rcp], [aff], acc=False)
        sc = k.sb("bis", [16, 8])
        self.MEMSET("dve", sc[:, 0:1], 0.0, [sc]); self.MEMSET("dve", sc[:, 1:2], 2.0, [sc], acc=True)
        lo, hi, mid, cn, se, dd = (sc[:, i:i + 1] for i in range(6))
        for it in range(40):
            self.TS("dve", mid, lo, hi, 0.5, ALU.add, ALU.mult, [sc], [sc])
            self.TS("dve", junk[:], aff[:], mid, 0.0, ALU.is_ge, ALU.add, [aff, sc], [junk, sc], accum_out=cn)
            self.TS("dve", se, cn, float(cap), None, ALU.is_ge, None, [sc], [sc])
            self.TT("dve", dd, mid, lo, ALU.subtract, [sc], [sc])
            self.STT(lo, dd, se, lo, ALU.mult, ALU.add, [sc], [sc])
            self.TT("dve", dd, hi, mid, ALU.subtract, [sc], [sc])
            self.STT(hi, dd, se, mid, ALU.mult, ALU.add, [sc], [sc])
        mask = junk
        self.TS("dve", mask[:], aff[:], lo, None, ALU.is_ge, None, [aff, sc], [mask])
        self.MEMSET("pool", onesr[:], 1.0, [onesr])
        k.op("dve", lambda: nc.vector.tensor_tensor_scan(out=cum[:], data0=onesr[:], data1=mask[:], initial=0.0, op0=ALU.mult, op1=ALU.add), [onesr, mask], [cum])
        self.TT("dve", self.posm[:], cum[:], mask[:], ALU.mult, [cum, mask], [self.posm])
        self.TS("dve", self.posm[:], self.posm[:], -1.0, None, ALU.add, None, [self.posm], [self.posm])
        self.TT("dve", self.gm[:], aff[:], mask[:], ALU.mult, [aff, mask], [self.gm])
        es = k.sb("esel", [16, 2]); k.dma("sp", es[:], I["esel"][:, :], r=[I["esel"]], w=[es])
        pp = k.ps("mpp", [128, NT * 2])
        for ti in range(NT):
            self.MM(pp[:, ti * 2:(ti + 1) * 2], self.posm[:, ti * 128:(ti + 1) * 128], es[:], True, True, [self.posm, es], [pp], acc=(ti > 0))
        self.AC(ploc[:].rearrange("p a b -> p (a b)"), pp[:], ACT.Copy, [pp], [ploc])
        k.dma("sp", self.posd[:, :], self.posm[:], r=[self.posm], w=[self.posd])
        k.dma("sp", self.gmd[:, :], self.gm[:], r=[self.gm], w=[self.gmd])
        k.phase_end()
        self.xgT = k.dram("xgT", [D, 2 * cap], BF16)
        CB = min(512, cap)
        k.phase_begin()
        io_i = k.sb("io_i", [128, CB], I32); io_f = k.sb("io_f", [128, CB])
        k.op("pool", lambda: nc.gpsimd.iota(io_i[:], pattern=[[1, CB]], base=0, channel_multiplier=0), (), [io_i])
        k.op("dve", lambda: nc.vector.tensor_copy(out=io_f[:], in_=io_i[:]), [io_i], [io_f])
        sel = k.sb("sel", [128, NT, CB], BF16)
        hl = [k.sb(f"hl{b}", [128, 512], BF16) for b in range(3)]
        gps = [k.ps(f"gps{b}", [128, CB]) for b in range(4)]
        go = [k.sb(f"go{b}", [128, CB], BF16) for b in range(2)]
        DG = min(512, D); ND = DG // 128
        nl = 0; ng = 0
        for j in range(2):
            for cb in range(cap // CB):
                for ti in range(NT):
                    self.TS("dve", sel[:, ti, :], io_f[:], float(cb * CB), ploc[:, ti, j:j + 1], ALU.add, ALU.is_equal, [io_f, ploc], [sel], acc=(ti > 0))
                for dg in range(D // DG):
                    for ti in range(NT):
                        h_ = hl[nl % 3]; nl += 1
                        k.dma("sp", h_[:, :DG], self.h2tok[ti * 128:(ti + 1) * 128, dg * DG:(dg + 1) * DG], r=[self.h2tok], w=[h_])
                        for q in range(ND):
                            self.MM(gps[q][:], h_[:, q * 128:(q + 1) * 128], sel[:, ti, :], ti == 0, ti == NT - 1, [h_, sel], [gps[q]])
                    for q in range(ND):
                        g_ = go[ng % 2]; ng += 1
                        self.AC(g_[:], gps[q][:], ACT.Copy, [gps[q]], [g_])
                        r0 = dg * DG + q * 128
                        k.dma("sp", self.xgT[r0:r0 + 128, j * cap + cb * CB: j * cap + (cb + 1) * CB], g_[:], r=[g_], w=[self.xgT], acc=True)
        k.phase_end()
        TBm = min(512, cap)
        gtmp = k.dram("gtmp", [2 * F, cap]); self.hid_b = k.dram("hid_b", [2 * F, cap], BF16); self.hidT = k.dram("hidT", [16 * F, cap], BF16)
        for j in range(2):
            k.phase_begin()
            o = [k.sb(f"eo{b}", [128, TBm]) for b in range(3)]
            cnt = [0]
            def epi_g(c0, m, tb, ps, j=j, o=o, cnt=cnt):
                i = cnt[0] % 3; cnt[0] += 1
                self.AC(o[i][:m, :], ps[:m, :], ACT.Silu, [ps], [o[i]])
                k.dma("sp", gtmp[j * F + c0: j * F + c0 + m, tb * TBm:(tb + 1) * TBm], o[i][:m, :], r=[o[i]], w=[gtmp], acc=True)
            self.gemm([(self.xgT, 0, KT, self.wview(I["w_g"], 0, KT, r0=j * D), I["w_g"])], F, cap, epi_g, TB=TBm, t0=j * cap, tag=f"eg{j}")
            k.phase_end()
            k.phase_begin()
            gl = [k.sb(f"gl{b}", [128, TBm]) for b in range(3)]; ob = [k.sb(f"eob{b}", [128, TBm], BF16) for b in range(3)]
            cnt = [0]
            def epi_u(c0, m, tb, ps, j=j, gl=gl, ob=ob, cnt=cnt):
                i = cnt[0] % 3; cnt[0] += 1
                k.dma("sp", gl[i][:m, :], gtmp[j * F + c0: j * F + c0 + m, tb * TBm:(tb + 1) * TBm], r=[gtmp], w=[gl[i]])
                self.TT("dve", ob[i][:m, :], ps[:m, :], gl[i][:m, :], ALU.mult, [ps, gl[i]], [ob[i]])
                k.dma("sp", self.hid_b[j * F + c0: j * F + c0 + m, tb * TBm:(tb + 1) * TBm], ob[i][:m, :], r=[ob[i]], w=[self.hid_b], acc=True)
            self.gemm([(self.xgT, 0, KT, self.wview(I["w_u"], 0, KT, r0=j * D), I["w_u"])], F, cap, epi_u, TB=TBm, t0=j * cap, tag=f"eu{j}")
            k.phase_end()
        self.ag(self.hid_b, self.hidT)
        self.ydd = k.dram("ydd", [16 * cap, DC], BF16)
        k.phase_begin()
        hT = [k.sb(f"dh{b}", [128, FT, cap], BF16) for b in range(2)]
        wd = [k.sb(f"dw{b}", [128, FT, DC], BF16) for b in range(2)]
        dps = [k.ps(f"dps{b}", [128, DC]) for b in range(2)]
        yo = [k.sb(f"dyo{b}", [128, DC], BF16) for b in range(3)]
        n = 0
        for e in range(16):
            h_ = hT[e % 2]; w_ = wd[e % 2]
            k.dma("sp", h_[:], self.hidT.t.ap()[e * F:(e + 1) * F, :].rearrange("(ft p) c -> p ft c", p=128), r=[self.hidT], w=[h_])
            k.dma("pool", w_[:], I["w_d"].t.ap()[e * F:(e + 1) * F, :].rearrange("(ft p) c -> p ft c", p=128), r=[I["w_d"]], w=[w_])
            for ct in range(cap // 128):
                ps = dps[n % 2]; y_ = yo[n % 3]; n += 1
                for ft in range(FT):
                    self.MM(ps[:], h_[:, ft, ct * 128:(ct + 1) * 128], w_[:, ft, :], ft == 0, ft == FT - 1, [h_, w_], [ps])
                self.AC(y_[:], ps[:], ACT.Copy, [ps], [y_])
                k.dma("sp", self.ydd[e * cap + ct * 128: e * cap + (ct + 1) * 128, :], y_[:], r=[y_], w=[self.ydd], acc=True)
        k.phase_end()
        self.y2T = k.dram("y2T", [DC, S])
        ssqb = k.dram("mo_ssqb", [1, S]); ssqg = k.dram("mo_ssqg", [8, S])
        k.phase_begin()
        osl = k.sb("osl", [16, 16 * 128]); k.dma("sp", osl[:], I["onesel"][:, :], r=[I["onesel"]], w=[osl])
        cidx = k.sb("cidx", [128, max(1, cap // 128)]); k.dma("sp", cidx[:], I["cidx"][:, :], r=[I["cidx"]], w=[cidx])
        y2 = [k.ps(f"y2ps{b}", [128, 512]) for b in range(DCT)]
        bp = k.ps("bp", [128, 512]); bg = k.ps("bg", [128, 512]); pss = k.ps("cpss", [128, 512])
        bps = [k.sb(f"bps{b}", [128, 512]) for b in range(2)]; bgs = [k.sb(f"bgs{b}", [128, 512]) for b in range(2)]
        Pm = [k.sb(f"Pm{b}", [128, 512], BF16) for b in range(3)]
        yl = [k.sb(f"yl{b}", [128, DC], BF16) for b in range(3)]
        o = [k.sb(f"co{b}", [128, 512]) for b in range(2)]; sq = [k.sb(f"csq{b}", [128, 512]) for b in range(2)]
        row = [k.sb(f"crow{b}", [1, 512]) for b in range(2)]
        pzl = [k.sb(f"pzl{b}", [16, 512]) for b in range(2)]; gzl = [k.sb(f"gzl{b}", [16, 512]) for b in range(2)]
        n = 0
        NCT = cap // 128
        for tb in range(S // 512):
            ts_ = slice(tb * 512, (tb + 1) * 512)
            pz = pzl[tb % 2]; gz = gzl[tb % 2]
            k.dma("sp", pz[:], self.posd[:, ts_], r=[self.posd], w=[pz])
            k.dma("sp", gz[:], self.gmd[:, ts_], r=[self.gmd], w=[gz])
            for e in range(16):
                i2 = e % 2
                self.MM(bp[:], osl[:, e * 128:(e + 1) * 128], pz[:], True, True, [osl, pz], [bp])
                self.MM(bg[:], osl[:, e * 128:(e + 1) * 128], gz[:], True, True, [osl, gz], [bg])
                self.AC(bps[i2][:], bp[:], ACT.Copy, [bp], [bps[i2]])
                self.AC(bgs[i2][:], bg[:], ACT.Copy, [bg], [bgs[i2]])
                for ct in range(NCT):
                    pm = Pm[n % 3]; y_ = yl[n % 3]; n += 1
                    self.STT(pm[:], bps[i2][:], cidx[:, ct:ct + 1], bgs[i2][:], ALU.is_equal, ALU.mult, [bps[i2], cidx, bgs[i2]], [pm])
                    k.dma("sp", y_[:], self.ydd[e * cap + ct * 128: e * cap + (ct + 1) * 128, :], r=[self.ydd], w=[y_])
                    first = (e == 0 and ct == 0); last = (e == 15 and ct == NCT - 1)
                    for q in range(DCT):
                        self.MM(y2[q][:], y_[:, q * 128:(q + 1) * 128], pm[:], first, last, [y_, pm], [y2[q]])
            for q in range(DCT):
                i = q % 2
                self.AC(o[i][:], y2[q][:], ACT.Copy, [y2[q]], [o[i]])
                self.AC(sq[i][:], y2[q][:], ACT.Square, [y2[q]], [sq[i]])
                k.dma("sp", self.y2T[q * 128:(q + 1) * 128, ts_], o[i][:], r=[o[i]], w=[self.y2T], acc=True)
                self.MM(pss[:], self.ones_f[:], sq[i][:], q == 0, q == DCT - 1, [self.ones_f, sq[i]], [pss])
            self.AC(row[tb % 2][:], pss[0:1, :], ACT.Copy, [pss], [row[tb % 2]])
            k.dma("sp", ssqb[0:1, ts_], row[tb % 2][:], r=[row[tb % 2]], w=[ssqb], acc=True)
        k.phase_end()
        self.ag(ssqb, ssqg)
        self.ssq8["mo"] = ssqg

    def build(self):
        import os
        c, k = self.cfg, self.k
        stop = int(os.environ.get("STOP_AFTER", "99"))
        self.declare_inputs()
        self.consts()
        steps = []
        def hT_():
            self.hT = k.dram("hT", [c.D, c.S], BF16)
            self.norm_phase(self.xTf, self.A_m, self.B_m, self.hT)
            self.dbg("hT", self.hT, [c.D, c.S], BF16)
        def p2_():
            self.phase2(); self.dbg("projT", self.projT, [c.NCOL, c.S])
        def p3_():
            self.phase3_prep()
            self.dbg("Qd", self.Qd, [8 * c.R, c.S]); self.dbg("xcT", self.xcT, [c.DIc, c.S])
        def s0_():
            self.ssd_sweep(0); self.dbg("yf", self.yf, [c.S, c.DIc])
        def s1_():
            self.ssd_sweep(1); self.dbg("yssd_b", self.yssd_b, [c.DIc, c.S], BF16)
        def p4_():
            self.phase4(); self.dbg("ofT", self.ofT, [c.HW, c.S]); self.dbg("obT", self.obT, [c.HW, c.S])
        def p4b_():
            self.phase4b(); self.dbg("yhg_b", self.yhg_b, [c.HW, c.S], BF16)
        def p5_():
            self.phase5(); self.dbg("x1T_b", self.x1T_b, [c.DC, c.S])
        def n2_():
            self.logits = k.dram("logits", [16, c.S])
            self.h2T = k.dram("h2T", [c.D, c.S], BF16); self.h2tok = k.dram("h2tok", [c.S, c.D], BF16)
            self.norm_phase(self.x1Tf, self.A_f, self.B_f, self.h2T, router=True)
            self.dbg("h2T", self.h2T, [c.D, c.S], BF16)
        def p7_():
            self.phase7(); self.dbg("y2T", self.y2T, [c.DC, c.S])
            self.resid_pass(self.y2T, self.x1T_b, self.Gf, self.outT, "mo")
        steps = [self.phase0, hT_, p2_, p3_, s0_, s1_, p4_, p4b_, p5_, n2_, p7_]
        for i, st in enumerate(steps):
            if i > stop:
                break
            st()
        k.finish()
        return self.nc


def _lay(v, nt):
    return np.ascontiguousarray(np.asarray(v).reshape(nt, 128).T)


def shard_inputs(cfg, inp):
    c = cfg
    D, S, R = c.D, c.S, c.R
    x = np.asarray(inp["x"])[0]
    xT = np.ascontiguousarray(x.T)
    w_ada = np.asarray(inp["w_ada"])[0]; b_ada = np.asarray(inp["b_ada"])[0]
    w_in = np.asarray(inp["w_in"])[0]
    conv_w = np.asarray(inp["conv_w"])[0]; conv_b = np.asarray(inp["conv_b"])[0]
    sizes = [c.DI, c.DI + 2 * 8 * c.N, 8 * R, 8 * R, D, D, D, D, D, D, D]
    offs = np.cumsum([0] + sizes)
    o_z, o_xbc, o_dtf, o_dtb, o_q, o_ff, o_fb, o_i, o_g, o_ga, o_gb = offs[:11]
    maps = []
    eye16 = np.eye(16, dtype=np.float32)
    onesel = np.ascontiguousarray(np.repeat(eye16[:, :, None], 128, axis=2).reshape(16, 16 * 128))
    nct = max(1, c.cap // 128)
    cidx = (np.arange(128, dtype=np.float32)[:, None] + 128.0 * np.arange(nct, dtype=np.float32)[None, :]).astype(np.float32)
    rowmask = np.zeros((128, 4), np.float32)
    for j in range(4):
        rowmask[j * 32:(j + 1) * 32, j] = 1.0
    s_ = np.arange(128)[:, None]; t_ = np.arange(128)[None, :]
    same = (s_ // 32) == (t_ // 32)
    bmask = np.concatenate([(same & (s_ <= t_)), (same & (s_ >= t_))], axis=1).astype(np.float32)
    for g in range(8):
        m = {}
        cs_ = slice(g * c.DC, (g + 1) * c.DC)
        m["xT"] = np.ascontiguousarray(xT[cs_, :])
        m["c_l"] = _lay(np.asarray(inp["c"])[0], c.KT)
        n6 = 6 * D // 8
        cols = np.concatenate([np.arange(g * n6, (g + 1) * n6), 2 * D + np.arange(g * c.DC, (g + 1) * c.DC), 5 * D + np.arange(g * c.DC, (g + 1) * c.DC)])
        m["w_ada"] = np.ascontiguousarray(w_ada[:, cols])
        m["b_ada_l"] = _lay(b_ada[cols], c.KT)
        m["npm_l"] = _lay(np.asarray(inp["norm_pre_mix"])[0], c.KT)
        m["npf_l"] = _lay(np.asarray(inp["norm_pre_ffn"])[0], c.KT)
        m["npom_l"] = _lay(np.asarray(inp["norm_post_mix"])[0][cs_], c.DCT)
        m["npof_l"] = _lay(np.asarray(inp["norm_post_ffn"])[0][cs_], c.DCT)
        xch = o_xbc + np.arange(g * c.DIc, (g + 1) * c.DIc)
        bch = o_xbc + c.DI + np.arange(g * c.N, (g + 1) * c.N)
        cch = o_xbc + c.DI + 8 * c.N + np.arange(g * c.N, (g + 1) * c.N)
        hs = np.arange(g * c.HW, (g + 1) * c.HW)
        wcols = np.concatenate([o_z + np.arange(g * c.DIc, (g + 1) * c.DIc), xch, bch, cch,
                                o_q + hs, o_ff + hs, o_fb + hs, o_i + hs, o_g + hs,
                                o_ga + np.arange(g * c.DC, (g + 1) * c.DC), o_gb + np.arange(g * c.DC, (g + 1) * c.DC),
                                o_dtf + np.arange(g * R, (g + 1) * R), o_dtb + np.arange(g * R, (g + 1) * R)])
        assert len(wcols) == c.NCOL
        m["w_in"] = np.ascontiguousarray(w_in[:, wcols])
        cch_all = np.concatenate([xch, bch, cch]) - o_xbc
        cwg = conv_w[:, cch_all]
        m["convw_l"] = np.ascontiguousarray(cwg.T.reshape(c.CT3, 128, 5).transpose(1, 0, 2))
        m["convb_l"] = _lay(conv_b[cch_all], c.CT3)
        hr = slice(g * R, (g + 1) * R)
        dtb = [np.asarray(inp["dt_bias_fwd"])[0][hr], np.asarray(inp["dt_bias_bwd"])[0][hr]]
        alg = [np.asarray(inp["a_log_fwd"])[0][hr], np.asarray(inp["a_log_bwd"])[0][hr]]
        par8 = np.zeros((8 * R, 8), np.float32)
        for blk in range(4):
            for d in range(2):
                rows = slice(blk * 2 * R + d * R, blk * 2 * R + (d + 1) * R)
                par8[rows, 0] = dtb[d]; par8[rows, 1] = alg[d]
                par8[rows, 2 + blk] = 1.0
                par8[rows, 6 + d] = 1.0
        m["par8"] = par8
        m["par2"] = np.zeros((2 * R, 2 + 2 * R), np.float32); m["par2b"] = np.zeros((2 * R, 2), np.float32)
        m["dskip_bc"] = np.ascontiguousarray(np.broadcast_to(np.repeat(np.asarray(inp["d_skip"])[0][hr], 64)[None, :], (128, c.DIc))).astype(np.float32)
        m["ssdw_bc"] = np.ascontiguousarray(np.broadcast_to(np.asarray(inp["ssd_norm_w"])[0][g * c.DIc:(g + 1) * c.DIc][None, :], (128, c.DIc))).astype(np.float32)
        lbt = np.asarray(inp["hg_lower_bound"])[:, g * c.HW:(g + 1) * c.HW]
        m["lb_l"] = np.ascontiguousarray(lbt.reshape(2, c.HHc, 128).transpose(2, 1, 0))
        m["hgw_l"] = _lay(np.asarray(inp["hg_norm_w"])[0][g * c.HW:(g + 1) * c.HW], c.HHc)
        m["w_so"] = np.ascontiguousarray(np.asarray(inp["w_ssd_out"])[0][:, cs_])
        m["w_ho"] = np.ascontiguousarray(np.asarray(inp["w_hg_out"])[0][:, cs_])
        m["w_mo"] = np.ascontiguousarray(np.asarray(inp["w_mix_out"])[0][:, cs_])
        wr = np.asarray(inp["w_router"])[0]
        m["w_r_l"] = np.ascontiguousarray(wr.reshape(c.KT, 128, 16).transpose(1, 0, 2))
        m["w_g"] = np.ascontiguousarray(np.asarray(inp["w_gate"])[0][2 * g:2 * g + 2].reshape(2 * D, c.F))
        m["w_u"] = np.ascontiguousarray(np.asarray(inp["w_up"])[0][2 * g:2 * g + 2].reshape(2 * D, c.F))
        m["w_d"] = np.ascontiguousarray(np.asarray(inp["w_down"])[0][:, :, cs_].reshape(16 * c.F, c.DC))
        es = np.zeros((16, 2), np.float32); es[2 * g, 0] = 1.0; es[2 * g + 1, 1] = 1.0
        m["esel"] = es; m["onesel"] = onesel; m["cidx"] = cidx; m["rowmask"] = rowmask; m["bmask"] = bmask
        maps.append({k_: np.ascontiguousarray(v, dtype=np.float32) for k_, v in m.items()})
    return maps


_CACHE = {}


def run(cfg, inputs, debug=()):
    from concourse.bass_utils import run_bass_kernel_spmd
    key = (cfg.D, cfg.S, tuple(debug))
    if key not in _CACHE:
        p = Prog(cfg, debug)
        _CACHE[key] = p.build()
    nc = _CACHE[key]
    maps = shard_inputs(cfg, inputs)
    res = run_bass_kernel_spmd(nc, maps, core_ids=list(range(8)))
    out = np.empty((1, cfg.S, cfg.D), np.float32)
    for g in range(8):
        out[0][:, g * cfg.DC:(g + 1) * cfg.DC] = res.results[g]["outT"].T
    return out, res


def kernel(**inputs):
    out, _ = run(Cfg(4096, 8192), inputs)
    return out
```

```python
import numpy as np
from contextlib import ExitStack
import concourse.bass as bass
import concourse.mybir as mybir

F32 = mybir.dt.float32
BF16 = mybir.dt.bfloat16
I32 = mybir.dt.int32
ACT = mybir.ActivationFunctionType
ALU = mybir.AluOpType
AX = mybir.AxisListType

NQ = 16
STORE_Q = "act"


class Buf:
    __slots__ = ("t", "w_dma", "w_cmp", "r_dma", "r_cmp", "name")

    def __init__(self, t, name):
        self.t = t
        self.name = name
        self.w_dma = {}
        self.w_cmp = {}
        self.r_dma = {}
        self.r_cmp = {}

    def __getitem__(self, k):
        return self.t[k]


class Op:
    __slots__ = ("eng", "fn", "deps_c", "deps_d", "kind", "needed", "sem", "val", "dkey", "dval")


class K:
    def __init__(self, nc):
        self.nc = nc
        self.ops = []
        self.es = ExitStack()
        self.dma_cnt = {"sp": 0, "pool": 0, "act": 0}
        self.ncoll = 0
        self.uid = 0
        self.pes = None
        self.emitted = 0
        self.bar_idx = -1
        self.inited = False
        self.nwait = 0

    def sb(self, name, shape, dtype=F32):
        self.uid += 1
        es = self.pes if self.pes is not None else self.es
        t = es.enter_context(self.nc.sbuf_tensor(f"{name}_{self.uid}", list(shape), dtype))
        return Buf(t, name)

    def ps(self, name, shape, dtype=F32):
        self.uid += 1
        es = self.pes if self.pes is not None else self.es
        t = es.enter_context(self.nc.psum_tensor(f"{name}_{self.uid}", list(shape), dtype))
        return Buf(t, name)

    def dram(self, name, shape, dtype=F32):
        t = self.nc.dram_tensor(name, list(shape), dtype)
        return Buf(t, name)

    def ext(self, name, shape, dtype, kind):
        t = self.nc.dram_tensor(name, list(shape), dtype, kind=kind)
        return Buf(t, name)

    def _record(self, eng, fn, r, w, kind, acc):
        op = Op()
        op.eng = eng
        op.fn = fn
        op.kind = kind
        op.needed = False
        op.sem = None
        op.val = None
        dc = {}
        dd = {}

        def add_c(m):
            for e, i in m.items():
                if dc.get(e, -1) < i:
                    dc[e] = i

        def add_d(m):
            for k, v in m.items():
                if dd.get(k, 0) < v:
                    dd[k] = v

        for b in r:
            add_c(b.w_cmp)
            add_d(b.w_dma)
        for b in w:
            if not acc:
                add_c(b.w_cmp)
                add_d(b.w_dma)
                add_c(b.r_cmp)
                add_d(b.r_dma)
            else:
                add_c({e: i for e, i in b.w_cmp.items() if e != eng})
                add_d({kk: v for kk, v in b.w_dma.items() if kk[0] != eng})
                add_c({e: i for e, i in b.r_cmp.items() if e != eng})
                add_d({kk: v for kk, v in b.r_dma.items() if kk[0] != eng})
        idx = len(self.ops)
        op.dkey = None
        if kind == "dma":
            i = self.dma_cnt[eng]
            self.dma_cnt[eng] = i + 1
            op.dkey = (eng, i % NQ)
            op.dval = 16 * (i // NQ + 1)
        elif kind == "coll":
            op.dkey = ("coll", self.ncoll % NQ)
            op.dval = self.ncoll // NQ + 1
            self.ncoll += 1
        for b in r:
            if op.dkey is not None:
                if b.r_dma.get(op.dkey, 0) < op.dval:
                    b.r_dma[op.dkey] = op.dval
            else:
                b.r_cmp[eng] = idx
        for b in w:
            if not acc:
                b.w_cmp = {}
                b.w_dma = {}
                b.r_cmp = {}
                b.r_dma = {}
            if op.dkey is not None:
                if b.w_dma.get(op.dkey, 0) < op.dval:
                    b.w_dma[op.dkey] = op.dval
            else:
                b.w_cmp[eng] = idx
        op.deps_c = dc
        op.deps_d = dd
        self.ops.append(op)
        return op

    def op(self, eng, fn, r=(), w=(), acc=False):
        return self._record(eng, fn, r, w, "cmp", acc)

    def dma(self, q, out, in_, r=(), w=(), acc=False, **kw):
        nc = self.nc
        if q == "sp" and type(out.tensor).__name__.startswith("DRam") and type(in_.tensor).__name__.startswith("SB"):
            q = STORE_Q
        e = {"sp": nc.sync, "pool": nc.gpsimd, "act": nc.scalar}[q]
        return self._record(q, lambda: e.dma_start(out=out, in_=in_, **kw), r, w, "dma", acc)

    def allgather(self, src, dst):
        nc = self.nc
        import os
        if os.environ.get("NO_COLL"):
            rows = src.t.shape[0]
            for r0 in range(0, rows, 128):
                r1 = min(rows, r0 + 128)
                self.dma("sp", dst[r0:r1, :], src[r0:r1, :], r=[src], w=[dst], acc=True)
            return
        rows = src.t.shape[0]; cols = src.t.shape[1]
        isz = 2 if src.t.dtype == BF16 else 4
        cr = max(1, min(rows, (512 * 1024) // (cols * isz)))
        while rows % cr:
            cr -= 1
        self.uid += 1
        u = self.uid
        NR = 3
        ring = [(self.dram(f"agi{u}_{b}", [cr, cols], src.t.dtype), self.dram(f"ag2{u}_{b}", [2 * cr, cols], src.t.dtype),
                 self.dram(f"ag8{u}_{b}", [8 * cr, cols], src.t.dtype)) for b in range(NR)]
        g4 = [[0, 1, 2, 3], [4, 5, 6, 7]]
        g2 = [[0, 4], [1, 5], [2, 6], [3, 7]]
        def coll(groups, a, b_):
            return self._record(
                "pool",
                lambda: nc.gpsimd.collective_compute(
                    "AllGather", ALU.bypass, replica_groups=groups,
                    ins=[a.t.ap().opt()], outs=[b_.t.ap().opt()]),
                [a], [b_], "coll", False)
        starts = list(range(0, rows, cr))
        n = len(starts)
        for i in range(n + 1):
            if i < n:
                cin, c2, c8 = ring[i % NR]
                self.dma("sp", cin[:, :], src[starts[i]:starts[i] + cr, :], r=[src], w=[cin])
                coll(g2, cin, c2)
            if i >= 1:
                cin, c2, c8 = ring[(i - 1) % NR]
                r0 = starts[i - 1]
                coll(g4, c2, c8)
                for j in range(8):
                    core = (j // 2) + 4 * (j % 2)
                    self.dma("sp", dst[core * rows + r0: core * rows + r0 + cr, :], c8[j * cr:(j + 1) * cr, :], r=[c8], w=[dst], acc=True)

    def phase_begin(self):
        assert self.pes is None
        self.pes = ExitStack()

    def phase_end(self):
        self.barrier()
        self.flush()
        self.pes.close()
        self.pes = None

    def barrier(self):
        marks = {}
        for e in ("act", "dve", "pool"):
            op = Op()
            op.eng = e; op.fn = None; op.kind = "mark"; op.needed = True; op.sem = None; op.val = None
            op.deps_c = {}; op.deps_d = {}; op.dkey = None
            marks[e] = len(self.ops)
            self.ops.append(op)
        dd = {}
        for q, n in self.dma_cnt.items():
            for s_ in range(NQ):
                cnt = (n - s_ + NQ - 1) // NQ if n > s_ else 0
                if cnt > 0:
                    dd[(q, s_)] = 16 * cnt
        for s_ in range(NQ):
            cnt = (self.ncoll - s_ + NQ - 1) // NQ if self.ncoll > s_ else 0
            if cnt > 0:
                dd[("coll", s_)] = cnt
        for e in ("pe", "act", "dve", "pool", "sp"):
            op = Op()
            op.eng = e; op.fn = None; op.kind = "barwait"; op.needed = False; op.sem = None; op.val = None
            op.deps_c = dict(marks); op.deps_d = dict(dd); op.dkey = None
            self.ops.append(op)
        self.bar_idx = len(self.ops)

    def _init_emit(self):
        nc = self.nc
        self.engs = {"pe": nc.tensor, "act": nc.scalar, "dve": nc.vector, "pool": nc.gpsimd, "sp": nc.sync}
        self.csem = {e: self.es.enter_context(nc.semaphore(f"c_{e}")) for e in ("pe", "act", "dve", "pool")}
        self.ccnt = {e: 0 for e in self.csem}
        self.dsem = {}
        for q in ("sp", "pool", "act"):
            for s_ in range(NQ):
                self.dsem[(q, s_)] = self.es.enter_context(nc.semaphore(f"d_{q}{s_}"))
        for s_ in range(NQ):
            self.dsem[("coll", s_)] = self.es.enter_context(nc.semaphore(f"cc{s_}"))
        self.seen = {e: {} for e in self.engs}
        self.marktile = {e: self.es.enter_context(nc.sbuf_tensor(f"mark_{e}", [1, 8], F32)) for e in ("act", "dve", "pool")}
        self.markps = self.es.enter_context(nc.sbuf_tensor("mark_pe_in", [1, 8], BF16))
        nc.vector.memset(self.marktile["act"][:], 0.0)
        self.inited = True

    def flush(self):
        nc = self.nc
        if not self.inited:
            self._init_emit()
        ops = self.ops
        start = self.emitted
        for o in ops[start:]:
            for e, i in o.deps_c.items():
                ops[i].needed = True
        engs, csem, dsem, ccnt = self.engs, self.csem, self.dsem, self.ccnt
        for idx in range(start, len(ops)):
            o = ops[idx]
            e = o.eng
            h = engs[e]
            sn = self.seen[e]
            for de, di in o.deps_c.items():
                d = ops[di]
                if de == "pe" and e == "pe":
                    continue
                if d.val is None:
                    continue
                key = ("c", de)
                if sn.get(key, 0) >= d.val:
                    continue
                h.wait_ge(csem[de], d.val)
                self.nwait += 1
                sn[key] = d.val
            for dk, dv in o.deps_d.items():
                if sn.get(dk, 0) >= dv:
                    continue
                if dk not in dsem:
                    dsem[dk] = self.es.enter_context(nc.semaphore(f"cc{dk[1]}"))
                h.wait_ge(dsem[dk], dv)
                self.nwait += 1
                sn[dk] = dv
            if o.kind == "dma":
                prev = o.dval - 16
                if prev > 0 and sn.get(o.dkey, 0) < prev:
                    h.wait_ge(dsem[o.dkey], prev)
                    self.nwait += 1
                    sn[o.dkey] = prev
                o.fn().then_inc(dsem[o.dkey], 16)
            elif o.kind == "coll":
                prev = o.dval - 1
                if prev > 0 and sn.get(o.dkey, 0) < prev:
                    h.wait_ge(dsem[o.dkey], prev)
                    self.nwait += 1
                    sn[o.dkey] = prev
                o.fn().then_inc(dsem[o.dkey])
            elif o.kind == "barwait":
                pass
            else:
                if o.kind == "mark":
                    if e == "pe":
                        ins = nc.tensor.nop()
                    elif e == "act":
                        ins = nc.scalar.copy(out=self.marktile["act"][:, 0:4], in_=self.marktile["act"][:, 4:8])
                    elif e == "dve":
                        ins = nc.vector.memset(self.marktile["dve"][:], 0.0)
                    else:
                        ins = nc.gpsimd.memset(self.marktile["pool"][:], 0.0)
                else:
                    ins = o.fn()
                if o.needed:
                    ccnt[e] += 1
                    ins.then_inc(csem[e], 1)
                    o.val = ccnt[e]
            o.fn = None
        self.emitted = len(ops)

    def finish(self):
        assert self.pes is None
        self.barrier()
        self.flush()
        self.stats = dict(nops=len(self.ops), nwait=self.nwait)
        self.es.close()


class Cfg:
    def __init__(self, D=4096, S=8192):
        self.D = D; self.S = S
        self.KT = D // 128
        self.DC = D // 8; self.DCT = self.DC // 128
        self.DI = 2 * D; self.DIc = self.DI // 8; self.XT = self.DIc // 128
        self.P = 64; self.N = 128; self.R = self.DIc // 64
        self.HHc = D // 128 // 8; self.HW = self.HHc * 128
        self.E = 16; self.cap = 2 * S // 16; self.F = D // 2; self.FT = self.F // 128
        self.NT = S // 128
        c = self
        o = 0
        c.oz = o; o += c.DIc
        c.ox = o; o += c.DIc
        c.oB = o; o += c.N
        c.oC = o; o += c.N
        c.oq = o; o += c.HW
        c.off = o; o += c.HW
        c.ofb = o; o += c.HW
        c.oi = o; o += c.HW
        c.og = o; o += c.HW
        c.oga = o; o += c.DC
        c.ogb = o; o += c.DC
        c.odt = o; o += 2 * c.R
        c.NCOL = o
        c.CT3 = (c.DIc + 2 * c.N) // 128


def _eng(nc, e):
    return {"dve": nc.vector, "pool": nc.gpsimd}[e]


class Prog:
    def __init__(self, cfg, debug=()):
        self.cfg = cfg
        self.debug = set(debug)
        self.nc = bass.Bass("TRN2", target_bir_lowering=False)
        self.k = K(self.nc)
        self.k._init_emit()

    def TT(self, e, out, in0, in1, op, r, w, acc=False):
        en = _eng(self.nc, e)
        return self.k.op(e, lambda: en.tensor_tensor(out=out, in0=in0, in1=in1, op=op), r, w, acc)

    def TS(self, e, out, in0, s1, s2, op0, op1=None, r=(), w=(), acc=False, accum_out=None):
        en = _eng(self.nc, e)
        if op1 is None:
            return self.k.op(e, lambda: en.tensor_scalar(out=out, in0=in0, scalar1=s1, scalar2=None, op0=op0), r, w, acc)
        if accum_out is not None:
            return self.k.op(e, lambda: en.tensor_scalar(out=out, in0=in0, scalar1=s1, scalar2=s2, op0=op0, op1=op1, accum_out=accum_out), r, w, acc)
        return self.k.op(e, lambda: en.tensor_scalar(out=out, in0=in0, scalar1=s1, scalar2=s2, op0=op0, op1=op1), r, w, acc)

    def STT(self, out, in0, scalar, in1, op0, op1, r, w, acc=False):
        nc = self.nc
        return self.k.op("dve", lambda: nc.vector.scalar_tensor_tensor(out=out, in0=in0, scalar=scalar, in1=in1, op0=op0, op1=op1), r, w, acc)

    def AC(self, out, in_, func, r, w, bias=None, scale=None, acc=False, accum_out=None):
        nc = self.nc
        kw = {}
        if bias is not None:
            kw["bias"] = bias
        if scale is not None:
            kw["scale"] = scale
        if accum_out is not None:
            kw["accum_out"] = accum_out
        return self.k.op("act", lambda: nc.scalar.activation(out=out, in_=in_, func=func, **kw), r, w, acc)

    def MM(self, ps, lhsT, rhs, start, stop, r, w, acc=None):
        nc = self.nc
        if acc is None:
            acc = not start
        return self.k.op("pe", lambda: nc.tensor.matmul(ps, lhsT=lhsT, rhs=rhs, start=start, stop=stop), r, w, acc)

    def TR(self, ps, in_, ident, r, w, acc=False):
        nc = self.nc
        return self.k.op("pe", lambda: nc.tensor.transpose(ps, in_, ident), r, w, acc)

    def MEMSET(self, e, ap, val, w, acc=False):
        en = _eng(self.nc, e)
        return self.k.op(e, lambda: en.memset(ap, val), (), w, acc)

    def dbg(self, name, src_buf, shape, dtype=F32):
        if name not in self.debug:
            return
        o = self.k.ext("dbg_" + name, shape, dtype, "ExternalOutput")
        rows = shape[0]
        for r0 in range(0, rows, 128):
            r1 = min(rows, r0 + 128)
            self.k.dma("sp", o[r0:r1, :], src_buf[r0:r1, :], r=[src_buf], w=[o], acc=True)

    def consts(self):
        k, nc = self.k, self.nc
        self.ones_f = k.sb("ones_f", [128, 128]); self.ident_f = k.sb("ident_f", [128, 128])
        self.ones_b = k.sb("ones_b", [128, 128], BF16); self.ident_b = k.sb("ident_b", [128, 128], BF16)
        self.eps = k.sb("eps", [128, 1]); self.onec = k.sb("onec", [128, 1])
        self.maskF = k.sb("maskF", [128, 128]); self.maskB = k.sb("maskB", [128, 128])
        self.MEMSET("pool", self.ones_f[:], 1.0, [self.ones_f])
        self.MEMSET("pool", self.ones_b[:], 1.0, [self.ones_b])
        self.MEMSET("pool", self.eps[:], 1e-6, [self.eps])
        self.MEMSET("pool", self.onec[:], 1.0, [self.onec])
        of, idf, mF, mB = self.ones_f, self.ident_f, self.maskF, self.maskB
        k.op("pool", lambda: nc.gpsimd.affine_select(out=idf[:], in_=of[:], pattern=[[-1, 128]], compare_op=ALU.is_equal, fill=0.0, base=0, channel_multiplier=1), [of], [idf])
        k.op("pool", lambda: nc.gpsimd.affine_select(out=mF[:], in_=of[:], pattern=[[1, 128]], compare_op=ALU.is_ge, fill=0.0, base=0, channel_multiplier=-1), [of], [mF])
        k.op("pool", lambda: nc.gpsimd.affine_select(out=mB[:], in_=of[:], pattern=[[-1, 128]], compare_op=ALU.is_ge, fill=0.0, base=0, channel_multiplier=1), [of], [mB])
        ib = self.ident_b
        k.op("pool", lambda: nc.gpsimd.tensor_copy(out=ib[:], in_=idf[:]), [idf], [ib])

    def gemm(self, segs, C, T, epi, TB=512, t0=0, tag="g", CG=512):
        k = self.k
        nseg = len(segs)
        wr = [[k.sb(f"{tag}w{si}_{b}", [128, segs[si][2], CG], BF16) for b in range(2)] for si in range(nseg)]
        ar = [[k.sb(f"{tag}a{si}_{b}", [128, segs[si][2], TB], BF16) for b in range(2)] for si in range(nseg)]
        psr = [k.ps(f"{tag}ps{b}", [128, TB]) for b in range(3)]
        total = sum(s[2] for s in segs)
        na = 0; npz = 0
        for cg in range((C + CG - 1) // CG):
            c0 = cg * CG; cw = min(CG, C - c0)
            wbs = []
            for si, (act, kt0, nkt, wfn, wbuf) in enumerate(segs):
                wb = wr[si][cg % 2]
                k.dma("pool", wb[:, :, :cw], wfn(c0, cw), r=[wbuf], w=[wb])
                wbs.append(wb)
            for tb in range(T // TB):
                abs_ = []
                for si, (act, kt0, nkt, wfn, wbuf) in enumerate(segs):
                    ab = ar[si][na % 2]
                    src = act.t.ap().rearrange("(kt p) s -> p kt s", p=128)[:, kt0:kt0 + nkt, t0 + tb * TB: t0 + (tb + 1) * TB]
                    k.dma("sp", ab[:], src, r=[act], w=[ab])
                    abs_.append(ab)
                na += 1
                for ct in range((cw + 127) // 128):
                    m = min(128, cw - ct * 128)
                    ps = psr[npz % 3]; npz += 1
                    i = 0
                    for si, (act, kt0, nkt, wfn, wbuf) in enumerate(segs):
                        for kt in range(nkt):
                            self.MM(ps[:m, :], wbs[si][:, kt, ct * 128: ct * 128 + m], abs_[si][:, kt, :],
                                    i == 0, i == total - 1, [wbs[si], abs_[si]], [ps])
                            i += 1
                    epi(c0 + ct * 128, m, tb, ps)

    def ag(self, src, dst):
        self.k.allgather(src, dst)

    def declare_inputs(self):
        c, k = self.cfg, self.k
        I = {}
        def inp(name, shape, dt=F32):
            I[name] = k.ext(name, shape, dt, "ExternalInput")
        inp("xT", [c.DC, c.S]); inp("c_l", [128, c.KT]); inp("w_ada", [c.D, c.D]); inp("b_ada_l", [128, c.KT])
        inp("npm_l", [128, c.KT]); inp("npf_l", [128, c.KT]); inp("npom_l", [128, c.DCT]); inp("npof_l", [128, c.DCT])
        inp("w_in", [c.D, c.NCOL]); inp("convw_l", [128, c.CT3, 5]); inp("convb_l", [128, c.CT3])
        inp("par8", [8 * c.R, 8]); inp("par2", [2 * c.R, 2 + 2 * c.R]); inp("par2b", [2 * c.R, 2])
        inp("dskip_bc", [128, c.DIc]); inp("ssdw_bc", [128, c.DIc])
        inp("lb_l", [128, c.HHc, 2]); inp("hgw_l", [128, c.HHc])
        inp("w_so", [c.DI, c.DC]); inp("w_ho", [c.D, c.DC]); inp("w_mo", [c.D, c.DC])
        inp("w_r_l", [128, c.KT, 16]); inp("w_g", [2 * c.D, c.F]); inp("w_u", [2 * c.D, c.F]); inp("w_d", [16 * c.F, c.DC])
        inp("esel", [16, 2]); inp("onesel", [16, 16 * 128]); inp("cidx", [128, max(1, c.cap // 128)])
        inp("rowmask", [128, 4]); inp("bmask", [128, 256])
        self.I = I
        self.outT = k.ext("outT", [c.DC, c.S], F32, "ExternalOutput")

    def wview(self, wbuf, kt0, nkt, r0=0):
        def fn(c0, cw):
            return wbuf.t.ap()[r0:, :].rearrange("(kt p) c -> p kt c", p=128)[:, kt0:kt0 + nkt, c0:c0 + cw]
        return fn

    def phase0(self):
        c, k, nc, I = self.cfg, self.k, self.nc, self.I
        KT = c.KT
        self.xTb = k.dram("xTb", [c.DC, c.S]); self.xTf = k.dram("xTf", [c.D, c.S])
        for r0 in range(0, c.DC, 128):
            k.dma("sp", self.xTb[r0:r0 + 128, :], I["xT"][r0:r0 + 128, :], r=[I["xT"]], w=[self.xTb], acc=True)
        self.ag(self.xTb, self.xTf)
        self.A_m = k.sb("A_m", [128, KT]); self.B_m = k.sb("B_m", [128, KT])
        self.A_f = k.sb("A_f", [128, KT]); self.B_f = k.sb("B_f", [128, KT])
        self.Gm = k.sb("Gm", [128, c.DCT]); self.Gf = k.sb("Gf", [128, c.DCT])
        k.phase_begin()
        nj = 6 * KT // 8
        cl = k.sb("cl", [128, KT]); cact = k.sb("cact", [128, KT]); bl = k.sb("bl", [128, KT]); modl = k.sb("modl", [128, KT])
        k.dma("sp", cl[:], I["c_l"][:, :], r=[I["c_l"]], w=[cl])
        k.dma("sp", bl[:], I["b_ada_l"][:, :], r=[I["b_ada_l"]], w=[bl])
        self.AC(cact[:], cl[:], ACT.Silu, [cl], [cact])
        CGW = 512 if c.D >= 512 else c.D
        war = [k.sb(f"wa{b}", [128, KT, CGW]) for b in range(2)]
        psmod = k.ps("psmod", [128, KT])
        wv = I["w_ada"].t.ap().rearrange("(kt p) c -> p kt c", p=128)
        first = True
        for cg in range(c.D // CGW):
            wb = war[cg % 2]
            k.dma("sp", wb[:], wv[:, :, cg * CGW:(cg + 1) * CGW], r=[I["w_ada"]], w=[wb])
            for j in range(CGW // 128):
                col = cg * (CGW // 128) + j
                for kt in range(KT):
                    self.MM(psmod[:, col:col + 1], wb[:, kt, j * 128:(j + 1) * 128], cact[:, kt:kt + 1],
                            kt == 0, kt == KT - 1, [wb, cact], [psmod], acc=not first)
                    first = False
        self.TT("dve", modl[:], psmod[:], bl[:], ALU.add, [psmod, bl], [modl])
        modb = k.dram("modb", [nj, 128]); modg = k.dram("modg", [8 * nj, 128])
        pst = k.ps("pst", [128, 128])
        tsb = k.sb("tsb", [128, 128])
        self.TR(pst[:nj, :], modl[:, 0:nj], self.ident_f[:], [modl, self.ident_f], [pst])
        self.AC(tsb[:nj, :], pst[:nj, :], ACT.Copy, [pst], [tsb])
        k.dma("sp", modb[:, :], tsb[:nj, :], r=[tsb], w=[modb])
        self.ag(modb, modg)
        mod = k.sb("mod", [128, 6 * KT])
        half = 3 * KT
        for hh in range(2):
            gsb = k.sb(f"gsb{hh}", [128, 128])
            k.dma("sp", gsb[:half, :], modg[hh * half:(hh + 1) * half, :], r=[modg], w=[gsb])
            pst2 = k.ps(f"pst2{hh}", [128, 128])
            self.TR(pst2[:, :half], gsb[:half, :], self.ident_f[:half, :half], [gsb, self.ident_f], [pst2])
            self.AC(mod[:, hh * half:(hh + 1) * half], pst2[:, :half], ACT.Copy, [pst2], [mod], acc=(hh == 1))
        nm = k.sb("nm", [128, 2 * KT + 2 * c.DCT])
        k.dma("sp", nm[:, 0:KT], I["npm_l"][:, :], r=[I["npm_l"]], w=[nm])
        k.dma("sp", nm[:, KT:2 * KT], I["npf_l"][:, :], r=[I["npf_l"]], w=[nm], acc=True)
        k.dma("sp", nm[:, 2 * KT:2 * KT + c.DCT], I["npom_l"][:, :], r=[I["npom_l"]], w=[nm], acc=True)
        k.dma("sp", nm[:, 2 * KT + c.DCT:], I["npof_l"][:, :], r=[I["npof_l"]], w=[nm], acc=True)
        self.STT(self.A_m[:], mod[:, KT:2 * KT], 1.0, nm[:, 0:KT], ALU.add, ALU.mult, [mod, nm], [self.A_m])
        self.k.op("dve", (lambda a=self.B_m, m=mod: nc.vector.tensor_copy(out=a[:], in_=m[:, 0:KT])), [mod], [self.B_m])
        self.STT(self.A_f[:], mod[:, 4 * KT:5 * KT], 1.0, nm[:, KT:2 * KT], ALU.add, ALU.mult, [mod, nm], [self.A_f])
        self.k.op("dve", (lambda a=self.B_f, m=mod: nc.vector.tensor_copy(out=a[:], in_=m[:, 3 * KT:4 * KT])), [mod], [self.B_f])
        self.TT("dve", self.Gm[:], modl[:, nj:nj + c.DCT], nm[:, 2 * KT:2 * KT + c.DCT], ALU.mult, [modl, nm], [self.Gm])
        self.TT("dve", self.Gf[:], modl[:, nj + c.DCT:nj + 2 * c.DCT], nm[:, 2 * KT + c.DCT:], ALU.mult, [modl, nm], [self.Gf])
        k.phase_end()

    def norm_phase(self, src, A, B, dst, router=False):
        c, k, nc, I = self.cfg, self.k, self.nc, self.I
        KT = c.KT; TBn = 256
        k.phase_begin()
        xr = [k.sb(f"nx{b}", [128, KT, TBn]) for b in range(2)]
        sq = [k.sb(f"nsq{b}", [128, KT, TBn]) for b in range(1)]
        hb = [k.sb(f"nhb{b}", [128, KT, TBn], BF16) for b in range(2)]
        rs = [k.sb(f"nrs{b}", [128, TBn]) for b in range(2)]
        pss = [k.ps(f"nps{b}", [128, TBn]) for b in range(2)]
        sv = src.t.ap().rearrange("(kt p) s -> p kt s", p=128)
        dv = dst.t.ap().rearrange("(kt p) s -> p kt s", p=128)
        if router:
            wr = k.sb("wr", [128, KT, 16])
            k.dma("sp", wr[:], I["w_r_l"][:, :, :], r=[I["w_r_l"]], w=[wr])
            psl = [k.ps(f"psl{b}", [16, TBn]) for b in range(2)]
            pstr = [k.ps(f"pstr{b}", [128, 512]) for b in range(2)]
            htk = [k.sb(f"htk{b}", [128, c.D], BF16) for b in range(2)]
            lgt = [k.sb(f"lgt{b}", [16, TBn]) for b in range(2)]
        A3 = A[:, :].unsqueeze(2).broadcast_to([128, KT, TBn])
        B3 = B[:, :].unsqueeze(2).broadcast_to([128, KT, TBn])
        for tb in range(c.S // TBn):
            x = xr[tb % 2]; s = sq[0]; h = hb[tb % 2]; r_ = rs[tb % 2]; ps = pss[tb % 2]
            k.dma("sp", x[:], sv[:, :, tb * TBn:(tb + 1) * TBn], r=[src], w=[x])
            self.AC(s[:].rearrange("p a b -> p (a b)"), x[:].rearrange("p a b -> p (a b)"), ACT.Square, [x], [s])
            for kt in range(KT):
                self.MM(ps[:], self.ones_f[:], s[:, kt, :], kt == 0, kt == KT - 1, [self.ones_f, s], [ps])
            self.AC(r_[:], ps[:], ACT.Sqrt, [ps, self.eps], [r_], bias=self.eps[:], scale=1.0 / c.D)
            nc_ = nc
            k.op("dve", (lambda r_=r_: nc_.vector.reciprocal(out=r_[:], in_=r_[:])), [r_], [r_])
            self.TT("dve", s[:], x[:], r_[:].unsqueeze(1).broadcast_to([128, KT, TBn]), ALU.mult, [x, r_], [s])
            self.TT("pool", s[:], s[:], A3, ALU.mult, [s, A], [s])
            if not router:
                self.TT("dve", h[:], s[:], B3, ALU.add, [s, B], [h])
            else:
                self.TT("dve", s[:], s[:], B3, ALU.add, [s, B], [s])
                self.AC(h[:].rearrange("p a b -> p (a b)"), s[:].rearrange("p a b -> p (a b)"), ACT.Copy, [s], [h])
                pl = psl[tb % 2]
                for kt in range(KT):
                    self.MM(pl[:], wr[:, kt, :], s[:, kt, :], kt == 0, kt == KT - 1, [wr, s], [pl])
                lt = lgt[tb % 2]
                self.TS("dve", lt[:], pl[:], 1.0, None, ALU.mult, None, [pl], [lt])
                k.dma("sp", self.logits[:, tb * TBn:(tb + 1) * TBn], lt[:], r=[lt], w=[self.logits], acc=True)
                for th in range(TBn // 128):
                    ht = htk[(tb * 2 + th) % 2]
                    for g4 in range(KT // 4):
                        pt = pstr[g4 % 2]
                        for q in range(4):
                            kt = g4 * 4 + q
                            self.MM(pt[:, q * 128:(q + 1) * 128], h[:, kt, th * 128:(th + 1) * 128], self.ident_b[:], True, True, [h, self.ident_b], [pt], acc=(q > 0))
                        if g4 % 2 == 0:
                            self.AC(ht[:, g4 * 512:(g4 + 1) * 512], pt[:], ACT.Copy, [pt], [ht], acc=(g4 > 0))
                        else:
                            k.op("pool" if False else "dve", (lambda ht=ht, pt=pt, g4=g4: nc_.vector.tensor_copy(out=ht[:, g4 * 512:(g4 + 1) * 512], in_=pt[:])), [pt], [ht], acc=True)
                    t_ = tb * (TBn // 128) + th
                    k.dma("sp", self.h2tok[t_ * 128:(t_ + 1) * 128, :], ht[:], r=[ht], w=[self.h2tok], acc=True)
            k.dma("sp", dv[:, :, tb * TBn:(tb + 1) * TBn], h[:], r=[h], w=[dst], acc=True)
        k.phase_end()

    def phase2(self):
        c, k, nc, I = self.cfg, self.k, self.nc, self.I
        self.projT = k.dram("projT", [c.NCOL, c.S])
        k.phase_begin()
        ot = [k.sb(f"po{b}", [128, 512]) for b in range(3)]
        cnt = [0]
        def func_for(c0):
            if c0 < c.ox: return ACT.Silu
            if c.og <= c0 < c.oga: return ACT.Silu
            if c.oga <= c0 < c.odt: return ACT.Sigmoid
            return ACT.Copy
        def epi(c0, m, tb, ps):
            o = ot[cnt[0] % 3]; cnt[0] += 1
            self.AC(o[:m, :], ps[:m, :], func_for(c0), [ps], [o])
            k.dma("sp", self.projT[c0:c0 + m, tb * 512:(tb + 1) * 512], o[:m, :], r=[o], w=[self.projT], acc=True)
        self.gemm([(self.hT, 0, c.KT, self.wview(I["w_in"], 0, c.KT), I["w_in"])], c.NCOL, c.S, epi, TB=512, tag="ip", CG=1024)
        k.phase_end()

    def phase3_prep(self):
        c, k, nc, I = self.cfg, self.k, self.nc, self.I
        R = c.R; PR = 8 * R; S = c.S; NC = c.NT
        self.Qd = k.dram("Qd", [PR, S]); self.etd = k.dram("etd", [PR, NC])
        self.xcT = k.dram("xcT", [c.DIc, S]); self.BCb = k.dram("BCb", [2 * c.N, S], BF16)
        k.phase_begin()
        par = k.sb("par", [PR, 8]); k.dma("sp", par[:], I["par8"][:, :], r=[I["par8"]], w=[par])
        b1 = k.sb("b1", [PR, S]); b2 = k.sb("b2", [PR, S]); b3 = k.sb("b3", [PR, S]); b4 = k.sb("b4", [PR, S]); b5 = k.sb("b5", [PR, S])
        rm = k.sb("rm128", [PR, S], BF16)
        aexp = k.sb("aexp", [PR, 1]); totc = k.sb("totc", [PR, NC]); etot = k.sb("etot", [PR, NC])
        for blk in range(4):
            k.dma("sp", b1[blk * 2 * R:(blk + 1) * 2 * R, :], self.projT[c.odt:c.odt + 2 * R, :], r=[self.projT], w=[b1], acc=(blk > 0))
        self.MEMSET("pool", rm[:], 1.0, [rm])
        self.MEMSET("pool", rm[:].rearrange("p (c t) -> p c t", t=128)[:, :, 0:1], 0.0, [rm])
        self.AC(aexp[:], par[:, 1:2], ACT.Exp, [par], [aexp])
        self.AC(b1[:], b1[:], ACT.Exp, [b1, par], [b1], bias=par[:, 0:1])
        self.AC(b1[:], b1[:], ACT.Ln, [b1, self.onec], [b1], bias=self.onec[:PR, :])
        self.TS("dve", b2[:], b1[:], aexp[:, 0:1], -1.0, ALU.mult, ALU.mult, [b1, aexp], [b2])
        k.op("dve", lambda: nc.vector.tensor_tensor_scan(out=b3[:], data0=rm[:], data1=b2[:], initial=0.0, op0=ALU.mult, op1=ALU.add), [rm, b2], [b3])
        cs3 = b3[:].rearrange("p (c t) -> p c t", t=128)
        k.op("dve", lambda: nc.vector.tensor_copy(out=totc[:].unsqueeze(2), in_=cs3[:, :, 127:128]), [b3], [totc])
        tot3 = totc[:].unsqueeze(2).broadcast_to([PR, NC, 128])
        v3 = lambda b: b[:].rearrange("p (c t) -> p c t", t=128)
        self.TT("dve", b4[:], b2[:], b3[:], ALU.subtract, [b2, b3], [b4])
        self.TT("dve", v3(b4), v3(b4), tot3, ALU.add, [b4, totc], [b4])
        self.TS("dve", b5[:], b3[:], par[:, 6:7], None, ALU.mult, None, [b3, par], [b5])
        self.STT(b5[:], b4[:], par[:, 7:8], b5[:], ALU.mult, ALU.add, [b4, par, b5], [b5])
        self.TT("dve", v3(b4), tot3, v3(b3), ALU.subtract, [totc, b3], [b4])
        self.TS("dve", b4[:], b4[:], par[:, 6:7], None, ALU.mult, None, [b4, par], [b4])
        self.TT("dve", b2[:], b3[:], b2[:], ALU.subtract, [b3, b2], [b2])
        self.STT(b4[:], b2[:], par[:, 7:8], b4[:], ALU.mult, ALU.add, [b2, par, b4], [b4])
        self.AC(b4[:], b4[:], ACT.Exp, [b4], [b4])
        self.AC(b3[:], b5[:], ACT.Exp, [b5], [b3])
        self.TS("dve", b1[:], b1[:], par[:, 2:3], None, ALU.mult, None, [b1, par], [b1])
        self.STT(b1[:], b5[:], par[:, 3:4], b1[:], ALU.mult, ALU.add, [b5, par, b1], [b1])
        self.STT(b1[:], b4[:], par[:, 4:5], b1[:], ALU.mult, ALU.add, [b4, par, b1], [b1])
        self.STT(b1[:], b3[:], par[:, 5:6], b1[:], ALU.mult, ALU.add, [b3, par, b1], [b1])
        self.AC(etot[:], totc[:], ACT.Exp, [totc], [etot])
        k.dma("sp", self.Qd[:, :], b1[:], r=[b1], w=[self.Qd])
        k.dma("sp", self.etd[:, :], etot[:], r=[etot], w=[self.etd])
        k.phase_end()
        k.phase_begin()
        cw = k.sb("cw", [128, c.CT3, 5]); cb = k.sb("cb", [128, c.CT3])
        k.dma("sp", cw[:], I["convw_l"][:, :, :], r=[I["convw_l"]], w=[cw])
        k.dma("sp", cb[:], I["convb_l"][:, :], r=[I["convb_l"]], w=[cb])
        xin = [k.sb(f"xin{b}", [128, S + 4]) for b in range(2)]
        acc_ = [k.sb(f"cacc{b}", [128, S]) for b in range(2)]
        ob = [k.sb(f"cob{b}", [128, S], BF16) for b in range(1)]
        for ci in range(c.CT3):
            xi = xin[ci % 2]; a = acc_[ci % 2]
            self.MEMSET("pool", xi[:, 0:2], 0.0, [xi])
            self.MEMSET("pool", xi[:, S + 2:S + 4], 0.0, [xi], acc=True)
            k.dma("sp", xi[:, 2:S + 2], self.projT[c.ox + ci * 128: c.ox + (ci + 1) * 128, :], r=[self.projT], w=[xi], acc=True)
            self.TS("dve", a[:], xi[:, 0:S], cw[:, ci, 0:1], None, ALU.mult, None, [xi, cw], [a])
            for j in range(1, 5):
                self.STT(a[:], xi[:, j:S + j], cw[:, ci, j:j + 1], a[:], ALU.mult, ALU.add, [xi, cw, a], [a])
            if ci < c.XT:
                self.AC(a[:], a[:], ACT.Silu, [a, cb], [a], bias=cb[:, ci:ci + 1])
                k.dma("sp", self.xcT[ci * 128:(ci + 1) * 128, :], a[:], r=[a], w=[self.xcT], acc=True)
            else:
                o = ob[0]
                self.AC(o[:], a[:], ACT.Silu, [a, cb], [o], bias=cb[:, ci:ci + 1])
                j0 = (ci - c.XT) * 128
                k.dma("sp", self.BCb[j0:j0 + 128, :], o[:], r=[o], w=[self.BCb], acc=True)
        k.phase_end()

    def ssd_sweep(self, d):
        c, k, nc, I = self.cfg, self.k, self.nc, self.I
        R = c.R; PR = 8 * R; S = c.S; NC = c.NT; DIc = c.DIc; XT = c.XT
        HWD = min(512, DIc); H = DIc // HWD; HPH = HWD // 64; TPH = HWD // 128; G4 = HPH // 4
        if d == 0:
            self.yf = k.dram("yf", [S, DIc])
        else:
            self.yssd_b = k.dram("yssd_b", [DIc, S], BF16)
        k.phase_begin()
        idf = self.ident_f
        et = k.sb("et", [R, NC]); k.dma("sp", et[:], self.etd[d * R:(d + 1) * R, :], r=[self.etd], w=[et])
        diagE = k.sb("diagE", [R, NC, R])
        self.TT("dve", diagE[:], idf[0:R, 0:R].unsqueeze(1).broadcast_to([R, NC, R]), et[:].unsqueeze(2).broadcast_to([R, NC, R]), ALU.mult, [idf, et], [diagE])
        edec = k.sb("edec", [128, NC, R])
        negI = k.sb("negI", [R, R, 128])
        self.TS("dve", negI[:], idf[0:R, 0:R].unsqueeze(2).broadcast_to([R, R, 128]), -1.0, None, ALU.mult, None, [idf], [negI])
        xtp = k.ps("xtp", [128, 512]); ydg = k.ps("ydg", [128, 512]); yof = k.ps("yof", [128, 512]); stp = k.ps("stp", [128, 512])
        Dp = [k.ps(f"Dp{b}", [128, 512]) for b in range(2)]
        misc = k.ps("misc", [128, 512]); tb16 = k.ps("tb16", [128, 384])
        cbT = qtp = edp = misc
        btp = ybt = tb16
        dE = diagE[:].rearrange("p a b -> p (a b)"); eD = edec[:].rearrange("p a b -> p (a b)")
        ncol = NC * R
        for b0 in range(0, ncol, 256):
            w_ = min(256, ncol - b0)
            self.MM(edp[:, 256:256 + w_], self.ones_f[0:R, :], dE[:, b0:b0 + w_], True, True, [self.ones_f, diagE], [edp])
            self.AC(eD[:, b0:b0 + w_], edp[:, 256:256 + w_], ACT.Copy, [edp], [edec], acc=(b0 > 0))
        mask = self.maskF if d == 0 else self.maskB
        st32 = k.sb("st32", [128, DIc]); stb = k.sb("stb", [128, DIc], BF16)
        self.MEMSET("dve", st32[:], 0.0, [st32]); self.MEMSET("dve", stb[:], 0.0, [stb])
        dsk = k.sb("dsk", [128, DIc]); snw = k.sb("snw", [128, DIc])
        if d == 1:
            k.dma("sp", dsk[:], I["dskip_bc"][:, :], r=[I["dskip_bc"]], w=[dsk])
            k.dma("sp", snw[:], I["ssdw_bc"][:, :], r=[I["ssdw_bc"]], w=[snw])
        NB = 2
        xch = [k.sb(f"xch{b}", [128, XT, 128]) for b in range(NB)]
        Bc = [k.sb(f"Bc{b}", [128, 128], BF16) for b in range(NB)]
        Cc = [k.sb(f"Cc{b}", [128, 128], BF16) for b in range(NB)]
        Qc = [k.sb(f"Qc{b}", [PR, 128]) for b in range(NB)]
        C0 = [k.sb(f"C0{b}", [R, 128]) for b in range(NB)]
        qtok = [k.sb(f"qtok{b}", [128, PR]) for b in range(NB)]
        dtds = [k.sb(f"dtds{b}", [128, R]) for b in range(NB)]
        Btok = [k.sb(f"Btok{b}", [128, 128], BF16) for b in range(NB)]
        cbm = [k.sb(f"cbm{b}", [128, 128]) for b in range(NB)]
        BD = [k.sb(f"BD{b}", [R, R, 128]) for b in range(NB)]
        xdt = [k.sb(f"xdt{b}", [128, HWD], BF16) for b in range(NB)]
        xds = [k.sb(f"xds{b}", [128, HWD], BF16) for b in range(NB)]
        Dm = [k.sb(f"Dm{b}", [128, 512]) for b in range(NB)]
        Mt = [k.sb(f"Mt{b}", [128, 4, 128], BF16) for b in range(NB)]
        ydir = [k.sb(f"ydir{b}", [128, DIc]) for b in range(NB)]
        if d == 1:
            yfc = [k.sb(f"yfc{b}", [128, DIc]) for b in range(NB)]
            zc = [k.sb(f"zc{b}", [128, XT, 128]) for b in range(NB)]
            ssq = [k.sb(f"ssq{b}", [128, 4]) for b in range(NB)]
            junk = k.sb("junk", [128, HWD])
            yn = [k.sb(f"yn{b}", [128, DIc], BF16) for b in range(NB)]
            ysb = [k.sb(f"ysb{b}", [128, XT, 128], BF16) for b in range(NB)]
        xv = self.xcT.t.ap().rearrange("(j p) s -> p j s", p=128)
        zv = self.projT.t.ap()[c.oz:c.oz + DIc, :].rearrange("(j p) s -> p j s", p=128)
        n4 = 0
        import os
        cut = int(os.environ.get("SSD_CUT", "9"))
        for ci in range(NC if cut > 0 else 0):
            ch = ci if d == 0 else NC - 1 - ci
            b = ci % NB
            sl = slice(ch * 128, (ch + 1) * 128)
            k.dma("sp", xch[b][:], xv[:, :, sl], r=[self.xcT], w=[xch[b]])
            k.dma("sp", Bc[b][:], self.BCb[0:128, sl], r=[self.BCb], w=[Bc[b]])
            k.dma("sp", Cc[b][:], self.BCb[128:256, sl], r=[self.BCb], w=[Cc[b]])
            k.dma("sp", Qc[b][:], self.Qd[:, sl], r=[self.Qd], w=[Qc[b]])
            k.dma("sp", C0[b][:], self.Qd[2 * R + d * R: 2 * R + (d + 1) * R, sl], r=[self.Qd], w=[C0[b]])
            if d == 1:
                k.dma("sp", yfc[b][:], self.yf[sl, :], r=[self.yf], w=[yfc[b]])
                k.dma("sp", zc[b][:], zv[:, :, sl], r=[self.projT], w=[zc[b]])
            if cut < 2:
                continue
            skip = os.environ.get("SSD_SKIP", "")
            if "qtp" not in skip:
                self.TR(qtp[:, 128:128 + PR], Qc[b][:, :], idf[0:PR, 0:PR], [Qc[b], idf], [qtp])
                self.AC(qtok[b][:], qtp[:, 128:128 + PR], ACT.Copy, [qtp], [qtok[b]])
            qt = qtok[b]
            if "dtds" not in skip:
              self.TT("dve", dtds[b][:], qt[:, d * R:(d + 1) * R], qt[:, 4 * R + d * R:4 * R + (d + 1) * R], ALU.mult, [qt], [dtds[b]])
            skip = os.environ.get("SSD_SKIP", "")
            if "btp" not in skip:
                self.MM(btp[:, 0:128], Bc[b][:], self.ident_b[:], True, True, [Bc[b], self.ident_b], [btp])
                self.AC(Btok[b][:], btp[:, 0:128], ACT.Copy, [btp], [Btok[b]])
            if "cbt" not in skip:
                self.MM(cbT[:, 0:128], Bc[b][:], Cc[b][:], True, True, [Bc[b], Cc[b]], [cbT])
                self.TT("dve", cbm[b][:], cbT[:, 0:128], mask[:], ALU.mult, [cbT, mask], [cbm[b]])
            if "bd" not in skip:
              self.TT("dve", BD[b][:], idf[0:R, 0:R].unsqueeze(2).broadcast_to([R, R, 128]), C0[b][:].unsqueeze(1).broadcast_to([R, R, 128]), ALU.mult, [idf, C0[b]], [BD[b]])
            if d == 1:
                self.MEMSET("dve", ssq[b][:], 0.0, [ssq[b]])
            if cut < 3:
                continue
            for hf in range(H):
                hs = slice(hf * HWD, (hf + 1) * HWD)
                for j in range(TPH):
                    self.TR(xtp[:, j * 128:(j + 1) * 128], xch[b][:, hf * TPH + j, :], idf[:], [xch[b], idf], [xtp], acc=(j > 0))
                r0 = hf * HPH
                x3 = xtp[:, 0:HWD].rearrange("p (r q) -> p r q", q=64)
                dt_b = qt[:, d * R + r0: d * R + r0 + HPH].unsqueeze(2).broadcast_to([128, HPH, 64])
                dd_b = dtds[b][:, r0:r0 + HPH].unsqueeze(2).broadcast_to([128, HPH, 64])
                E_b = qt[:, 6 * R + d * R + r0: 6 * R + d * R + r0 + HPH].unsqueeze(2).broadcast_to([128, HPH, 64])
                self.TT("dve", xdt[b][:].rearrange("p (r q) -> p r q", q=64), x3, dt_b, ALU.mult, [xtp, qt], [xdt[b]])
                self.TT("dve", xds[b][:].rearrange("p (r q) -> p r q", q=64), x3, dd_b, ALU.mult, [xtp, dtds[b]], [xds[b]])
                yd = ydir[b]
                if d == 1:
                    self.TT("dve", yd[:, hs], xtp[:, 0:HWD], dsk[:, hs], ALU.mult, [xtp, dsk], [yd], acc=(hf > 0))
                for g4 in range(G4 if cut > 3 else 0):
                    dp = Dp[n4 % 2]; dm = Dm[n4 % 2]; mt = Mt[n4 % 2]; n4 += 1
                    h0 = r0 + g4 * 4
                    self.MM(dp[:], self.ones_f[0:R, :], BD[b][:, h0:h0 + 4, :].rearrange("p a b -> p (a b)"), True, False, [self.ones_f, BD[b]], [dp], acc=False)
                    self.MM(dp[:], C0[b][:, :], negI[:, h0:h0 + 4, :].rearrange("p a b -> p (a b)"), False, True, [C0[b], negI], [dp], acc=True)
                    self.TS("dve", dm[:], dp[:], 0.0, None, ALU.min, None, [dp], [dm])
                    self.AC(dm[:], dm[:], ACT.Exp, [dm], [dm])
                    self.TT("dve", mt[:], dm[:].rearrange("p (a b) -> p a b", b=128), cbm[b][:].unsqueeze(1).broadcast_to([128, 4, 128]), ALU.mult, [dm, cbm[b]], [mt])
                    for h in range(4):
                        hl = g4 * 4 + h
                        self.MM(ydg[:, hl * 64:(hl + 1) * 64], mt[:, h, :], xdt[b][:, hl * 64:(hl + 1) * 64], True, True, [mt, xdt[b]], [ydg], acc=(hl > 0))
                if cut < 5:
                    continue
                self.MM(yof[:, 0:HWD], Cc[b][:], stb[:, hs], True, True, [Cc[b], stb], [yof])
                if d == 0:
                    self.TT("dve", yd[:, hs].rearrange("p (r q) -> p r q", q=64), yof[:, 0:HWD].rearrange("p (r q) -> p r q", q=64), E_b, ALU.mult, [yof, qt], [yd], acc=(hf > 0))
                else:
                    tmp = Dm[n4 % 2]
                    self.TT("dve", tmp[:, 0:HWD].rearrange("p (r q) -> p r q", q=64), yof[:, 0:HWD].rearrange("p (r q) -> p r q", q=64), E_b, ALU.mult, [yof, qt], [tmp])
                    self.TT("dve", yd[:, hs], yd[:, hs], tmp[:, 0:HWD], ALU.add, [yd, tmp], [yd], acc=True)
                self.TT("dve", yd[:, hs], yd[:, hs], ydg[:, 0:HWD], ALU.add, [yd, ydg], [yd], acc=True)
                self.MM(stp[:, 0:HWD], Btok[b][:], xds[b][:], True, True, [Btok[b], xds[b]], [stp])
                e_b = edec[:, ch, r0:r0 + HPH].unsqueeze(2).broadcast_to([128, HPH, 64])
                self.TT("dve", st32[:, hs].rearrange("p (r q) -> p r q", q=64), st32[:, hs].rearrange("p (r q) -> p r q", q=64), e_b, ALU.mult, [st32, edec], [st32], acc=(hf > 0))
                self.TT("dve", st32[:, hs], st32[:, hs], stp[:, 0:HWD], ALU.add, [st32, stp], [st32], acc=True)
                self.AC(stb[:, hs], st32[:, hs], ACT.Copy, [st32], [stb], acc=(hf > 0))
                if d == 1:
                    self.TT("dve", yd[:, hs], yd[:, hs], yfc[b][:, hs], ALU.add, [yd, yfc[b]], [yd], acc=True)
                    for j in range(TPH):
                        self.TR(xtp[:, j * 128:(j + 1) * 128], zc[b][:, hf * TPH + j, :], idf[:], [zc[b], idf], [xtp], acc=(j > 0))
                    self.TT("dve", yd[:, hs], yd[:, hs], xtp[:, 0:HWD], ALU.mult, [yd, xtp], [yd], acc=True)
                    self.AC(junk[:], yd[:, hs], ACT.Square, [yd], [junk, ssq[b]], accum_out=ssq[b][:, hf:hf + 1])
            if cut < 6:
                continue
            if d == 0:
                k.dma("sp", self.yf[sl, :], ydir[b][:], r=[ydir[b]], w=[self.yf], acc=True)
            else:
                sq_ = ssq[b]
                if H > 1:
                    self.TT("dve", sq_[:, 0:1], sq_[:, 0:1], sq_[:, 1:2], ALU.add, [sq_], [sq_])
                self.AC(sq_[:, 2:3], sq_[:, 0:1], ACT.Sqrt, [sq_, self.eps], [sq_], bias=self.eps[:], scale=1.0 / DIc)
                k.op("dve", (lambda sq_=sq_: nc.vector.reciprocal(out=sq_[:, 3:4], in_=sq_[:, 2:3])), [sq_], [sq_])
                self.STT(yn[b][:], ydir[b][:], sq_[:, 3:4], snw[:], ALU.mult, ALU.mult, [ydir[b], sq_, snw], [yn[b]])
                for j in range(XT):
                    self.MM(ybt[:, 128 + (j % 2) * 128: 256 + (j % 2) * 128], yn[b][:, j * 128:(j + 1) * 128], self.ident_b[:], True, True, [yn[b], self.ident_b], [ybt])
                    self.AC(ysb[b][:, j, :], ybt[:, 128 + (j % 2) * 128: 256 + (j % 2) * 128], ACT.Copy, [ybt], [ysb[b]], acc=(j > 0))
                k.dma("sp", self.yssd_b.t.ap().rearrange("(j p) s -> p j s", p=128)[:, :, sl], ysb[b][:], r=[ysb[b]], w=[self.yssd_b], acc=True)
        k.phase_end()

    def phase4(self):
        c, k, nc, I = self.cfg, self.k, self.nc, self.I
        S = c.S; NT = c.NT; HHc = c.HHc; HW = c.HW
        NC32 = S // 32
        self.ofT = k.dram("ofT", [HW, S]); self.obT = k.dram("obT", [HW, S])
        k.phase_begin()
        idf, idb = self.ident_f, self.ident_b
        lbt = k.sb("lbt", [128, HHc, 2]); k.dma("sp", lbt[:], I["lb_l"][:, :, :], r=[I["lb_l"]], w=[lbt])
        lb = k.sb("lb", [128, HHc]); oml = k.sb("oml", [128, HHc])
        self.TT("dve", lb[:], lbt[:, :, 0], lbt[:, :, 1], ALU.subtract, [lbt], [lb])
        self.AC(lb[:], lb[:], ACT.Sigmoid, [lb], [lb])
        self.TS("dve", oml[:], lb[:], -1.0, 1.0, ALU.mult, ALU.add, [lb], [oml])
        rmask = k.sb("rmask", [128, 4]); k.dma("sp", rmask[:], I["rowmask"][:, :], r=[I["rowmask"]], w=[rmask])
        bm = k.sb("bm", [128, 256]); k.dma("sp", bm[:], I["bmask"][:, :], r=[I["bmask"]], w=[bm])
        PB = min(2048, S)
        rm = k.sb("rm32", [128, PB], BF16)
        self.MEMSET("pool", rm[:], 1.0, [rm])
        self.MEMSET("pool", rm[:].rearrange("p (c t) -> p c t", t=32)[:, :, 0:1], 0.0, [rm])
        t1 = k.sb("t1", [128, PB]); t2 = k.sb("t2", [128, PB]); t3 = k.sb("t3", [128, PB]); t4 = k.sb("t4", [128, PB])
        totc = k.sb("h_totc", [128, PB // 32])
        qt = [k.sb(f"qt{d}", [128, S], BF16) for d in range(2)]
        kt_ = [k.sb(f"kt{d}", [128, S], BF16) for d in range(2)]
        kd = [k.sb(f"kd{d}", [128, S], BF16) for d in range(2)]
        egl = [k.sb(f"egl{d}", [128, NC32]) for d in range(2)]
        vtok = k.sb("vtok", [128, NT, 128], BF16)
        pw = [k.ps(f"pw{d}", [128, 512]) for d in range(2)]
        pk = [k.ps(f"pk{d}", [128, 128]) for d in range(2)]
        pv = k.ps("pv", [128, 128])
        S32 = [k.sb(f"S32_{d}", [128, 128]) for d in range(2)]
        Sb = [[k.sb(f"Sb{d}_{b}", [128, 128], BF16) for b in range(2)] for d in range(2)]
        scm = [[k.sb(f"scm{d}_{b}", [128, 128], BF16) for b in range(2)] for d in range(2)]
        kdtok = [[k.sb(f"kdtok{d}_{b}", [128, 128], BF16) for b in range(2)] for d in range(2)]
        kdm = [[k.sb(f"kdm{d}_{b}", [128, 4, 128], BF16) for b in range(2)] for d in range(2)]
        oin = [[k.sb(f"oin{d}_{b}", [128, 128]) for b in range(2)] for d in range(2)]
        otl = [[k.sb(f"otl{d}_{b}", [128, 128]) for b in range(2)] for d in range(2)]
        vld = [k.sb(f"vld{b}", [128, 128]) for b in range(2)]
        for h in range(HHc):
            hr = h * 128
            for ti in range(NT):
                vb = vld[ti % 2]
                k.dma("sp", vb[:], self.projT[c.oi + hr: c.oi + hr + 128, ti * 128:(ti + 1) * 128], r=[self.projT], w=[vb])
                self.TR(pv[:], vb[:], idf[:], [vb, idf], [pv])
                self.AC(vtok[:, ti, :], pv[:], ACT.Copy, [pv], [vtok], acc=(ti > 0))
            for d in range(2):
                fo = c.off if d == 0 else c.ofb
                for pb in range(S // PB):
                    ps_ = slice(pb * PB, (pb + 1) * PB)
                    k.dma("sp", t1[:], self.projT[fo + hr: fo + hr + 128, ps_], r=[self.projT], w=[t1])
                    k.dma("sp", t4[:], self.projT[c.oq + hr: c.oq + hr + 128, ps_], r=[self.projT], w=[t4])
                    self.AC(t1[:], t1[:], ACT.Sigmoid, [t1], [t1])
                    self.TS("dve", t1[:], t1[:], oml[:, h:h + 1], lb[:, h:h + 1], ALU.mult, ALU.add, [t1, oml, lb], [t1])
                    self.AC(t2[:], t1[:], ACT.Ln, [t1], [t2])
                    self.TS("dve", t1[:], t1[:], -1.0, 1.0, ALU.mult, ALU.add, [t1], [t1])
                    k.op("dve", (lambda: nc.vector.tensor_tensor_scan(out=t3[:], data0=rm[:], data1=t2[:], initial=0.0, op0=ALU.mult, op1=ALU.add)), [rm, t2], [t3])
                    g3 = t3[:].rearrange("p (c t) -> p c t", t=32)
                    if d == 1:
                        k.op("dve", (lambda g3=g3: nc.vector.tensor_copy(out=totc[:].unsqueeze(2), in_=g3[:, :, 31:32])), [t3], [totc])
                        self.TT("dve", t3[:], t2[:], t3[:], ALU.subtract, [t2, t3], [t3])
                        self.TT("dve", g3, g3, totc[:].unsqueeze(2).broadcast_to([128, PB // 32, 32]), ALU.add, [t3, totc], [t3])
                    self.AC(t2[:], t3[:], ACT.Exp, [t3], [t2])
                    e3 = t2[:].rearrange("p (c t) -> p c t", t=32)
                    pos = 31 if d == 0 else 0
                    eg_dst = egl[d][:, pb * (PB // 32):(pb + 1) * (PB // 32)]
                    k.op("dve", (lambda e3=e3, eg_dst=eg_dst, pos=pos: nc.vector.tensor_copy(out=eg_dst.unsqueeze(2), in_=e3[:, :, pos:pos + 1])), [t2], [egl[d]], acc=(pb > 0))
                    self.TT("dve", qt[d][:, ps_], t4[:], t2[:], ALU.mult, [t4, t2], [qt[d]], acc=(pb > 0))
                    self.AC(t2[:], t3[:], ACT.Exp, [t3], [t2], scale=-1.0)
                    self.TT("dve", t1[:], t1[:], t2[:], ALU.mult, [t1, t2], [t1])
                    self.AC(kt_[d][:, ps_], t1[:], ACT.Copy, [t1], [kt_[d]], acc=(pb > 0))
                    self.TT("dve", kd[d][:, ps_].rearrange("p (c t) -> p c t", t=32), t1[:].rearrange("p (c t) -> p c t", t=32),
                            eg_dst.unsqueeze(2).broadcast_to([128, PB // 32, 32]), ALU.mult, [t1, egl[d]], [kd[d]], acc=(pb > 0))
                self.MEMSET("dve", S32[d][:], 0.0, [S32[d]])
                self.MEMSET("dve", Sb[d][0][:], 0.0, [Sb[d][0]])
            nS = [0, 0]
            for i in range(NT):
                for d in range(2):
                    ti = i if d == 0 else NT - 1 - i
                    tsl = slice(ti * 128, (ti + 1) * 128)
                    b = i % 2
                    P = pw[d]
                    scT = Buf(P.t, "scT"); oia = Buf(P.t, "oia"); oie = Buf(P.t, "oie"); stp = Buf(P.t, "stp")
                    self.MM(scT[:, 0:128], kt_[d][:, tsl], qt[d][:, tsl], True, True, [kt_[d], qt[d]], [P])
                    self.TT("dve", scm[d][b][:], P[:, 0:128], bm[:, d * 128:(d + 1) * 128], ALU.mult, [P, bm], [scm[d][b]])
                    self.MM(pk[d][:], kd[d][:, tsl], idb[:], True, True, [kd[d], idb], [pk[d]])
                    self.AC(kdtok[d][b][:], pk[d][:], ACT.Copy, [pk[d]], [kdtok[d][b]])
                    for j in range(4):
                        self.TS("dve", kdm[d][b][:, j, :], kdtok[d][b][:], rmask[:, j:j + 1], None, ALU.mult, None, [kdtok[d][b], rmask], [kdm[d][b]], acc=(j > 0))
                    self.MM(P[:, 128:256], vtok[:, ti, :], scm[d][b][:], True, True, [vtok, scm[d][b]], [P])
                    self.AC(oin[d][b][:], P[:, 128:256], ACT.Copy, [P], [oin[d][b]])
                    for jj in range(4):
                        j = jj if d == 0 else 3 - jj
                        cs_ = slice(ti * 128 + j * 32, ti * 128 + (j + 1) * 32)
                        sb_cur = Sb[d][nS[d] % 2]; sb_nxt = Sb[d][(nS[d] + 1) % 2]; nS[d] += 1
                        self.MM(P[:, 256 + j * 32:256 + (j + 1) * 32], sb_cur[:], qt[d][:, cs_], True, True, [sb_cur, qt[d]], [P])
                        self.MM(P[:, 384:512], kdm[d][b][:, j, :], vtok[:, ti, :], True, True, [kdm[d][b], vtok], [P])
                        cidx = ti * 4 + j
                        self.STT(S32[d][:], S32[d][:], egl[d][:, cidx:cidx + 1], P[:, 384:512], ALU.mult, ALU.add, [S32[d], egl[d], P], [S32[d]])
                        self.AC(sb_nxt[:], S32[d][:], ACT.Copy, [S32[d]], [sb_nxt])
                    self.TT("dve", otl[d][b][:], oin[d][b][:], P[:, 256:384], ALU.add, [oin[d][b], P], [otl[d][b]])
                    dst = self.ofT if d == 0 else self.obT
                    k.dma("sp", dst[hr:hr + 128, tsl], otl[d][b][:], r=[otl[d][b]], w=[dst], acc=True)
        k.phase_end()

    def phase4b(self):
        c, k, nc, I = self.cfg, self.k, self.nc, self.I
        S = c.S; HHc = c.HHc; HW = c.HW; TB = 512
        self.oT = k.dram("oT", [HW, S]); ssqb = k.dram("hssq_b", [1, S]); ssqg = k.dram("hssq_g", [8, S])
        self.yhg_b = k.dram("yhg_b", [HW, S], BF16)
        k.phase_begin()
        a = [k.sb(f"ha{b}", [128, HHc, TB]) for b in range(2)]
        bb = [k.sb(f"hb{b}", [128, HHc, TB]) for b in range(2)]
        sq = k.sb("hsq", [128, HHc, TB])
        ps = [k.ps(f"hps{b}", [128, TB]) for b in range(2)]
        row = [k.sb(f"hrow{b}", [1, TB]) for b in range(2)]
        ov = lambda t: t.t.ap().rearrange("(h p) s -> p h s", p=128)
        for tb in range(S // TB):
            ts_ = slice(tb * TB, (tb + 1) * TB); i = tb % 2
            k.dma("sp", a[i][:], ov(self.ofT)[:, :, ts_], r=[self.ofT], w=[a[i]])
            k.dma("sp", bb[i][:], ov(self.obT)[:, :, ts_], r=[self.obT], w=[bb[i]])
            self.TT("dve", a[i][:], a[i][:], bb[i][:], ALU.add, [a[i], bb[i]], [a[i]])
            self.AC(sq[:].rearrange("p a b -> p (a b)"), a[i][:].rearrange("p a b -> p (a b)"), ACT.Square, [a[i]], [sq])
            for h in range(HHc):
                self.MM(ps[i][:], self.ones_f[:], sq[:, h, :], h == 0, h == HHc - 1, [self.ones_f, sq], [ps[i]])
            self.AC(row[i][:], ps[i][0:1, :], ACT.Copy, [ps[i]], [row[i]])
            k.dma("sp", ssqb[0:1, ts_], row[i][:], r=[row[i]], w=[ssqb], acc=True)
            k.dma("sp", ov(self.oT)[:, :, ts_], a[i][:], r=[a[i]], w=[self.oT], acc=True)
        k.phase_end()
        self.ag(ssqb, ssqg)
        k.phase_begin()
        hgw = k.sb("hgw", [128, HHc]); k.dma("sp", hgw[:], I["hgw_l"][:, :], r=[I["hgw_l"]], w=[hgw])
        a = [k.sb(f"ha{b}", [128, HHc, TB]) for b in range(2)]
        g = [k.sb(f"hg{b}", [128, HHc, TB]) for b in range(2)]
        s8 = [k.sb(f"s8{b}", [8, TB]) for b in range(2)]
        rs = [k.sb(f"hrs{b}", [128, TB]) for b in range(2)]
        yo = [k.sb(f"hyo{b}", [128, HHc, TB], BF16) for b in range(2)]
        ps = [k.ps(f"hps{b}", [128, TB]) for b in range(2)]
        gv = self.projT.t.ap()[c.og:c.og + HW, :].rearrange("(h p) s -> p h s", p=128)
        for tb in range(S // TB):
            ts_ = slice(tb * TB, (tb + 1) * TB); i = tb % 2
            k.dma("sp", a[i][:], ov(self.oT)[:, :, ts_], r=[self.oT], w=[a[i]])
            k.dma("sp", g[i][:], gv[:, :, ts_], r=[self.projT], w=[g[i]])
            k.dma("sp", s8[i][:], ssqg[:, ts_], r=[ssqg], w=[s8[i]])
            self.MM(ps[i][:], self.ones_f[0:8, :], s8[i][:], True, True, [self.ones_f, s8[i]], [ps[i]])
            self.AC(rs[i][:], ps[i][:], ACT.Sqrt, [ps[i], self.eps], [rs[i]], bias=self.eps[:], scale=1.0 / c.D)
            k.op("dve", (lambda r_=rs[i]: nc.vector.reciprocal(out=r_[:], in_=r_[:])), [rs[i]], [rs[i]])
            self.TT("dve", a[i][:], a[i][:], rs[i][:].unsqueeze(1).broadcast_to([128, HHc, TB]), ALU.mult, [a[i], rs[i]], [a[i]])
            self.TT("pool", a[i][:], a[i][:], hgw[:, :].unsqueeze(2).broadcast_to([128, HHc, TB]), ALU.mult, [a[i], hgw], [a[i]])
            self.TT("dve", yo[i][:], a[i][:], g[i][:], ALU.mult, [a[i], g[i]], [yo[i]])
            k.dma("sp", ov(self.yhg_b)[:, :, ts_], yo[i][:], r=[yo[i]], w=[self.yhg_b], acc=True)
        k.phase_end()

    def phase5(self):
        c, k, nc, I = self.cfg, self.k, self.nc, self.I
        S = c.S; DC = c.DC; TB = 256
        self.yssdT = k.dram("yssdT", [c.DI, S], BF16); self.yhgT = k.dram("yhgT", [c.D, S], BF16)
        self.ag(self.yssd_b, self.yssdT); self.ag(self.yhg_b, self.yhgT)
        t1d = k.dram("t1d", [DC, S]); self.merged_b = k.dram("merged_b", [DC, S], BF16)
        self.mergedT = k.dram("mergedT", [c.D, S], BF16)
        k.phase_begin()
        sg = [k.sb(f"sg{b}", [128, TB]) for b in range(3)]; o = [k.sb(f"o5{b}", [128, TB]) for b in range(3)]
        cnt = [0]
        def epi_a(c0, m, tb, ps):
            i = cnt[0] % 3; cnt[0] += 1
            ts_ = slice(tb * TB, (tb + 1) * TB)
            k.dma("sp", sg[i][:m, :], self.projT[c.oga + c0: c.oga + c0 + m, ts_], r=[self.projT], w=[sg[i]])
            self.TT("dve", o[i][:m, :], ps[:m, :], sg[i][:m, :], ALU.mult, [ps, sg[i]], [o[i]])
            k.dma("sp", t1d[c0:c0 + m, ts_], o[i][:m, :], r=[o[i]], w=[t1d], acc=True)
        nk = c.DI // 128
        segs = []
        for s0 in range(0, nk, 32):
            n_ = min(32, nk - s0)
            segs.append((self.yssdT, s0, n_, self.wview(I["w_so"], s0, n_), I["w_so"]))
        self.gemm(segs, DC, S, epi_a, TB=TB, tag="ya")
        k.phase_end()
        k.phase_begin()
        sg = [k.sb(f"sg{b}", [128, TB]) for b in range(3)]; t1 = [k.sb(f"t1{b}", [128, TB]) for b in range(3)]
        o = [k.sb(f"o5{b}", [128, TB]) for b in range(3)]; ob = [k.sb(f"ob5{b}", [128, TB], BF16) for b in range(3)]
        cnt = [0]
        def epi_b(c0, m, tb, ps):
            i = cnt[0] % 3; cnt[0] += 1
            ts_ = slice(tb * TB, (tb + 1) * TB)
            k.dma("sp", sg[i][:m, :], self.projT[c.ogb + c0: c.ogb + c0 + m, ts_], r=[self.projT], w=[sg[i]])
            k.dma("sp", t1[i][:m, :], t1d[c0:c0 + m, ts_], r=[t1d], w=[t1[i]])
            self.TT("dve", o[i][:m, :], ps[:m, :], sg[i][:m, :], ALU.mult, [ps, sg[i]], [o[i]])
            self.TT("dve", ob[i][:m, :], o[i][:m, :], t1[i][:m, :], ALU.add, [o[i], t1[i]], [ob[i]])
            k.dma("sp", self.merged_b[c0:c0 + m, ts_], ob[i][:m, :], r=[ob[i]], w=[self.merged_b], acc=True)
        self.gemm([(self.yhgT, 0, c.KT, self.wview(I["w_ho"], 0, c.KT), I["w_ho"])], DC, S, epi_b, TB=TB, tag="yb")
        k.phase_end()
        self.ag(self.merged_b, self.mergedT)
        self.ymixT = k.dram("ymixT", [DC, S])
        self.resid_gemm(self.mergedT, c.KT, self.wview(I["w_mo"], 0, c.KT), I["w_mo"], self.ymixT, "mx")
        self.x1T_b = k.dram("x1T_b", [DC, S]); self.x1Tf = k.dram("x1Tf", [c.D, S])
        self.resid_pass(self.ymixT, self.I["xT"], self.Gm, self.x1T_b, "mx")
        self.ag(self.x1T_b, self.x1Tf)

    def resid_gemm(self, actT, nkt, wfn, wbuf, ydst, tag):
        c, k, nc = self.cfg, self.k, self.nc
        S = c.S; DC = c.DC; TB = 512
        ssqb = k.dram(f"{tag}_ssqb", [1, S]); ssqg = k.dram(f"{tag}_ssqg", [8, S])
        k.phase_begin()
        o = [k.sb(f"ro{b}", [128, TB]) for b in range(3)]; sq = [k.sb(f"rsq{b}", [128, TB]) for b in range(2)]
        pss = k.ps("rpss", [128, TB]); row = [k.sb(f"rrow{b}", [1, TB]) for b in range(2)]
        cnt = [0]
        def epi(c0, m, tb, ps):
            i = cnt[0] % 3; j = cnt[0] % 2; cnt[0] += 1
            ts_ = slice(tb * TB, (tb + 1) * TB)
            ct = c0 // 128
            self.AC(o[i][:m, :], ps[:m, :], ACT.Copy, [ps], [o[i]])
            self.AC(sq[j][:m, :], ps[:m, :], ACT.Square, [ps], [sq[j]])
            k.dma("sp", ydst[c0:c0 + m, ts_], o[i][:m, :], r=[o[i]], w=[ydst], acc=True)
            self.MM(pss[:], self.ones_f[:m, :], sq[j][:m, :], ct == 0, ct == c.DCT - 1, [self.ones_f, sq[j]], [pss])
            if ct == c.DCT - 1:
                self.AC(row[tb % 2][:], pss[0:1, :], ACT.Copy, [pss], [row[tb % 2]])
                k.dma("sp", ssqb[0:1, ts_], row[tb % 2][:], r=[row[tb % 2]], w=[ssqb], acc=True)
        self.gemm([(actT, 0, nkt, wfn, wbuf)], DC, S, epi, TB=TB, tag=tag)
        k.phase_end()
        self.ag(ssqb, ssqg)
        if not hasattr(self, "ssq8"):
            self.ssq8 = {}
        self.ssq8[tag] = ssqg

    def resid_pass(self, yT, srcT, G, dst, tag):
        c, k, nc = self.cfg, self.k, self.nc
        S = c.S; DCT = c.DCT; TB = 512
        ssqg = self.ssq8[tag]
        k.phase_begin()
        y = [k.sb(f"py{b}", [128, DCT, TB]) for b in range(2)]; x = [k.sb(f"px{b}", [128, DCT, TB]) for b in range(2)]
        s8 = [k.sb(f"ps8{b}", [8, TB]) for b in range(2)]; rs = [k.sb(f"prs{b}", [128, TB]) for b in range(2)]
        ps = [k.ps(f"pps{b}", [128, TB]) for b in range(2)]
        v = lambda t: t.t.ap().rearrange("(j p) s -> p j s", p=128)
        for tb in range(S // TB):
            ts_ = slice(tb * TB, (tb + 1) * TB); i = tb % 2
            k.dma("sp", y[i][:], v(yT)[:, :, ts_], r=[yT], w=[y[i]])
            k.dma("sp", x[i][:], v(srcT)[:, :, ts_], r=[srcT], w=[x[i]])
            k.dma("sp", s8[i][:], ssqg[:, ts_], r=[ssqg], w=[s8[i]])
            self.MM(ps[i][:], self.ones_f[0:8, :], s8[i][:], True, True, [self.ones_f, s8[i]], [ps[i]])
            self.AC(rs[i][:], ps[i][:], ACT.Sqrt, [ps[i], self.eps], [rs[i]], bias=self.eps[:], scale=1.0 / c.D)
            k.op("dve", (lambda r_=rs[i]: nc.vector.reciprocal(out=r_[:], in_=r_[:])), [rs[i]], [rs[i]])
            self.TT("dve", y[i][:], y[i][:], rs[i][:].unsqueeze(1).broadcast_to([128, DCT, TB]), ALU.mult, [y[i], rs[i]], [y[i]])
            self.TT("pool", y[i][:], y[i][:], G[:, :].unsqueeze(2).broadcast_to([128, DCT, TB]), ALU.mult, [y[i], G], [y[i]])
            self.TT("dve", x[i][:], x[i][:], y[i][:], ALU.add, [x[i], y[i]], [x[i]])
            k.dma("sp", v(dst)[:, :, ts_], x[i][:], r=[x[i]], w=[dst], acc=True)
        k.phase_end()

    def phase7(self):
        c, k, nc, I = self.cfg, self.k, self.nc, self.I
        S = c.S; NT = c.NT; cap = c.cap; D = c.D; F = c.F; KT = c.KT; FT = c.FT; DC = c.DC; DCT = c.DCT
        self.posd = k.dram("posd", [16, S]); self.gmd = k.dram("gmd", [16, S])
        ploc = k.sb("ploc", [128, NT, 2])
        k.phase_begin()
        lg = k.sb("lg", [16, S]); k.dma("sp", lg[:], self.logits[:, :], r=[self.logits], w=[lg])
        aff = lg; junk = k.sb("mjunk", [16, S]); cum = k.sb("cum", [16, S]); onesr = k.sb("onesr", [16, S], BF16)
        self.posm = cum; self.gm = aff
        pst = k.ps("mps", [16, 512]); rcp = k.sb("rcp", [16, 512])
        self.AC(aff[:], lg[:], ACT.Exp, [lg], [aff])
        for tb in range(S // 512):
            ts_ = slice(tb * 512, (tb + 1) * 512)
            self.MM(pst[:], self.ones_f[0:16, 0:16], aff[:, ts_], True, True, [self.ones_f, aff], [pst])
            k.op("dve", (lambda ts_=ts_: nc.vector.reciprocal(out=rcp[:], in_=pst[:])), [pst], [rcp])
            self.TT("dve", aff[:, ts_], aff[:, ts_], rcp[:], ALU.mult, [aff, rcp], [aff], acc=False)
        sc = k.sb("bis", [16, 8])
        self.MEMSET("dve", sc[:, 0:1], 0.0, [sc]); self.MEMSET("dve", sc[:, 1:2], 2.0, [sc], acc=True)
        lo, hi, mid, cn, se, dd = (sc[:, i:i + 1] for i in range(6))
        for it in range(40):
            self.TS("dve", mid, lo, hi, 0.5, ALU.add, ALU.mult, [sc], [sc])
            self.TS("dve", junk[:], aff[:], mid, 0.0, ALU.is_ge, ALU.add, [aff, sc], [junk, sc], accum_out=cn)
            self.TS("dve", se, cn, float(cap), None, ALU.is_ge, None, [sc], [sc])
            self.TT("dve", dd, mid, lo, ALU.subtract, [sc], [sc])
            self.STT(lo, dd, se, lo, ALU.mult, ALU.add, [sc], [sc])
            self.TT("dve", dd, hi, mid, ALU.subtract, [sc], [sc])
            self.STT(hi, dd, se, mid, ALU.mult, ALU.add, [sc], [sc])
        mask = junk
        self.TS("dve", mask[:], aff[:], lo, None, ALU.is_ge, None, [aff, sc], [mask])
        self.MEMSET("pool", onesr[:], 1.0, [onesr])
        k.op("dve", lambda: nc.vector.tensor_tensor_scan(out=cum[:], data0=onesr[:], data1=mask[:], initial=0.0, op0=ALU.mult, op1=ALU.add), [onesr, mask], [cum])
        self.TT("dve", self.posm[:], cum[:], mask[:], ALU.mult, [cum, mask], [self.posm])
        self.TS("dve", self.posm[:], self.posm[:], -1.0, None, ALU.add, None, [self.posm], [self.posm])
        self.TT("dve", self.gm[:], aff[:], mask[:], ALU.mult, [aff, mask], [self.gm])
        es = k.sb("esel", [16, 2]); k.dma("sp", es[:], I["esel"][:, :], r=[I["esel"]], w=[es])
        pp = k.ps("mpp", [128, NT * 2])
        for ti in range(NT):
            self.MM(pp[:, ti * 2:(ti + 1) * 2], self.posm[:, ti * 128:(ti + 1) * 128], es[:], True, True, [self.posm, es], [pp], acc=(ti > 0))
        self.AC(ploc[:].rearrange("p a b -> p (a b)"), pp[:], ACT.Copy, [pp], [ploc])
        k.dma("sp", self.posd[:, :], self.posm[:], r=[self.posm], w=[self.posd])
        k.dma("sp", self.gmd[:, :], self.gm[:], r=[self.gm], w=[self.gmd])
        k.phase_end()
        self.xgT = k.dram("xgT", [D, 2 * cap], BF16)
        CB = min(512, cap)
        k.phase_begin()
        io_i = k.sb("io_i", [128, CB], I32); io_f = k.sb("io_f", [128, CB])
        k.op("pool", lambda: nc.gpsimd.iota(io_i[:], pattern=[[1, CB]], base=0, channel_multiplier=0), (), [io_i])
        k.op("dve", lambda: nc.vector.tensor_copy(out=io_f[:], in_=io_i[:]), [io_i], [io_f])
        sel = k.sb("sel", [128, NT, CB], BF16)
        hl = [k.sb(f"hl{b}", [128, 512], BF16) for b in range(3)]
        gps = [k.ps(f"gps{b}", [128, CB]) for b in range(4)]
        go = [k.sb(f"go{b}", [128, CB], BF16) for b in range(2)]
        DG = min(512, D); ND = DG // 128
        nl = 0; ng = 0
        for j in range(2):
            for cb in range(cap // CB):
                for ti in range(NT):
                    self.TS("dve", sel[:, ti, :], io_f[:], float(cb * CB), ploc[:, ti, j:j + 1], ALU.add, ALU.is_equal, [io_f, ploc], [sel], acc=(ti > 0))
                for dg in range(D // DG):
                    for ti in range(NT):
                        h_ = hl[nl % 3]; nl += 1
                        k.dma("sp", h_[:, :DG], self.h2tok[ti * 128:(ti + 1) * 128, dg * DG:(dg + 1) * DG], r=[self.h2tok], w=[h_])
                        for q in range(ND):
                            self.MM(gps[q][:], h_[:, q * 128:(q + 1) * 128], sel[:, ti, :], ti == 0, ti == NT - 1, [h_, sel], [gps[q]])
                    for q in range(ND):
                        g_ = go[ng % 2]; ng += 1
                        self.AC(g_[:], gps[q][:], ACT.Copy, [gps[q]], [g_])
                        r0 = dg * DG + q * 128
                        k.dma("sp", self.xgT[r0:r0 + 128, j * cap + cb * CB: j * cap + (cb + 1) * CB], g_[:], r=[g_], w=[self.xgT], acc=True)
        k.phase_end()
        TBm = min(512, cap)
        gtmp = k.dram("gtmp", [2 * F, cap]); self.hid_b = k.dram("hid_b", [2 * F, cap], BF16); self.hidT = k.dram("hidT", [16 * F, cap], BF16)
        for j in range(2):
            k.phase_begin()
            o = [k.sb(f"eo{b}", [128, TBm]) for b in range(3)]
            cnt = [0]
            def epi_g(c0, m, tb, ps, j=j, o=o, cnt=cnt):
                i = cnt[0] % 3; cnt[0] += 1
                self.AC(o[i][:m, :], ps[:m, :], ACT.Silu, [ps], [o[i]])
                k.dma("sp", gtmp[j * F + c0: j * F + c0 + m, tb * TBm:(tb + 1) * TBm], o[i][:m, :], r=[o[i]], w=[gtmp], acc=True)
            self.gemm([(self.xgT, 0, KT, self.wview(I["w_g"], 0, KT, r0=j * D), I["w_g"])], F, cap, epi_g, TB=TBm, t0=j * cap, tag=f"eg{j}")
            k.phase_end()
            k.phase_begin()
            gl = [k.sb(f"gl{b}", [128, TBm]) for b in range(3)]; ob = [k.sb(f"eob{b}", [128, TBm], BF16) for b in range(3)]
            cnt = [0]
            def epi_u(c0, m, tb, ps, j=j, gl=gl, ob=ob, cnt=cnt):
                i = cnt[0] % 3; cnt[0] += 1
                k.dma("sp", gl[i][:m, :], gtmp[j * F + c0: j * F + c0 + m, tb * TBm:(tb + 1) * TBm], r=[gtmp], w=[gl[i]])
                self.TT("dve", ob[i][:m, :], ps[:m, :], gl[i][:m, :], ALU.mult, [ps, gl[i]], [ob[i]])
                k.dma("sp", self.hid_b[j * F + c0: j * F + c0 + m, tb * TBm:(tb + 1) * TBm], ob[i][:m, :], r=[ob[i]], w=[self.hid_b], acc=True)
            self.gemm([(self.xgT, 0, KT, self.wview(I["w_u"], 0, KT, r0=j * D), I["w_u"])], F, cap, epi_u, TB=TBm, t0=j * cap, tag=f"eu{j}")
            k.phase_end()
        self.ag(self.hid_b, self.hidT)
        self.ydd = k.dram("ydd", [16 * cap, DC], BF16)
        k.phase_begin()
        hT = [k.sb(f"dh{b}", [128, FT, cap], BF16) for b in range(2)]
        wd = [k.sb(f"dw{b}", [128, FT, DC], BF16) for b in range(2)]
        dps = [k.ps(f"dps{b}", [128, DC]) for b in range(2)]
        yo = [k.sb(f"dyo{b}", [128, DC], BF16) for b in range(3)]
        n = 0
        for e in range(16):
            h_ = hT[e % 2]; w_ = wd[e % 2]
            k.dma("sp", h_[:], self.hidT.t.ap()[e * F:(e + 1) * F, :].rearrange("(ft p) c -> p ft c", p=128), r=[self.hidT], w=[h_])
            k.dma("pool", w_[:], I["w_d"].t.ap()[e * F:(e + 1) * F, :].rearrange("(ft p) c -> p ft c", p=128), r=[I["w_d"]], w=[w_])
            for ct in range(cap // 128):
                ps = dps[n % 2]; y_ = yo[n % 3]; n += 1
                for ft in range(FT):
                    self.MM(ps[:], h_[:, ft, ct * 128:(ct + 1) * 128], w_[:, ft, :], ft == 0, ft == FT - 1, [h_, w_], [ps])
                self.AC(y_[:], ps[:], ACT.Copy, [ps], [y_])
                k.dma("sp", self.ydd[e * cap + ct * 128: e * cap + (ct + 1) * 128, :], y_[:], r=[y_], w=[self.ydd], acc=True)
        k.phase_end()
        self.y2T = k.dram("y2T", [DC, S])
        ssqb = k.dram("mo_ssqb", [1, S]); ssqg = k.dram("mo_ssqg", [8, S])
        k.phase_begin()
        osl = k.sb("osl", [16, 16 * 128]); k.dma("sp", osl[:], I["onesel"][:, :], r=[I["onesel"]], w=[osl])
        cidx = k.sb("cidx", [128, max(1, cap // 128)]); k.dma("sp", cidx[:], I["cidx"][:, :], r=[I["cidx"]], w=[cidx])
        y2 = [k.ps(f"y2ps{b}", [128, 512]) for b in range(DCT)]
        bp = k.ps("bp", [128, 512]); bg = k.ps("bg", [128, 512]); pss = k.ps("cpss", [128, 512])
        bps = [k.sb(f"bps{b}", [128, 512]) for b in range(2)]; bgs = [k.sb(f"bgs{b}", [128, 512]) for b in range(2)]
        Pm = [k.sb(f"Pm{b}", [128, 512], BF16) for b in range(3)]
        yl = [k.sb(f"yl{b}", [128, DC], BF16) for b in range(3)]
        o = [k.sb(f"co{b}", [128, 512]) for b in range(2)]; sq = [k.sb(f"csq{b}", [128, 512]) for b in range(2)]
        row = [k.sb(f"crow{b}", [1, 512]) for b in range(2)]
        pzl = [k.sb(f"pzl{b}", [16, 512]) for b in range(2)]; gzl = [k.sb(f"gzl{b}", [16, 512]) for b in range(2)]
        n = 0
        NCT = cap // 128
        for tb in range(S // 512):
            ts_ = slice(tb * 512, (tb + 1) * 512)
            pz = pzl[tb % 2]; gz = gzl[tb % 2]
            k.dma("sp", pz[:], self.posd[:, ts_], r=[self.posd], w=[pz])
            k.dma("sp", gz[:], self.gmd[:, ts_], r=[self.gmd], w=[gz])
            for e in range(16):
                i2 = e % 2
                self.MM(bp[:], osl[:, e * 128:(e + 1) * 128], pz[:], True, True, [osl, pz], [bp])
                self.MM(bg[:], osl[:, e * 128:(e + 1) * 128], gz[:], True, True, [osl, gz], [bg])
                self.AC(bps[i2][:], bp[:], ACT.Copy, [bp], [bps[i2]])
                self.AC(bgs[i2][:], bg[:], ACT.Copy, [bg], [bgs[i2]])
                for ct in range(NCT):
                    pm = Pm[n % 3]; y_ = yl[n % 3]; n += 1
                    self.STT(pm[:], bps[i2][:], cidx[:, ct:ct + 1], bgs[i2][:], ALU.is_equal, ALU.mult, [bps[i2], cidx, bgs[i2]], [pm])
                    k.dma("sp", y_[:], self.ydd[e * cap + ct * 128: e * cap + (ct + 1) * 128, :], r=[self.ydd], w=[y_])
                    first = (e == 0 and ct == 0); last = (e == 15 and ct == NCT - 1)
                    for q in range(DCT):
                        self.MM(y2[q][:], y_[:, q * 128:(q + 1) * 128], pm[:], first, last, [y_, pm], [y2[q]])
            for q in range(DCT):
                i = q % 2
                self.AC(o[i][:], y2[q][:], ACT.Copy, [y2[q]], [o[i]])
                self.AC(sq[i][:], y2[q][:], ACT.Square, [y2[q]], [sq[i]])
                k.dma("sp", self.y2T[q * 128:(q + 1) * 128, ts_], o[i][:], r=[o[i]], w=[self.y2T], acc=True)
                self.MM(pss[:], self.ones_f[:], sq[i][:], q == 0, q == DCT - 1, [self.ones_f, sq[i]], [pss])
            self.AC(row[tb % 2][:], pss[0:1, :], ACT.Copy, [pss], [row[tb % 2]])
            k.dma("sp", ssqb[0:1, ts_], row[tb % 2][:], r=[row[tb % 2]], w=[ssqb], acc=True)
        k.phase_end()
        self.ag(ssqb, ssqg)
        self.ssq8["mo"] = ssqg

    def build(self):
        import os
        c, k = self.cfg, self.k
        stop = int(os.environ.get("STOP_AFTER", "99"))
        self.declare_inputs()
        self.consts()
        steps = []
        def hT_():
            self.hT = k.dram("hT", [c.D, c.S], BF16)
            self.norm_phase(self.xTf, self.A_m, self.B_m, self.hT)
            self.dbg("hT", self.hT, [c.D, c.S], BF16)
        def p2_():
            self.phase2(); self.dbg("projT", self.projT, [c.NCOL, c.S])
        def p3_():
            self.phase3_prep()
            self.dbg("Qd", self.Qd, [8 * c.R, c.S]); self.dbg("xcT", self.xcT, [c.DIc, c.S])
        def s0_():
            self.ssd_sweep(0); self.dbg("yf", self.yf, [c.S, c.DIc])
        def s1_():
            self.ssd_sweep(1); self.dbg("yssd_b", self.yssd_b, [c.DIc, c.S], BF16)
        def p4_():
            self.phase4(); self.dbg("ofT", self.ofT, [c.HW, c.S]); self.dbg("obT", self.obT, [c.HW, c.S])
        def p4b_():
            self.phase4b(); self.dbg("yhg_b", self.yhg_b, [c.HW, c.S], BF16)
        def p5_():
            self.phase5(); self.dbg("x1T_b", self.x1T_b, [c.DC, c.S])
        def n2_():
            self.logits = k.dram("logits", [16, c.S])
            self.h2T = k.dram("h2T", [c.D, c.S], BF16); self.h2tok = k.dram("h2tok", [c.S, c.D], BF16)
            self.norm_phase(self.x1Tf, self.A_f, self.B_f, self.h2T, router=True)
            self.dbg("h2T", self.h2T, [c.D, c.S], BF16)
        def p7_():
            self.phase7(); self.dbg("y2T", self.y2T, [c.DC, c.S])
            self.resid_pass(self.y2T, self.x1T_b, self.Gf, self.outT, "mo")
        steps = [self.phase0, hT_, p2_, p3_, s0_, s1_, p4_, p4b_, p5_, n2_, p7_]
        for i, st in enumerate(steps):
            if i > stop:
                break
            st()
        k.finish()
        return self.nc


def _lay(v, nt):
    return np.ascontiguousarray(np.asarray(v).reshape(nt, 128).T)


def shard_inputs(cfg, inp):
    c = cfg
    D, S, R = c.D, c.S, c.R
    x = np.asarray(inp["x"])[0]
    xT = np.ascontiguousarray(x.T)
    w_ada = np.asarray(inp["w_ada"])[0]; b_ada = np.asarray(inp["b_ada"])[0]
    w_in = np.asarray(inp["w_in"])[0]
    conv_w = np.asarray(inp["conv_w"])[0]; conv_b = np.asarray(inp["conv_b"])[0]
    sizes = [c.DI, c.DI + 2 * 8 * c.N, 8 * R, 8 * R, D, D, D, D, D, D, D]
    offs = np.cumsum([0] + sizes)
    o_z, o_xbc, o_dtf, o_dtb, o_q, o_ff, o_fb, o_i, o_g, o_ga, o_gb = offs[:11]
    maps = []
    eye16 = np.eye(16, dtype=np.float32)
    onesel = np.ascontiguousarray(np.repeat(eye16[:, :, None], 128, axis=2).reshape(16, 16 * 128))
    nct = max(1, c.cap // 128)
    cidx = (np.arange(128, dtype=np.float32)[:, None] + 128.0 * np.arange(nct, dtype=np.float32)[None, :]).astype(np.float32)
    rowmask = np.zeros((128, 4), np.float32)
    for j in range(4):
        rowmask[j * 32:(j + 1) * 32, j] = 1.0
    s_ = np.arange(128)[:, None]; t_ = np.arange(128)[None, :]
    same = (s_ // 32) == (t_ // 32)
    bmask = np.concatenate([(same & (s_ <= t_)), (same & (s_ >= t_))], axis=1).astype(np.float32)
    for g in range(8):
        m = {}
        cs_ = slice(g * c.DC, (g + 1) * c.DC)
        m["xT"] = np.ascontiguousarray(xT[cs_, :])
        m["c_l"] = _lay(np.asarray(inp["c"])[0], c.KT)
        n6 = 6 * D // 8
        cols = np.concatenate([np.arange(g * n6, (g + 1) * n6), 2 * D + np.arange(g * c.DC, (g + 1) * c.DC), 5 * D + np.arange(g * c.DC, (g + 1) * c.DC)])
        m["w_ada"] = np.ascontiguousarray(w_ada[:, cols])
        m["b_ada_l"] = _lay(b_ada[cols], c.KT)
        m["npm_l"] = _lay(np.asarray(inp["norm_pre_mix"])[0], c.KT)
        m["npf_l"] = _lay(np.asarray(inp["norm_pre_ffn"])[0], c.KT)
        m["npom_l"] = _lay(np.asarray(inp["norm_post_mix"])[0][cs_], c.DCT)
        m["npof_l"] = _lay(np.asarray(inp["norm_post_ffn"])[0][cs_], c.DCT)
        xch = o_xbc + np.arange(g * c.DIc, (g + 1) * c.DIc)
        bch = o_xbc + c.DI + np.arange(g * c.N, (g + 1) * c.N)
        cch = o_xbc + c.DI + 8 * c.N + np.arange(g * c.N, (g + 1) * c.N)
        hs = np.arange(g * c.HW, (g + 1) * c.HW)
        wcols = np.concatenate([o_z + np.arange(g * c.DIc, (g + 1) * c.DIc), xch, bch, cch,
                                o_q + hs, o_ff + hs, o_fb + hs, o_i + hs, o_g + hs,
                                o_ga + np.arange(g * c.DC, (g + 1) * c.DC), o_gb + np.arange(g * c.DC, (g + 1) * c.DC),
                                o_dtf + np.arange(g * R, (g + 1) * R), o_dtb + np.arange(g * R, (g + 1) * R)])
        assert len(wcols) == c.NCOL
        m["w_in"] = np.ascontiguousarray(w_in[:, wcols])
        cch_all = np.concatenate([xch, bch, cch]) - o_xbc
        cwg = conv_w[:, cch_all]
        m["convw_l"] = np.ascontiguousarray(cwg.T.reshape(c.CT3, 128, 5).transpose(1, 0, 2))
        m["convb_l"] = _lay(conv_b[cch_all], c.CT3)
        hr = slice(g * R, (g + 1) * R)
        dtb = [np.asarray(inp["dt_bias_fwd"])[0][hr], np.asarray(inp["dt_bias_bwd"])[0][hr]]
        alg = [np.asarray(inp["a_log_fwd"])[0][hr], np.asarray(inp["a_log_bwd"])[0][hr]]
        par8 = np.zeros((8 * R, 8), np.float32)
        for blk in range(4):
            for d in range(2):
                rows = slice(blk * 2 * R + d * R, blk * 2 * R + (d + 1) * R)
                par8[rows, 0] = dtb[d]; par8[rows, 1] = alg[d]
                par8[rows, 2 + blk] = 1.0
                par8[rows, 6 + d] = 1.0
        m["par8"] = par8
        m["par2"] = np.zeros((2 * R, 2 + 2 * R), np.float32); m["par2b"] = np.zeros((2 * R, 2), np.float32)
        m["dskip_bc"] = np.ascontiguousarray(np.broadcast_to(np.repeat(np.asarray(inp["d_skip"])[0][hr], 64)[None, :], (128, c.DIc))).astype(np.float32)
        m["ssdw_bc"] = np.ascontiguousarray(np.broadcast_to(np.asarray(inp["ssd_norm_w"])[0][g * c.DIc:(g + 1) * c.DIc][None, :], (128, c.DIc))).astype(np.float32)
        lbt = np.asarray(inp["hg_lower_bound"])[:, g * c.HW:(g + 1) * c.HW]
        m["lb_l"] = np.ascontiguousarray(lbt.reshape(2, c.HHc, 128).transpose(2, 1, 0))
        m["hgw_l"] = _lay(np.asarray(inp["hg_norm_w"])[0][g * c.HW:(g + 1) * c.HW], c.HHc)
        m["w_so"] = np.ascontiguousarray(np.asarray(inp["w_ssd_out"])[0][:, cs_])
        m["w_ho"] = np.ascontiguousarray(np.asarray(inp["w_hg_out"])[0][:, cs_])
        m["w_mo"] = np.ascontiguousarray(np.asarray(inp["w_mix_out"])[0][:, cs_])
        wr = np.asarray(inp["w_router"])[0]
        m["w_r_l"] = np.ascontiguousarray(wr.reshape(c.KT, 128, 16).transpose(1, 0, 2))
        m["w_g"] = np.ascontiguousarray(np.asarray(inp["w_gate"])[0][2 * g:2 * g + 2].reshape(2 * D, c.F))
        m["w_u"] = np.ascontiguousarray(np.asarray(inp["w_up"])[0][2 * g:2 * g + 2].reshape(2 * D, c.F))
        m["w_d"] = np.ascontiguousarray(np.asarray(inp["w_down"])[0][:, :, cs_].reshape(16 * c.F, c.DC))
        es = np.zeros((16, 2), np.float32); es[2 * g, 0] = 1.0; es[2 * g + 1, 1] = 1.0
        m["esel"] = es; m["onesel"] = onesel; m["cidx"] = cidx; m["rowmask"] = rowmask; m["bmask"] = bmask
        maps.append({k_: np.ascontiguousarray(v, dtype=np.float32) for k_, v in m.items()})
    return maps


_CACHE = {}


def run(cfg, inputs, debug=()):
    from concourse.bass_utils import run_bass_kernel_spmd
    key = (cfg.D, cfg.S, tuple(debug))
    if key not in _CACHE:
        p = Prog(cfg, debug)
        _CACHE[key] = p.build()
    nc = _CACHE[key]
    maps = shard_inputs(cfg, inputs)
    res = run_bass_kernel_spmd(nc, maps, core_ids=list(range(8)))
    out = np.empty((1, cfg.S, cfg.D), np.float32)
    for g in range(8):
        out[0][:, g * cfg.DC:(g + 1) * cfg.DC] = res.results[g]["outT"].T
    return out, res


def kernel(**inputs):
    out, _ = run(Cfg(4096, 8192), inputs)
    return out
```

```python
import numpy as np
from contextlib import ExitStack
import concourse.bass as bass
import concourse.mybir as mybir

F32 = mybir.dt.float32
BF16 = mybir.dt.bfloat16
I32 = mybir.dt.int32
ACT = mybir.ActivationFunctionType
ALU = mybir.AluOpType
AX = mybir.AxisListType

NQ = 16
STORE_Q = "act"


class Buf:
    __slots__ = ("t", "w_dma", "w_cmp", "r_dma", "r_cmp", "name")

    def __init__(self, t, name):
        self.t = t
        self.name = name
        self.w_dma = {}
        self.w_cmp = {}
        self.r_dma = {}
        self.r_cmp = {}

    def __getitem__(self, k):
        return self.t[k]


class Op:
    __slots__ = ("eng", "fn", "deps_c", "deps_d", "kind", "needed", "sem", "val", "dkey", "dval")


class K:
    def __init__(self, nc):
        self.nc = nc
        self.ops = []
        self.es = ExitStack()
        self.dma_cnt = {"sp": 0, "pool": 0, "act": 0}
        self.ncoll = 0
        self.uid = 0
        self.pes = None
        self.emitted = 0
        self.bar_idx = -1
        self.inited = False
        self.nwait = 0

    def sb(self, name, shape, dtype=F32):
        self.uid += 1
        es = self.pes if self.pes is not None else self.es
        t = es.enter_context(self.nc.sbuf_tensor(f"{name}_{self.uid}", list(shape), dtype))
        return Buf(t, name)

    def ps(self, name, shape, dtype=F32):
        self.uid += 1
        es = self.pes if self.pes is not None else self.es
        t = es.enter_context(self.nc.psum_tensor(f"{name}_{self.uid}", list(shape), dtype))
        return Buf(t, name)

    def dram(self, name, shape, dtype=F32):
        t = self.nc.dram_tensor(name, list(shape), dtype)
        return Buf(t, name)

    def ext(self, name, shape, dtype, kind):
        t = self.nc.dram_tensor(name, list(shape), dtype, kind=kind)
        return Buf(t, name)

    def _record(self, eng, fn, r, w, kind, acc):
        op = Op()
        op.eng = eng
        op.fn = fn
        op.kind = kind
        op.needed = False
        op.sem = None
        op.val = None
        dc = {}
        dd = {}

        def add_c(m):
            for e, i in m.items():
                if dc.get(e, -1) < i:
                    dc[e] = i

        def add_d(m):
            for k, v in m.items():
                if dd.get(k, 0) < v:
                    dd[k] = v

        for b in r:
            add_c(b.w_cmp)
            add_d(b.w_dma)
        for b in w:
            if not acc:
                add_c(b.w_cmp)
                add_d(b.w_dma)
                add_c(b.r_cmp)
                add_d(b.r_dma)
            else:
                add_c({e: i for e, i in b.w_cmp.items() if e != eng})
                add_d({kk: v for kk, v in b.w_dma.items() if kk[0] != eng})
                add_c({e: i for e, i in b.r_cmp.items() if e != eng})
                add_d({kk: v for kk, v in b.r_dma.items() if kk[0] != eng})
        idx = len(self.ops)
        op.dkey = None
        if kind == "dma":
            i = self.dma_cnt[eng]
            self.dma_cnt[eng] = i + 1
            op.dkey = (eng, i % NQ)
            op.dval = 16 * (i // NQ + 1)
        elif kind == "coll":
            op.dkey = ("coll", self.ncoll % NQ)
            op.dval = self.ncoll // NQ + 1
            self.ncoll += 1
        for b in r:
            if op.dkey is not None:
                if b.r_dma.get(op.dkey, 0) < op.dval:
                    b.r_dma[op.dkey] = op.dval
            else:
                b.r_cmp[eng] = idx
        for b in w:
            if not acc:
                b.w_cmp = {}
                b.w_dma = {}
                b.r_cmp = {}
                b.r_dma = {}
            if op.dkey is not None:
                if b.w_dma.get(op.dkey, 0) < op.dval:
                    b.w_dma[op.dkey] = op.dval
            else:
                b.w_cmp[eng] = idx
        op.deps_c = dc
        op.deps_d = dd
        self.ops.append(op)
        return op

    def op(self, eng, fn, r=(), w=(), acc=False):
        return self._record(eng, fn, r, w, "cmp", acc)

    def dma(self, q, out, in_, r=(), w=(), acc=False, **kw):
        nc = self.nc
        if q == "sp" and type(out.tensor).__name__.startswith("DRam") and type(in_.tensor).__name__.startswith("SB"):
            q = STORE_Q
        e = {"sp": nc.sync, "pool": nc.gpsimd, "act": nc.scalar}[q]
        return self._record(q, lambda: e.dma_start(out=out, in_=in_, **kw), r, w, "dma", acc)

    def allgather(self, src, dst):
        nc = self.nc
        import os
        if os.environ.get("NO_COLL"):
            rows = src.t.shape[0]
            for r0 in range(0, rows, 128):
                r1 = min(rows, r0 + 128)
                self.dma("sp", dst[r0:r1, :], src[r0:r1, :], r=[src], w=[dst], acc=True)
            return
        rows = src.t.shape[0]; cols = src.t.shape[1]
        isz = 2 if src.t.dtype == BF16 else 4
        cr = max(1, min(rows, (512 * 1024) // (cols * isz)))
        while rows % cr:
            cr -= 1
        self.uid += 1
        u = self.uid
        NR = 3
        ring = [(self.dram(f"agi{u}_{b}", [cr, cols], src.t.dtype), self.dram(f"ag2{u}_{b}", [2 * cr, cols], src.t.dtype),
                 self.dram(f"ag8{u}_{b}", [8 * cr, cols], src.t.dtype)) for b in range(NR)]
        g4 = [[0, 1, 2, 3], [4, 5, 6, 7]]
        g2 = [[0, 4], [1, 5], [2, 6], [3, 7]]
        def coll(groups, a, b_):
            return self._record(
                "pool",
                lambda: nc.gpsimd.collective_compute(
                    "AllGather", ALU.bypass, replica_groups=groups,
                    ins=[a.t.ap().opt()], outs=[b_.t.ap().opt()]),
                [a], [b_], "coll", False)
        starts = list(range(0, rows, cr))
        n = len(starts)
        for i in range(n + 1):
            if i < n:
                cin, c2, c8 = ring[i % NR]
                self.dma("pool", cin[:, :], src[starts[i]:starts[i] + cr, :], r=[src], w=[cin])
                coll(g2, cin, c2)
            if i >= 1:
                cin, c2, c8 = ring[(i - 1) % NR]
                r0 = starts[i - 1]
                coll(g4, c2, c8)
                for j in range(8):
                    core = (j // 2) + 4 * (j % 2)
                    self.dma("pool", dst[core * rows + r0: core * rows + r0 + cr, :], c8[j * cr:(j + 1) * cr, :], r=[c8], w=[dst], acc=True)

    def phase_begin(self):
        assert self.pes is None
        self.pes = ExitStack()

    def phase_end(self):
        self.barrier()
        self.flush()
        self.pes.close()
        self.pes = None

    def barrier(self):
        marks = {}
        for e in ("act", "dve", "pool"):
            op = Op()
            op.eng = e; op.fn = None; op.kind = "mark"; op.needed = True; op.sem = None; op.val = None
            op.deps_c = {}; op.deps_d = {}; op.dkey = None
            marks[e] = len(self.ops)
            self.ops.append(op)
        dd = {}
        for q, n in self.dma_cnt.items():
            for s_ in range(NQ):
                cnt = (n - s_ + NQ - 1) // NQ if n > s_ else 0
                if cnt > 0:
                    dd[(q, s_)] = 16 * cnt
        for s_ in range(NQ):
            cnt = (self.ncoll - s_ + NQ - 1) // NQ if self.ncoll > s_ else 0
            if cnt > 0:
                dd[("coll", s_)] = cnt
        for e in ("pe", "act", "dve", "pool", "sp"):
            op = Op()
            op.eng = e; op.fn = None; op.kind = "barwait"; op.needed = False; op.sem = None; op.val = None
            op.deps_c = dict(marks); op.deps_d = dict(dd); op.dkey = None
            self.ops.append(op)
        self.bar_idx = len(self.ops)

    def _init_emit(self):
        nc = self.nc
        self.engs = {"pe": nc.tensor, "act": nc.scalar, "dve": nc.vector, "pool": nc.gpsimd, "sp": nc.sync}
        self.csem = {e: self.es.enter_context(nc.semaphore(f"c_{e}")) for e in ("pe", "act", "dve", "pool")}
        self.ccnt = {e: 0 for e in self.csem}
        self.dsem = {}
        for q in ("sp", "pool", "act"):
            for s_ in range(NQ):
                self.dsem[(q, s_)] = self.es.enter_context(nc.semaphore(f"d_{q}{s_}"))
        for s_ in range(NQ):
            self.dsem[("coll", s_)] = self.es.enter_context(nc.semaphore(f"cc{s_}"))
        self.seen = {e: {} for e in self.engs}
        self.marktile = {e: self.es.enter_context(nc.sbuf_tensor(f"mark_{e}", [1, 8], F32)) for e in ("act", "dve", "pool")}
        self.markps = self.es.enter_context(nc.sbuf_tensor("mark_pe_in", [1, 8], BF16))
        nc.vector.memset(self.marktile["act"][:], 0.0)
        self.inited = True

    def flush(self):
        nc = self.nc
        if not self.inited:
            self._init_emit()
        ops = self.ops
        start = self.emitted
        for o in ops[start:]:
            for e, i in o.deps_c.items():
                ops[i].needed = True
        engs, csem, dsem, ccnt = self.engs, self.csem, self.dsem, self.ccnt
        for idx in range(start, len(ops)):
            o = ops[idx]
            e = o.eng
            h = engs[e]
            sn = self.seen[e]
            for de, di in o.deps_c.items():
                d = ops[di]
                if de == "pe" and e == "pe":
                    continue
                if d.val is None:
                    continue
                key = ("c", de)
                if sn.get(key, 0) >= d.val:
                    continue
                h.wait_ge(csem[de], d.val)
                self.nwait += 1
                sn[key] = d.val
            for dk, dv in o.deps_d.items():
                if sn.get(dk, 0) >= dv:
                    continue
                if dk not in dsem:
                    dsem[dk] = self.es.enter_context(nc.semaphore(f"cc{dk[1]}"))
                h.wait_ge(dsem[dk], dv)
                self.nwait += 1
                sn[dk] = dv
            if o.kind == "dma":
                prev = o.dval - 16
                if prev > 0 and sn.get(o.dkey, 0) < prev:
                    h.wait_ge(dsem[o.dkey], prev)
                    self.nwait += 1
                    sn[o.dkey] = prev
                o.fn().then_inc(dsem[o.dkey], 16)
            elif o.kind == "coll":
                prev = o.dval - 1
                if prev > 0 and sn.get(o.dkey, 0) < prev:
                    h.wait_ge(dsem[o.dkey], prev)
                    self.nwait += 1
                    sn[o.dkey] = prev
                o.fn().then_inc(dsem[o.dkey])
            elif o.kind == "barwait":
                pass
            else:
                if o.kind == "mark":
                    if e == "pe":
                        ins = nc.tensor.nop()
                    elif e == "act":
                        ins = nc.scalar.copy(out=self.marktile["act"][:, 0:4], in_=self.marktile["act"][:, 4:8])
                    elif e == "dve":
                        ins = nc.vector.memset(self.marktile["dve"][:], 0.0)
                    else:
                        ins = nc.gpsimd.memset(self.marktile["pool"][:], 0.0)
                else:
                    ins = o.fn()
                if o.needed:
                    ccnt[e] += 1
                    ins.then_inc(csem[e], 1)
                    o.val = ccnt[e]
            o.fn = None
        self.emitted = len(ops)

    def finish(self):
        assert self.pes is None
        self.barrier()
        self.flush()
        self.stats = dict(nops=len(self.ops), nwait=self.nwait)
        self.es.close()


class Cfg:
    def __init__(self, D=4096, S=8192):
        self.D = D; self.S = S
        self.KT = D // 128
        self.DC = D // 8; self.DCT = self.DC // 128
        self.DI = 2 * D; self.DIc = self.DI // 8; self.XT = self.DIc // 128
        self.P = 64; self.N = 128; self.R = self.DIc // 64
        self.HHc = D // 128 // 8; self.HW = self.HHc * 128
        self.E = 16; self.cap = 2 * S // 16; self.F = D // 2; self.FT = self.F // 128
        self.NT = S // 128
        c = self
        o = 0
        c.oz = o; o += c.DIc
        c.ox = o; o += c.DIc
        c.oB = o; o += c.N
        c.oC = o; o += c.N
        c.oq = o; o += c.HW
        c.off = o; o += c.HW
        c.ofb = o; o += c.HW
        c.oi = o; o += c.HW
        c.og = o; o += c.HW
        c.oga = o; o += c.DC
        c.ogb = o; o += c.DC
        c.odt = o; o += 2 * c.R
        c.NCOL = o
        c.CT3 = (c.DIc + 2 * c.N) // 128


def _eng(nc, e):
    return {"dve": nc.vector, "pool": nc.gpsimd}[e]


class Prog:
    def __init__(self, cfg, debug=()):
        self.cfg = cfg
        self.debug = set(debug)
        self.nc = bass.Bass("TRN2", target_bir_lowering=False)
        self.k = K(self.nc)
        self.k._init_emit()

    def TT(self, e, out, in0, in1, op, r, w, acc=False):
        en = _eng(self.nc, e)
        return self.k.op(e, lambda: en.tensor_tensor(out=out, in0=in0, in1=in1, op=op), r, w, acc)

    def TS(self, e, out, in0, s1, s2, op0, op1=None, r=(), w=(), acc=False, accum_out=None):
        en = _eng(self.nc, e)
        if op1 is None:
            return self.k.op(e, lambda: en.tensor_scalar(out=out, in0=in0, scalar1=s1, scalar2=None, op0=op0), r, w, acc)
        if accum_out is not None:
            return self.k.op(e, lambda: en.tensor_scalar(out=out, in0=in0, scalar1=s1, scalar2=s2, op0=op0, op1=op1, accum_out=accum_out), r, w, acc)
        return self.k.op(e, lambda: en.tensor_scalar(out=out, in0=in0, scalar1=s1, scalar2=s2, op0=op0, op1=op1), r, w, acc)

    def STT(self, out, in0, scalar, in1, op0, op1, r, w, acc=False):
        nc = self.nc
        return self.k.op("dve", lambda: nc.vector.scalar_tensor_tensor(out=out, in0=in0, scalar=scalar, in1=in1, op0=op0, op1=op1), r, w, acc)

    def AC(self, out, in_, func, r, w, bias=None, scale=None, acc=False, accum_out=None):
        nc = self.nc
        kw = {}
        if bias is not None:
            kw["bias"] = bias
        if scale is not None:
            kw["scale"] = scale
        if accum_out is not None:
            kw["accum_out"] = accum_out
        return self.k.op("act", lambda: nc.scalar.activation(out=out, in_=in_, func=func, **kw), r, w, acc)

    def MM(self, ps, lhsT, rhs, start, stop, r, w, acc=None):
        nc = self.nc
        if acc is None:
            acc = not start
        return self.k.op("pe", lambda: nc.tensor.matmul(ps, lhsT=lhsT, rhs=rhs, start=start, stop=stop), r, w, acc)

    def TR(self, ps, in_, ident, r, w, acc=False):
        nc = self.nc
        return self.k.op("pe", lambda: nc.tensor.transpose(ps, in_, ident), r, w, acc)

    def MEMSET(self, e, ap, val, w, acc=False):
        en = _eng(self.nc, e)
        return self.k.op(e, lambda: en.memset(ap, val), (), w, acc)

    def dbg(self, name, src_buf, shape, dtype=F32):
        if name not in self.debug:
            return
        o = self.k.ext("dbg_" + name, shape, dtype, "ExternalOutput")
        rows = shape[0]
        for r0 in range(0, rows, 128):
            r1 = min(rows, r0 + 128)
            self.k.dma("sp", o[r0:r1, :], src_buf[r0:r1, :], r=[src_buf], w=[o], acc=True)

    def consts(self):
        k, nc = self.k, self.nc
        self.ones_f = k.sb("ones_f", [128, 128]); self.ident_f = k.sb("ident_f", [128, 128])
        self.ones_b = k.sb("ones_b", [128, 128], BF16); self.ident_b = k.sb("ident_b", [128, 128], BF16)
        self.eps = k.sb("eps", [128, 1]); self.onec = k.sb("onec", [128, 1])
        self.maskF = k.sb("maskF", [128, 128]); self.maskB = k.sb("maskB", [128, 128])
        self.MEMSET("pool", self.ones_f[:], 1.0, [self.ones_f])
        self.MEMSET("pool", self.ones_b[:], 1.0, [self.ones_b])
        self.MEMSET("pool", self.eps[:], 1e-6, [self.eps])
        self.MEMSET("pool", self.onec[:], 1.0, [self.onec])
        of, idf, mF, mB = self.ones_f, self.ident_f, self.maskF, self.maskB
        k.op("pool", lambda: nc.gpsimd.affine_select(out=idf[:], in_=of[:], pattern=[[-1, 128]], compare_op=ALU.is_equal, fill=0.0, base=0, channel_multiplier=1), [of], [idf])
        k.op("pool", lambda: nc.gpsimd.affine_select(out=mF[:], in_=of[:], pattern=[[1, 128]], compare_op=ALU.is_ge, fill=0.0, base=0, channel_multiplier=-1), [of], [mF])
        k.op("pool", lambda: nc.gpsimd.affine_select(out=mB[:], in_=of[:], pattern=[[-1, 128]], compare_op=ALU.is_ge, fill=0.0, base=0, channel_multiplier=1), [of], [mB])
        ib = self.ident_b
        k.op("pool", lambda: nc.gpsimd.tensor_copy(out=ib[:], in_=idf[:]), [idf], [ib])

    def gemm(self, segs, C, T, epi, TB=512, t0=0, tag="g", CG=512):
        k = self.k
        nseg = len(segs)
        wr = [[k.sb(f"{tag}w{si}_{b}", [128, segs[si][2], CG], BF16) for b in range(2)] for si in range(nseg)]
        ar = [[k.sb(f"{tag}a{si}_{b}", [128, segs[si][2], TB], BF16) for b in range(2)] for si in range(nseg)]
        psr = [k.ps(f"{tag}ps{b}", [128, TB]) for b in range(3)]
        total = sum(s[2] for s in segs)
        na = 0; npz = 0
        for cg in range((C + CG - 1) // CG):
            c0 = cg * CG; cw = min(CG, C - c0)
            wbs = []
            for si, (act, kt0, nkt, wfn, wbuf) in enumerate(segs):
                wb = wr[si][cg % 2]
                k.dma("pool", wb[:, :, :cw], wfn(c0, cw), r=[wbuf], w=[wb])
                wbs.append(wb)
            for tb in range(T // TB):
                abs_ = []
                for si, (act, kt0, nkt, wfn, wbuf) in enumerate(segs):
                    ab = ar[si][na % 2]
                    src = act.t.ap().rearrange("(kt p) s -> p kt s", p=128)[:, kt0:kt0 + nkt, t0 + tb * TB: t0 + (tb + 1) * TB]
                    k.dma("sp", ab[:], src, r=[act], w=[ab])
                    abs_.append(ab)
                na += 1
                for ct in range((cw + 127) // 128):
                    m = min(128, cw - ct * 128)
                    ps = psr[npz % 3]; npz += 1
                    i = 0
                    for si, (act, kt0, nkt, wfn, wbuf) in enumerate(segs):
                        for kt in range(nkt):
                            self.MM(ps[:m, :], wbs[si][:, kt, ct * 128: ct * 128 + m], abs_[si][:, kt, :],
                                    i == 0, i == total - 1, [wbs[si], abs_[si]], [ps])
                            i += 1
                    epi(c0 + ct * 128, m, tb, ps)

    def ag(self, src, dst):
        self.k.allgather(src, dst)

    def declare_inputs(self):
        c, k = self.cfg, self.k
        I = {}
        def inp(name, shape, dt=F32):
            I[name] = k.ext(name, shape, dt, "ExternalInput")
        inp("xT", [c.DC, c.S]); inp("c_l", [128, c.KT]); inp("w_ada", [c.D, c.D]); inp("b_ada_l", [128, c.KT])
        inp("npm_l", [128, c.KT]); inp("npf_l", [128, c.KT]); inp("npom_l", [128, c.DCT]); inp("npof_l", [128, c.DCT])
        inp("w_in", [c.D, c.NCOL]); inp("convw_l", [128, c.CT3, 5]); inp("convb_l", [128, c.CT3])
        inp("par8", [8 * c.R, 8]); inp("par2", [2 * c.R, 2 + 2 * c.R]); inp("par2b", [2 * c.R, 2])
        inp("dskip_bc", [128, c.DIc]); inp("ssdw_bc", [128, c.DIc])
        inp("lb_l", [128, c.HHc, 2]); inp("hgw_l", [128, c.HHc])
        inp("w_so", [c.DI, c.DC]); inp("w_ho", [c.D, c.DC]); inp("w_mo", [c.D, c.DC])
        inp("w_r_l", [128, c.KT, 16]); inp("w_g", [2 * c.D, c.F]); inp("w_u", [2 * c.D, c.F]); inp("w_d", [16 * c.F, c.DC])
        inp("esel", [16, 2]); inp("onesel", [16, 16 * 128]); inp("cidx", [128, max(1, c.cap // 128)])
        inp("rowmask", [128, 4]); inp("bmask", [128, 256])
        self.I = I
        self.outT = k.ext("outT", [c.DC, c.S], F32, "ExternalOutput")

    def wview(self, wbuf, kt0, nkt, r0=0):
        def fn(c0, cw):
            return wbuf.t.ap()[r0:, :].rearrange("(kt p) c -> p kt c", p=128)[:, kt0:kt0 + nkt, c0:c0 + cw]
        return fn

    def phase0(self):
        c, k, nc, I = self.cfg, self.k, self.nc, self.I
        KT = c.KT
        self.xTb = k.dram("xTb", [c.DC, c.S]); self.xTf = k.dram("xTf", [c.D, c.S])
        for r0 in range(0, c.DC, 128):
            k.dma("sp", self.xTb[r0:r0 + 128, :], I["xT"][r0:r0 + 128, :], r=[I["xT"]], w=[self.xTb], acc=True)
        self.ag(self.xTb, self.xTf)
        self.A_m = k.sb("A_m", [128, KT]); self.B_m = k.sb("B_m", [128, KT])
        self.A_f = k.sb("A_f", [128, KT]); self.B_f = k.sb("B_f", [128, KT])
        self.Gm = k.sb("Gm", [128, c.DCT]); self.Gf = k.sb("Gf", [128, c.DCT])
        k.phase_begin()
        nj = 6 * KT // 8
        cl = k.sb("cl", [128, KT]); cact = k.sb("cact", [128, KT]); bl = k.sb("bl", [128, KT]); modl = k.sb("modl", [128, KT])
        k.dma("sp", cl[:], I["c_l"][:, :], r=[I["c_l"]], w=[cl])
        k.dma("sp", bl[:], I["b_ada_l"][:, :], r=[I["b_ada_l"]], w=[bl])
        self.AC(cact[:], cl[:], ACT.Silu, [cl], [cact])
        CGW = 512 if c.D >= 512 else c.D
        war = [k.sb(f"wa{b}", [128, KT, CGW]) for b in range(2)]
        psmod = k.ps("psmod", [128, KT])
        wv = I["w_ada"].t.ap().rearrange("(kt p) c -> p kt c", p=128)
        first = True
        for cg in range(c.D // CGW):
            wb = war[cg % 2]
            k.dma("sp", wb[:], wv[:, :, cg * CGW:(cg + 1) * CGW], r=[I["w_ada"]], w=[wb])
            for j in range(CGW // 128):
                col = cg * (CGW // 128) + j
                for kt in range(KT):
                    self.MM(psmod[:, col:col + 1], wb[:, kt, j * 128:(j + 1) * 128], cact[:, kt:kt + 1],
                            kt == 0, kt == KT - 1, [wb, cact], [psmod], acc=not first)
                    first = False
        self.TT("dve", modl[:], psmod[:], bl[:], ALU.add, [psmod, bl], [modl])
        modb = k.dram("modb", [nj, 128]); modg = k.dram("modg", [8 * nj, 128])
        pst = k.ps("pst", [128, 128])
        tsb = k.sb("tsb", [128, 128])
        self.TR(pst[:nj, :], modl[:, 0:nj], self.ident_f[:], [modl, self.ident_f], [pst])
        self.AC(tsb[:nj, :], pst[:nj, :], ACT.Copy, [pst], [tsb])
        k.dma("sp", modb[:, :], tsb[:nj, :], r=[tsb], w=[modb])
        self.ag(modb, modg)
        mod = k.sb("mod", [128, 6 * KT])
        half = 3 * KT
        for hh in range(2):
            gsb = k.sb(f"gsb{hh}", [128, 128])
            k.dma("sp", gsb[:half, :], modg[hh * half:(hh + 1) * half, :], r=[modg], w=[gsb])
            pst2 = k.ps(f"pst2{hh}", [128, 128])
            self.TR(pst2[:, :half], gsb[:half, :], self.ident_f[:half, :half], [gsb, self.ident_f], [pst2])
            self.AC(mod[:, hh * half:(hh + 1) * half], pst2[:, :half], ACT.Copy, [pst2], [mod], acc=(hh == 1))
        nm = k.sb("nm", [128, 2 * KT + 2 * c.DCT])
        k.dma("sp", nm[:, 0:KT], I["npm_l"][:, :], r=[I["npm_l"]], w=[nm])
        k.dma("sp", nm[:, KT:2 * KT], I["npf_l"][:, :], r=[I["npf_l"]], w=[nm], acc=True)
        k.dma("sp", nm[:, 2 * KT:2 * KT + c.DCT], I["npom_l"][:, :], r=[I["npom_l"]], w=[nm], acc=True)
        k.dma("sp", nm[:, 2 * KT + c.DCT:], I["npof_l"][:, :], r=[I["npof_l"]], w=[nm], acc=True)
        self.STT(self.A_m[:], mod[:, KT:2 * KT], 1.0, nm[:, 0:KT], ALU.add, ALU.mult, [mod, nm], [self.A_m])
        self.k.op("dve", (lambda a=self.B_m, m=mod: nc.vector.tensor_copy(out=a[:], in_=m[:, 0:KT])), [mod], [self.B_m])
        self.STT(self.A_f[:], mod[:, 4 * KT:5 * KT], 1.0, nm[:, KT:2 * KT], ALU.add, ALU.mult, [mod, nm], [self.A_f])
        self.k.op("dve", (lambda a=self.B_f, m=mod: nc.vector.tensor_copy(out=a[:], in_=m[:, 3 * KT:4 * KT])), [mod], [self.B_f])
        self.TT("dve", self.Gm[:], modl[:, nj:nj + c.DCT], nm[:, 2 * KT:2 * KT + c.DCT], ALU.mult, [modl, nm], [self.Gm])
        self.TT("dve", self.Gf[:], modl[:, nj + c.DCT:nj + 2 * c.DCT], nm[:, 2 * KT + c.DCT:], ALU.mult, [modl, nm], [self.Gf])
        k.phase_end()

    def norm_phase(self, src, A, B, dst, router=False):
        c, k, nc, I = self.cfg, self.k, self.nc, self.I
        KT = c.KT; TBn = 256
        k.phase_begin()
        xr = [k.sb(f"nx{b}", [128, KT, TBn]) for b in range(2)]
        sq = [k.sb(f"nsq{b}", [128, KT, TBn]) for b in range(1)]
        hb = [k.sb(f"nhb{b}", [128, KT, TBn], BF16) for b in range(2)]
        rs = [k.sb(f"nrs{b}", [128, TBn]) for b in range(2)]
        pss = [k.ps(f"nps{b}", [128, TBn]) for b in range(2)]
        sv = src.t.ap().rearrange("(kt p) s -> p kt s", p=128)
        dv = dst.t.ap().rearrange("(kt p) s -> p kt s", p=128)
        if router:
            wr = k.sb("wr", [128, KT, 16])
            k.dma("sp", wr[:], I["w_r_l"][:, :, :], r=[I["w_r_l"]], w=[wr])
            psl = [k.ps(f"psl{b}", [16, TBn]) for b in range(2)]
            pstr = [k.ps(f"pstr{b}", [128, 512]) for b in range(2)]
            htk = [k.sb(f"htk{b}", [128, c.D], BF16) for b in range(2)]
            lgt = [k.sb(f"lgt{b}", [16, TBn]) for b in range(2)]
        A3 = A[:, :].unsqueeze(2).broadcast_to([128, KT, TBn])
        B3 = B[:, :].unsqueeze(2).broadcast_to([128, KT, TBn])
        for tb in range(c.S // TBn):
            x = xr[tb % 2]; s = sq[0]; h = hb[tb % 2]; r_ = rs[tb % 2]; ps = pss[tb % 2]
            k.dma("sp", x[:], sv[:, :, tb * TBn:(tb + 1) * TBn], r=[src], w=[x])
            self.AC(s[:].rearrange("p a b -> p (a b)"), x[:].rearrange("p a b -> p (a b)"), ACT.Square, [x], [s])
            for kt in range(KT):
                self.MM(ps[:], self.ones_f[:], s[:, kt, :], kt == 0, kt == KT - 1, [self.ones_f, s], [ps])
            self.AC(r_[:], ps[:], ACT.Sqrt, [ps, self.eps], [r_], bias=self.eps[:], scale=1.0 / c.D)
            nc_ = nc
            k.op("dve", (lambda r_=r_: nc_.vector.reciprocal(out=r_[:], in_=r_[:])), [r_], [r_])
            self.TT("dve", s[:], x[:], r_[:].unsqueeze(1).broadcast_to([128, KT, TBn]), ALU.mult, [x, r_], [s])
            self.TT("pool", s[:], s[:], A3, ALU.mult, [s, A], [s])
            if not router:
                self.TT("dve", h[:], s[:], B3, ALU.add, [s, B], [h])
            else:
                self.TT("dve", s[:], s[:], B3, ALU.add, [s, B], [s])
                self.AC(h[:].rearrange("p a b -> p (a b)"), s[:].rearrange("p a b -> p (a b)"), ACT.Copy, [s], [h])
                pl = psl[tb % 2]
                for kt in range(KT):
                    self.MM(pl[:], wr[:, kt, :], s[:, kt, :], kt == 0, kt == KT - 1, [wr, s], [pl])
                lt = lgt[tb % 2]
                self.TS("dve", lt[:], pl[:], 1.0, None, ALU.mult, None, [pl], [lt])
                k.dma("sp", self.logits[:, tb * TBn:(tb + 1) * TBn], lt[:], r=[lt], w=[self.logits], acc=True)
                for th in range(TBn // 128):
                    ht = htk[(tb * 2 + th) % 2]
                    for g4 in range(KT // 4):
                        pt = pstr[g4 % 2]
                        for q in range(4):
                            kt = g4 * 4 + q
                            self.MM(pt[:, q * 128:(q + 1) * 128], h[:, kt, th * 128:(th + 1) * 128], self.ident_b[:], True, True, [h, self.ident_b], [pt], acc=(q > 0))
                        if g4 % 2 == 0:
                            self.AC(ht[:, g4 * 512:(g4 + 1) * 512], pt[:], ACT.Copy, [pt], [ht], acc=(g4 > 0))
                        else:
                            k.op("pool" if False else "dve", (lambda ht=ht, pt=pt, g4=g4: nc_.vector.tensor_copy(out=ht[:, g4 * 512:(g4 + 1) * 512], in_=pt[:])), [pt], [ht], acc=True)
                    t_ = tb * (TBn // 128) + th
                    k.dma("sp", self.h2tok[t_ * 128:(t_ + 1) * 128, :], ht[:], r=[ht], w=[self.h2tok], acc=True)
            k.dma("sp", dv[:, :, tb * TBn:(tb + 1) * TBn], h[:], r=[h], w=[dst], acc=True)
        k.phase_end()

    def phase2(self):
        c, k, nc, I = self.cfg, self.k, self.nc, self.I
        self.projT = k.dram("projT", [c.NCOL, c.S])
        k.phase_begin()
        ot = [k.sb(f"po{b}", [128, 512]) for b in range(3)]
        cnt = [0]
        def func_for(c0):
            if c0 < c.ox: return ACT.Silu
            if c.og <= c0 < c.oga: return ACT.Silu
            if c.oga <= c0 < c.odt: return ACT.Sigmoid
            return ACT.Copy
        def epi(c0, m, tb, ps):
            o = ot[cnt[0] % 3]; cnt[0] += 1
            self.AC(o[:m, :], ps[:m, :], func_for(c0), [ps], [o])
            k.dma("sp", self.projT[c0:c0 + m, tb * 512:(tb + 1) * 512], o[:m, :], r=[o], w=[self.projT], acc=True)
        self.gemm([(self.hT, 0, c.KT, self.wview(I["w_in"], 0, c.KT), I["w_in"])], c.NCOL, c.S, epi, TB=512, tag="ip", CG=1024)
        k.phase_end()

    def phase3_prep(self):
        c, k, nc, I = self.cfg, self.k, self.nc, self.I
        R = c.R; PR = 8 * R; S = c.S; NC = c.NT
        self.Qd = k.dram("Qd", [PR, S]); self.etd = k.dram("etd", [PR, NC])
        self.xcT = k.dram("xcT", [c.DIc, S]); self.BCb = k.dram("BCb", [2 * c.N, S], BF16)
        k.phase_begin()
        par = k.sb("par", [PR, 8]); k.dma("sp", par[:], I["par8"][:, :], r=[I["par8"]], w=[par])
        b1 = k.sb("b1", [PR, S]); b2 = k.sb("b2", [PR, S]); b3 = k.sb("b3", [PR, S]); b4 = k.sb("b4", [PR, S]); b5 = k.sb("b5", [PR, S])
        rm = k.sb("rm128", [PR, S], BF16)
        aexp = k.sb("aexp", [PR, 1]); totc = k.sb("totc", [PR, NC]); etot = k.sb("etot", [PR, NC])
        for blk in range(4):
            k.dma("sp", b1[blk * 2 * R:(blk + 1) * 2 * R, :], self.projT[c.odt:c.odt + 2 * R, :], r=[self.projT], w=[b1], acc=(blk > 0))
        self.MEMSET("pool", rm[:], 1.0, [rm])
        self.MEMSET("pool", rm[:].rearrange("p (c t) -> p c t", t=128)[:, :, 0:1], 0.0, [rm])
        self.AC(aexp[:], par[:, 1:2], ACT.Exp, [par], [aexp])
        self.AC(b1[:], b1[:], ACT.Exp, [b1, par], [b1], bias=par[:, 0:1])
        self.AC(b1[:], b1[:], ACT.Ln, [b1, self.onec], [b1], bias=self.onec[:PR, :])
        self.TS("dve", b2[:], b1[:], aexp[:, 0:1], -1.0, ALU.mult, ALU.mult, [b1, aexp], [b2])
        k.op("dve", lambda: nc.vector.tensor_tensor_scan(out=b3[:], data0=rm[:], data1=b2[:], initial=0.0, op0=ALU.mult, op1=ALU.add), [rm, b2], [b3])
        cs3 = b3[:].rearrange("p (c t) -> p c t", t=128)
        k.op("dve", lambda: nc.vector.tensor_copy(out=totc[:].unsqueeze(2), in_=cs3[:, :, 127:128]), [b3], [totc])
        tot3 = totc[:].unsqueeze(2).broadcast_to([PR, NC, 128])
        v3 = lambda b: b[:].rearrange("p (c t) -> p c t", t=128)
        self.TT("dve", b4[:], b2[:], b3[:], ALU.subtract, [b2, b3], [b4])
        self.TT("dve", v3(b4), v3(b4), tot3, ALU.add, [b4, totc], [b4])
        self.TS("dve", b5[:], b3[:], par[:, 6:7], None, ALU.mult, None, [b3, par], [b5])
        self.STT(b5[:], b4[:], par[:, 7:8], b5[:], ALU.mult, ALU.add, [b4, par, b5], [b5])
        self.TT("dve", v3(b4), tot3, v3(b3), ALU.subtract, [totc, b3], [b4])
        self.TS("dve", b4[:], b4[:], par[:, 6:7], None, ALU.mult, None, [b4, par], [b4])
        self.TT("dve", b2[:], b3[:], b2[:], ALU.subtract, [b3, b2], [b2])
        self.STT(b4[:], b2[:], par[:, 7:8], b4[:], ALU.mult, ALU.add, [b2, par, b4], [b4])
        self.AC(b4[:], b4[:], ACT.Exp, [b4], [b4])
        self.AC(b3[:], b5[:], ACT.Exp, [b5], [b3])
        self.TS("dve", b1[:], b1[:], par[:, 2:3], None, ALU.mult, None, [b1, par], [b1])
        self.STT(b1[:], b5[:], par[:, 3:4], b1[:], ALU.mult, ALU.add, [b5, par, b1], [b1])
        self.STT(b1[:], b4[:], par[:, 4:5], b1[:], ALU.mult, ALU.add, [b4, par, b1], [b1])
        self.STT(b1[:], b3[:], par[:, 5:6], b1[:], ALU.mult, ALU.add, [b3, par, b1], [b1])
        self.AC(etot[:], totc[:], ACT.Exp, [totc], [etot])
        k.dma("sp", self.Qd[:, :], b1[:], r=[b1], w=[self.Qd])
        k.dma("sp", self.etd[:, :], etot[:], r=[etot], w=[self.etd])
        k.phase_end()
        k.phase_begin()
        cw = k.sb("cw", [128, c.CT3, 5]); cb = k.sb("cb", [128, c.CT3])
        k.dma("sp", cw[:], I["convw_l"][:, :, :], r=[I["convw_l"]], w=[cw])
        k.dma("sp", cb[:], I["convb_l"][:, :], r=[I["convb_l"]], w=[cb])
        xin = [k.sb(f"xin{b}", [128, S + 4]) for b in range(2)]
        acc_ = [k.sb(f"cacc{b}", [128, S]) for b in range(2)]
        ob = [k.sb(f"cob{b}", [128, S], BF16) for b in range(1)]
        for ci in range(c.CT3):
            xi = xin[ci % 2]; a = acc_[ci % 2]
            self.MEMSET("pool", xi[:, 0:2], 0.0, [xi])
            self.MEMSET("pool", xi[:, S + 2:S + 4], 0.0, [xi], acc=True)
            k.dma("sp", xi[:, 2:S + 2], self.projT[c.ox + ci * 128: c.ox + (ci + 1) * 128, :], r=[self.projT], w=[xi], acc=True)
            self.TS("dve", a[:], xi[:, 0:S], cw[:, ci, 0:1], None, ALU.mult, None, [xi, cw], [a])
            for j in range(1, 5):
                self.STT(a[:], xi[:, j:S + j], cw[:, ci, j:j + 1], a[:], ALU.mult, ALU.add, [xi, cw, a], [a])
            if ci < c.XT:
                self.AC(a[:], a[:], ACT.Silu, [a, cb], [a], bias=cb[:, ci:ci + 1])
                k.dma("sp", self.xcT[ci * 128:(ci + 1) * 128, :], a[:], r=[a], w=[self.xcT], acc=True)
            else:
                o = ob[0]
                self.AC(o[:], a[:], ACT.Silu, [a, cb], [o], bias=cb[:, ci:ci + 1])
                j0 = (ci - c.XT) * 128
                k.dma("sp", self.BCb[j0:j0 + 128, :], o[:], r=[o], w=[self.BCb], acc=True)
        k.phase_end()

    def ssd_sweep(self, d):
        c, k, nc, I = self.cfg, self.k, self.nc, self.I
        R = c.R; PR = 8 * R; S = c.S; NC = c.NT; DIc = c.DIc; XT = c.XT
        HWD = min(512, DIc); H = DIc // HWD; HPH = HWD // 64; TPH = HWD // 128; G4 = HPH // 4
        if d == 0:
            self.yf = k.dram("yf", [S, DIc])
        else:
            self.yssd_b = k.dram("yssd_b", [DIc, S], BF16)
        k.phase_begin()
        idf = self.ident_f
        et = k.sb("et", [R, NC]); k.dma("sp", et[:], self.etd[d * R:(d + 1) * R, :], r=[self.etd], w=[et])
        diagE = k.sb("diagE", [R, NC, R])
        self.TT("dve", diagE[:], idf[0:R, 0:R].unsqueeze(1).broadcast_to([R, NC, R]), et[:].unsqueeze(2).broadcast_to([R, NC, R]), ALU.mult, [idf, et], [diagE])
        edec = k.sb("edec", [128, NC, R])
        negI = k.sb("negI", [R, R, 128])
        self.TS("dve", negI[:], idf[0:R, 0:R].unsqueeze(2).broadcast_to([R, R, 128]), -1.0, None, ALU.mult, None, [idf], [negI])
        xtp = k.ps("xtp", [128, 512]); ydg = k.ps("ydg", [128, 512]); yof = k.ps("yof", [128, 512]); stp = k.ps("stp", [128, 512])
        Dp = [k.ps(f"Dp{b}", [128, 512]) for b in range(2)]
        misc = k.ps("misc", [128, 512]); tb16 = k.ps("tb16", [128, 384])
        cbT = qtp = edp = misc
        btp = ybt = tb16
        dE = diagE[:].rearrange("p a b -> p (a b)"); eD = edec[:].rearrange("p a b -> p (a b)")
        ncol = NC * R
        for b0 in range(0, ncol, 256):
            w_ = min(256, ncol - b0)
            self.MM(edp[:, 256:256 + w_], self.ones_f[0:R, :], dE[:, b0:b0 + w_], True, True, [self.ones_f, diagE], [edp])
            self.AC(eD[:, b0:b0 + w_], edp[:, 256:256 + w_], ACT.Copy, [edp], [edec], acc=(b0 > 0))
        mask = self.maskF if d == 0 else self.maskB
        st32 = k.sb("st32", [128, DIc]); stb = k.sb("stb", [128, DIc], BF16)
        self.MEMSET("dve", st32[:], 0.0, [st32]); self.MEMSET("dve", stb[:], 0.0, [stb])
        dsk = k.sb("dsk", [128, DIc]); snw = k.sb("snw", [128, DIc])
        if d == 1:
            k.dma("sp", dsk[:], I["dskip_bc"][:, :], r=[I["dskip_bc"]], w=[dsk])
            k.dma("sp", snw[:], I["ssdw_bc"][:, :], r=[I["ssdw_bc"]], w=[snw])
        NB = 2
        xch = [k.sb(f"xch{b}", [128, XT, 128]) for b in range(NB)]
        Bc = [k.sb(f"Bc{b}", [128, 128], BF16) for b in range(NB)]
        Cc = [k.sb(f"Cc{b}", [128, 128], BF16) for b in range(NB)]
        Qc = [k.sb(f"Qc{b}", [PR, 128]) for b in range(NB)]
        C0 = [k.sb(f"C0{b}", [R, 128]) for b in range(NB)]
        qtok = [k.sb(f"qtok{b}", [128, PR]) for b in range(NB)]
        dtds = [k.sb(f"dtds{b}", [128, R]) for b in range(NB)]
        Btok = [k.sb(f"Btok{b}", [128, 128], BF16) for b in range(NB)]
        cbm = [k.sb(f"cbm{b}", [128, 128]) for b in range(NB)]
        BD = [k.sb(f"BD{b}", [R, R, 128]) for b in range(NB)]
        xdt = [k.sb(f"xdt{b}", [128, HWD], BF16) for b in range(NB)]
        xds = [k.sb(f"xds{b}", [128, HWD], BF16) for b in range(NB)]
        Dm = [k.sb(f"Dm{b}", [128, 512]) for b in range(NB)]
        Mt = [k.sb(f"Mt{b}", [128, 4, 128], BF16) for b in range(NB)]
        ydir = [k.sb(f"ydir{b}", [128, DIc]) for b in range(NB)]
        if d == 1:
            yfc = [k.sb(f"yfc{b}", [128, DIc]) for b in range(NB)]
            zc = [k.sb(f"zc{b}", [128, XT, 128]) for b in range(NB)]
            ssq = [k.sb(f"ssq{b}", [128, 4]) for b in range(NB)]
            junk = k.sb("junk", [128, HWD])
            yn = [k.sb(f"yn{b}", [128, DIc], BF16) for b in range(NB)]
            ysb = [k.sb(f"ysb{b}", [128, XT, 128], BF16) for b in range(NB)]
        xv = self.xcT.t.ap().rearrange("(j p) s -> p j s", p=128)
        zv = self.projT.t.ap()[c.oz:c.oz + DIc, :].rearrange("(j p) s -> p j s", p=128)
        n4 = 0
        import os
        cut = int(os.environ.get("SSD_CUT", "9"))
        for ci in range(NC if cut > 0 else 0):
            ch = ci if d == 0 else NC - 1 - ci
            b = ci % NB
            sl = slice(ch * 128, (ch + 1) * 128)
            k.dma("sp", xch[b][:], xv[:, :, sl], r=[self.xcT], w=[xch[b]])
            k.dma("sp", Bc[b][:], self.BCb[0:128, sl], r=[self.BCb], w=[Bc[b]])
            k.dma("sp", Cc[b][:], self.BCb[128:256, sl], r=[self.BCb], w=[Cc[b]])
            k.dma("sp", Qc[b][:], self.Qd[:, sl], r=[self.Qd], w=[Qc[b]])
            k.dma("sp", C0[b][:], self.Qd[2 * R + d * R: 2 * R + (d + 1) * R, sl], r=[self.Qd], w=[C0[b]])
            if d == 1:
                k.dma("sp", yfc[b][:], self.yf[sl, :], r=[self.yf], w=[yfc[b]])
                k.dma("sp", zc[b][:], zv[:, :, sl], r=[self.projT], w=[zc[b]])
            if cut < 2:
                continue
            skip = os.environ.get("SSD_SKIP", "")
            if "qtp" not in skip:
                self.TR(qtp[:, 128:128 + PR], Qc[b][:, :], idf[0:PR, 0:PR], [Qc[b], idf], [qtp])
                self.AC(qtok[b][:], qtp[:, 128:128 + PR], ACT.Copy, [qtp], [qtok[b]])
            qt = qtok[b]
            if "dtds" not in skip:
              self.TT("dve", dtds[b][:], qt[:, d * R:(d + 1) * R], qt[:, 4 * R + d * R:4 * R + (d + 1) * R], ALU.mult, [qt], [dtds[b]])
            skip = os.environ.get("SSD_SKIP", "")
            if "btp" not in skip:
                self.MM(btp[:, 0:128], Bc[b][:], self.ident_b[:], True, True, [Bc[b], self.ident_b], [btp])
                self.AC(Btok[b][:], btp[:, 0:128], ACT.Copy, [btp], [Btok[b]])
            if "cbt" not in skip:
                self.MM(cbT[:, 0:128], Bc[b][:], Cc[b][:], True, True, [Bc[b], Cc[b]], [cbT])
                self.TT("dve", cbm[b][:], cbT[:, 0:128], mask[:], ALU.mult, [cbT, mask], [cbm[b]])
            if "bd" not in skip:
              self.TT("dve", BD[b][:], idf[0:R, 0:R].unsqueeze(2).broadcast_to([R, R, 128]), C0[b][:].unsqueeze(1).broadcast_to([R, R, 128]), ALU.mult, [idf, C0[b]], [BD[b]])
            if d == 1:
                self.MEMSET("dve", ssq[b][:], 0.0, [ssq[b]])
            if cut < 3:
                continue
            for hf in range(H):
                hs = slice(hf * HWD, (hf + 1) * HWD)
                for j in range(TPH):
                    self.TR(xtp[:, j * 128:(j + 1) * 128], xch[b][:, hf * TPH + j, :], idf[:], [xch[b], idf], [xtp], acc=(j > 0))
                r0 = hf * HPH
                x3 = xtp[:, 0:HWD].rearrange("p (r q) -> p r q", q=64)
                dt_b = qt[:, d * R + r0: d * R + r0 + HPH].unsqueeze(2).broadcast_to([128, HPH, 64])
                dd_b = dtds[b][:, r0:r0 + HPH].unsqueeze(2).broadcast_to([128, HPH, 64])
                E_b = qt[:, 6 * R + d * R + r0: 6 * R + d * R + r0 + HPH].unsqueeze(2).broadcast_to([128, HPH, 64])
                self.TT("dve", xdt[b][:].rearrange("p (r q) -> p r q", q=64), x3, dt_b, ALU.mult, [xtp, qt], [xdt[b]])
                self.TT("dve", xds[b][:].rearrange("p (r q) -> p r q", q=64), x3, dd_b, ALU.mult, [xtp, dtds[b]], [xds[b]])
                yd = ydir[b]
                if d == 1:
                    self.TT("dve", yd[:, hs], xtp[:, 0:HWD], dsk[:, hs], ALU.mult, [xtp, dsk], [yd], acc=(hf > 0))
                for g4 in range(G4 if cut > 3 else 0):
                    dp = Dp[n4 % 2]; dm = Dm[n4 % 2]; mt = Mt[n4 % 2]; n4 += 1
                    h0 = r0 + g4 * 4
                    self.MM(dp[:], self.ones_f[0:R, :], BD[b][:, h0:h0 + 4, :].rearrange("p a b -> p (a b)"), True, False, [self.ones_f, BD[b]], [dp], acc=False)
                    self.MM(dp[:], C0[b][:, :], negI[:, h0:h0 + 4, :].rearrange("p a b -> p (a b)"), False, True, [C0[b], negI], [dp], acc=True)
                    self.TS("dve", dm[:], dp[:], 0.0, None, ALU.min, None, [dp], [dm])
                    self.AC(dm[:], dm[:], ACT.Exp, [dm], [dm])
                    self.TT("dve", mt[:], dm[:].rearrange("p (a b) -> p a b", b=128), cbm[b][:].unsqueeze(1).broadcast_to([128, 4, 128]), ALU.mult, [dm, cbm[b]], [mt])
                    for h in range(4):
                        hl = g4 * 4 + h
                        self.MM(ydg[:, hl * 64:(hl + 1) * 64], mt[:, h, :], xdt[b][:, hl * 64:(hl + 1) * 64], True, True, [mt, xdt[b]], [ydg], acc=(hl > 0))
                if cut < 5:
                    continue
                self.MM(yof[:, 0:HWD], Cc[b][:], stb[:, hs], True, True, [Cc[b], stb], [yof])
                if d == 0:
                    self.TT("dve", yd[:, hs].rearrange("p (r q) -> p r q", q=64), yof[:, 0:HWD].rearrange("p (r q) -> p r q", q=64), E_b, ALU.mult, [yof, qt], [yd], acc=(hf > 0))
                else:
                    tmp = Dm[n4 % 2]
                    self.TT("dve", tmp[:, 0:HWD].rearrange("p (r q) -> p r q", q=64), yof[:, 0:HWD].rearrange("p (r q) -> p r q", q=64), E_b, ALU.mult, [yof, qt], [tmp])
                    self.TT("dve", yd[:, hs], yd[:, hs], tmp[:, 0:HWD], ALU.add, [yd, tmp], [yd], acc=True)
                self.TT("dve", yd[:, hs], yd[:, hs], ydg[:, 0:HWD], ALU.add, [yd, ydg], [yd], acc=True)
                self.MM(stp[:, 0:HWD], Btok[b][:], xds[b][:], True, True, [Btok[b], xds[b]], [stp])
                e_b = edec[:, ch, r0:r0 + HPH].unsqueeze(2).broadcast_to([128, HPH, 64])
                self.TT("dve", st32[:, hs].rearrange("p (r q) -> p r q", q=64), st32[:, hs].rearrange("p (r q) -> p r q", q=64), e_b, ALU.mult, [st32, edec], [st32], acc=(hf > 0))
                self.TT("dve", st32[:, hs], st32[:, hs], stp[:, 0:HWD], ALU.add, [st32, stp], [st32], acc=True)
                self.AC(stb[:, hs], st32[:, hs], ACT.Copy, [st32], [stb], acc=(hf > 0))
                if d == 1:
                    self.TT("dve", yd[:, hs], yd[:, hs], yfc[b][:, hs], ALU.add, [yd, yfc[b]], [yd], acc=True)
                    for j in range(TPH):
                        self.TR(xtp[:, j * 128:(j + 1) * 128], zc[b][:, hf * TPH + j, :], idf[:], [zc[b], idf], [xtp], acc=(j > 0))
                    self.TT("dve", yd[:, hs], yd[:, hs], xtp[:, 0:HWD], ALU.mult, [yd, xtp], [yd], acc=True)
                    self.AC(junk[:], yd[:, hs], ACT.Square, [yd], [junk, ssq[b]], accum_out=ssq[b][:, hf:hf + 1])
            if cut < 6:
                continue
            if d == 0:
                k.dma("sp", self.yf[sl, :], ydir[b][:], r=[ydir[b]], w=[self.yf], acc=True)
            else:
                sq_ = ssq[b]
                if H > 1:
                    self.TT("dve", sq_[:, 0:1], sq_[:, 0:1], sq_[:, 1:2], ALU.add, [sq_], [sq_])
                self.AC(sq_[:, 2:3], sq_[:, 0:1], ACT.Sqrt, [sq_, self.eps], [sq_], bias=self.eps[:], scale=1.0 / DIc)
                k.op("dve", (lambda sq_=sq_: nc.vector.reciprocal(out=sq_[:, 3:4], in_=sq_[:, 2:3])), [sq_], [sq_])
                self.STT(yn[b][:], ydir[b][:], sq_[:, 3:4], snw[:], ALU.mult, ALU.mult, [ydir[b], sq_, snw], [yn[b]])
                for j in range(XT):
                    self.MM(ybt[:, 128 + (j % 2) * 128: 256 + (j % 2) * 128], yn[b][:, j * 128:(j + 1) * 128], self.ident_b[:], True, True, [yn[b], self.ident_b], [ybt])
                    self.AC(ysb[b][:, j, :], ybt[:, 128 + (j % 2) * 128: 256 + (j % 2) * 128], ACT.Copy, [ybt], [ysb[b]], acc=(j > 0))
                k.dma("sp", self.yssd_b.t.ap().rearrange("(j p) s -> p j s", p=128)[:, :, sl], ysb[b][:], r=[ysb[b]], w=[self.yssd_b], acc=True)
        k.phase_end()

    def phase4(self):
        c, k, nc, I = self.cfg, self.k, self.nc, self.I
        S = c.S; NT = c.NT; HHc = c.HHc; HW = c.HW
        NC32 = S // 32
        self.ofT = k.dram("ofT", [HW, S]); self.obT = k.dram("obT", [HW, S])
        k.phase_begin()
        idf, idb = self.ident_f, self.ident_b
        lbt = k.sb("lbt", [128, HHc, 2]); k.dma("sp", lbt[:], I["lb_l"][:, :, :], r=[I["lb_l"]], w=[lbt])
        lb = k.sb("lb", [128, HHc]); oml = k.sb("oml", [128, HHc])
        self.TT("dve", lb[:], lbt[:, :, 0], lbt[:, :, 1], ALU.subtract, [lbt], [lb])
        self.AC(lb[:], lb[:], ACT.Sigmoid, [lb], [lb])
        self.TS("dve", oml[:], lb[:], -1.0, 1.0, ALU.mult, ALU.add, [lb], [oml])
        rmask = k.sb("rmask", [128, 4]); k.dma("sp", rmask[:], I["rowmask"][:, :], r=[I["rowmask"]], w=[rmask])
        bm = k.sb("bm", [128, 256]); k.dma("sp", bm[:], I["bmask"][:, :], r=[I["bmask"]], w=[bm])
        PB = min(2048, S)
        rm = k.sb("rm32", [128, PB], BF16)
        self.MEMSET("dve", rm[:], 1.0, [rm])
        self.MEMSET("dve", rm[:].rearrange("p (c t) -> p c t", t=32)[:, :, 0:1], 0.0, [rm])
        t1 = k.sb("t1", [128, PB]); t2 = k.sb("t2", [128, PB]); t3 = k.sb("t3", [128, PB]); t4 = k.sb("t4", [128, PB])
        totc = k.sb("h_totc", [128, PB // 32])
        qt = [k.sb(f"qt{d}", [128, S], BF16) for d in range(2)]
        kt_ = [k.sb(f"kt{d}", [128, S], BF16) for d in range(2)]
        kd = [k.sb(f"kd{d}", [128, S], BF16) for d in range(2)]
        egl = [k.sb(f"egl{d}", [128, NC32]) for d in range(2)]
        vtok = k.sb("vtok", [128, NT, 128], BF16)
        pw = [k.ps(f"pw{d}", [128, 512]) for d in range(2)]
        pk = [k.ps(f"pk{d}", [128, 128]) for d in range(2)]
        pv = k.ps("pv", [128, 128])
        S32 = [k.sb(f"S32_{d}", [128, 128]) for d in range(2)]
        Sb = [[k.sb(f"Sb{d}_{b}", [128, 128], BF16) for b in range(2)] for d in range(2)]
        scm = [[k.sb(f"scm{d}_{b}", [128, 128], BF16) for b in range(2)] for d in range(2)]
        kdtok = [[k.sb(f"kdtok{d}_{b}", [128, 128], BF16) for b in range(2)] for d in range(2)]
        kdm = [[k.sb(f"kdm{d}_{b}", [128, 4, 128], BF16) for b in range(2)] for d in range(2)]
        oin = [[k.sb(f"oin{d}_{b}", [128, 128]) for b in range(2)] for d in range(2)]
        otl = [[k.sb(f"otl{d}_{b}", [128, 128]) for b in range(2)] for d in range(2)]
        vld = [k.sb(f"vld{b}", [128, 128]) for b in range(2)]
        for h in range(HHc):
            hr = h * 128
            for ti in range(NT):
                vb = vld[ti % 2]
                k.dma("sp", vb[:], self.projT[c.oi + hr: c.oi + hr + 128, ti * 128:(ti + 1) * 128], r=[self.projT], w=[vb])
                self.TR(pv[:], vb[:], idf[:], [vb, idf], [pv])
                self.AC(vtok[:, ti, :], pv[:], ACT.Copy, [pv], [vtok], acc=(ti > 0))
            for d in range(2):
                fo = c.off if d == 0 else c.ofb
                for pb in range(S // PB):
                    ps_ = slice(pb * PB, (pb + 1) * PB)
                    k.dma("sp", t1[:], self.projT[fo + hr: fo + hr + 128, ps_], r=[self.projT], w=[t1])
                    k.dma("sp", t4[:], self.projT[c.oq + hr: c.oq + hr + 128, ps_], r=[self.projT], w=[t4])
                    self.AC(t1[:], t1[:], ACT.Sigmoid, [t1], [t1])
                    self.TS("dve", t1[:], t1[:], oml[:, h:h + 1], lb[:, h:h + 1], ALU.mult, ALU.add, [t1, oml, lb], [t1])
                    self.AC(t2[:], t1[:], ACT.Ln, [t1], [t2])
                    self.TS("dve", t1[:], t1[:], -1.0, 1.0, ALU.mult, ALU.add, [t1], [t1])
                    k.op("dve", (lambda: nc.vector.tensor_tensor_scan(out=t3[:], data0=rm[:], data1=t2[:], initial=0.0, op0=ALU.mult, op1=ALU.add)), [rm, t2], [t3])
                    g3 = t3[:].rearrange("p (c t) -> p c t", t=32)
                    if d == 1:
                        k.op("dve", (lambda g3=g3: nc.vector.tensor_copy(out=totc[:].unsqueeze(2), in_=g3[:, :, 31:32])), [t3], [totc])
                        self.TT("dve", t3[:], t2[:], t3[:], ALU.subtract, [t2, t3], [t3])
                        self.TT("dve", g3, g3, totc[:].unsqueeze(2).broadcast_to([128, PB // 32, 32]), ALU.add, [t3, totc], [t3])
                    self.AC(t2[:], t3[:], ACT.Exp, [t3], [t2])
                    e3 = t2[:].rearrange("p (c t) -> p c t", t=32)
                    pos = 31 if d == 0 else 0
                    eg_dst = egl[d][:, pb * (PB // 32):(pb + 1) * (PB // 32)]
                    k.op("dve", (lambda e3=e3, eg_dst=eg_dst, pos=pos: nc.vector.tensor_copy(out=eg_dst.unsqueeze(2), in_=e3[:, :, pos:pos + 1])), [t2], [egl[d]], acc=(pb > 0))
                    self.TT("dve", qt[d][:, ps_], t4[:], t2[:], ALU.mult, [t4, t2], [qt[d]], acc=(pb > 0))
                    self.AC(t2[:], t3[:], ACT.Exp, [t3], [t2], scale=-1.0)
                    self.TT("dve", t1[:], t1[:], t2[:], ALU.mult, [t1, t2], [t1])
                    self.AC(kt_[d][:, ps_], t1[:], ACT.Copy, [t1], [kt_[d]], acc=(pb > 0))
                    self.TT("dve", kd[d][:, ps_].rearrange("p (c t) -> p c t", t=32), t1[:].rearrange("p (c t) -> p c t", t=32),
                            eg_dst.unsqueeze(2).broadcast_to([128, PB // 32, 32]), ALU.mult, [t1, egl[d]], [kd[d]], acc=(pb > 0))
                self.MEMSET("dve", S32[d][:], 0.0, [S32[d]])
                self.MEMSET("dve", Sb[d][0][:], 0.0, [Sb[d][0]])
            nS = [0, 0]
            for i in range(NT):
                for d in range(2):
                    ti = i if d == 0 else NT - 1 - i
                    tsl = slice(ti * 128, (ti + 1) * 128)
                    b = i % 2
                    P = pw[d]
                    scT = Buf(P.t, "scT"); oia = Buf(P.t, "oia"); oie = Buf(P.t, "oie"); stp = Buf(P.t, "stp")
                    self.MM(scT[:, 0:128], kt_[d][:, tsl], qt[d][:, tsl], True, True, [kt_[d], qt[d]], [P])
                    self.TT("dve", scm[d][b][:], P[:, 0:128], bm[:, d * 128:(d + 1) * 128], ALU.mult, [P, bm], [scm[d][b]])
                    self.MM(pk[d][:], kd[d][:, tsl], idb[:], True, True, [kd[d], idb], [pk[d]])
                    self.AC(kdtok[d][b][:], pk[d][:], ACT.Copy, [pk[d]], [kdtok[d][b]])
                    for j in range(4):
                        self.TS("dve", kdm[d][b][:, j, :], kdtok[d][b][:], rmask[:, j:j + 1], None, ALU.mult, None, [kdtok[d][b], rmask], [kdm[d][b]], acc=(j > 0))
                    self.MM(P[:, 128:256], vtok[:, ti, :], scm[d][b][:], True, True, [vtok, scm[d][b]], [P])
                    self.AC(oin[d][b][:], P[:, 128:256], ACT.Copy, [P], [oin[d][b]])
                    for jj in range(4):
                        j = jj if d == 0 else 3 - jj
                        cs_ = slice(ti * 128 + j * 32, ti * 128 + (j + 1) * 32)
                        sb_cur = Sb[d][nS[d] % 2]; sb_nxt = Sb[d][(nS[d] + 1) % 2]; nS[d] += 1
                        self.MM(P[:, 256 + j * 32:256 + (j + 1) * 32], sb_cur[:], qt[d][:, cs_], True, True, [sb_cur, qt[d]], [P])
                        self.MM(P[:, 384:512], kdm[d][b][:, j, :], vtok[:, ti, :], True, True, [kdm[d][b], vtok], [P])
                        cidx = ti * 4 + j
                        self.STT(S32[d][:], S32[d][:], egl[d][:, cidx:cidx + 1], P[:, 384:512], ALU.mult, ALU.add, [S32[d], egl[d], P], [S32[d]])
                        self.AC(sb_nxt[:], S32[d][:], ACT.Copy, [S32[d]], [sb_nxt])
                    self.TT("dve", otl[d][b][:], oin[d][b][:], P[:, 256:384], ALU.add, [oin[d][b], P], [otl[d][b]])
                    dst = self.ofT if d == 0 else self.obT
                    k.dma("sp", dst[hr:hr + 128, tsl], otl[d][b][:], r=[otl[d][b]], w=[dst], acc=True)
        k.phase_end()

    def phase4b(self):
        c, k, nc, I = self.cfg, self.k, self.nc, self.I
        S = c.S; HHc = c.HHc; HW = c.HW; TB = 512
        self.oT = k.dram("oT", [HW, S]); ssqb = k.dram("hssq_b", [1, S]); ssqg = k.dram("hssq_g", [8, S])
        self.yhg_b = k.dram("yhg_b", [HW, S], BF16)
        k.phase_begin()
        a = [k.sb(f"ha{b}", [128, HHc, TB]) for b in range(2)]
        bb = [k.sb(f"hb{b}", [128, HHc, TB]) for b in range(2)]
        sq = k.sb("hsq", [128, HHc, TB])
        ps = [k.ps(f"hps{b}", [128, TB]) for b in range(2)]
        row = [k.sb(f"hrow{b}", [1, TB]) for b in range(2)]
        ov = lambda t: t.t.ap().rearrange("(h p) s -> p h s", p=128)
        for tb in range(S // TB):
            ts_ = slice(tb * TB, (tb + 1) * TB); i = tb % 2
            k.dma("sp", a[i][:], ov(self.ofT)[:, :, ts_], r=[self.ofT], w=[a[i]])
            k.dma("sp", bb[i][:], ov(self.obT)[:, :, ts_], r=[self.obT], w=[bb[i]])
            self.TT("dve", a[i][:], a[i][:], bb[i][:], ALU.add, [a[i], bb[i]], [a[i]])
            self.AC(sq[:].rearrange("p a b -> p (a b)"), a[i][:].rearrange("p a b -> p (a b)"), ACT.Square, [a[i]], [sq])
            for h in range(HHc):
                self.MM(ps[i][:], self.ones_f[:], sq[:, h, :], h == 0, h == HHc - 1, [self.ones_f, sq], [ps[i]])
            self.AC(row[i][:], ps[i][0:1, :], ACT.Copy, [ps[i]], [row[i]])
            k.dma("sp", ssqb[0:1, ts_], row[i][:], r=[row[i]], w=[ssqb], acc=True)
            k.dma("sp", ov(self.oT)[:, :, ts_], a[i][:], r=[a[i]], w=[self.oT], acc=True)
        k.phase_end()
        self.ag(ssqb, ssqg)
        k.phase_begin()
        hgw = k.sb("hgw", [128, HHc]); k.dma("sp", hgw[:], I["hgw_l"][:, :], r=[I["hgw_l"]], w=[hgw])
        a = [k.sb(f"ha{b}", [128, HHc, TB]) for b in range(2)]
        g = [k.sb(f"hg{b}", [128, HHc, TB]) for b in range(2)]
        s8 = [k.sb(f"s8{b}", [8, TB]) for b in range(2)]
        rs = [k.sb(f"hrs{b}", [128, TB]) for b in range(2)]
        yo = [k.sb(f"hyo{b}", [128, HHc, TB], BF16) for b in range(2)]
        ps = [k.ps(f"hps{b}", [128, TB]) for b in range(2)]
        gv = self.projT.t.ap()[c.og:c.og + HW, :].rearrange("(h p) s -> p h s", p=128)
        for tb in range(S // TB):
            ts_ = slice(tb * TB, (tb + 1) * TB); i = tb % 2
            k.dma("sp", a[i][:], ov(self.oT)[:, :, ts_], r=[self.oT], w=[a[i]])
            k.dma("sp", g[i][:], gv[:, :, ts_], r=[self.projT], w=[g[i]])
            k.dma("sp", s8[i][:], ssqg[:, ts_], r=[ssqg], w=[s8[i]])
            self.MM(ps[i][:], self.ones_f[0:8, :], s8[i][:], True, True, [self.ones_f, s8[i]], [ps[i]])
            self.AC(rs[i][:], ps[i][:], ACT.Sqrt, [ps[i], self.eps], [rs[i]], bias=self.eps[:], scale=1.0 / c.D)
            k.op("dve", (lambda r_=rs[i]: nc.vector.reciprocal(out=r_[:], in_=r_[:])), [rs[i]], [rs[i]])
            self.TT("dve", a[i][:], a[i][:], rs[i][:].unsqueeze(1).broadcast_to([128, HHc, TB]), ALU.mult, [a[i], rs[i]], [a[i]])
            self.TT("pool", a[i][:], a[i][:], hgw[:, :].unsqueeze(2).broadcast_to([128, HHc, TB]), ALU.mult, [a[i], hgw], [a[i]])
            self.TT("dve", yo[i][:], a[i][:], g[i][:], ALU.mult, [a[i], g[i]], [yo[i]])
            k.dma("sp", ov(self.yhg_b)[:, :, ts_], yo[i][:], r=[yo[i]], w=[self.yhg_b], acc=True)
        k.phase_end()

    def phase5(self):
        c, k, nc, I = self.cfg, self.k, self.nc, self.I
        S = c.S; DC = c.DC; TB = 256
        self.yhgT = k.dram("yhgT", [c.D, S], BF16)
        self.ag(self.yhg_b, self.yhgT)
        t1d = k.dram("t1d", [DC, S]); self.merged_b = k.dram("merged_b", [DC, S], BF16)
        self.mergedT = k.dram("mergedT", [c.D, S], BF16)
        k.phase_begin()
        sg = [k.sb(f"sg{b}", [128, TB]) for b in range(3)]; o = [k.sb(f"o5{b}", [128, TB]) for b in range(3)]
        cnt = [0]
        def epi_a(c0, m, tb, ps):
            i = cnt[0] % 3; cnt[0] += 1
            ts_ = slice(tb * TB, (tb + 1) * TB)
            k.dma("sp", sg[i][:m, :], self.projT[c.oga + c0: c.oga + c0 + m, ts_], r=[self.projT], w=[sg[i]])
            self.TT("dve", o[i][:m, :], ps[:m, :], sg[i][:m, :], ALU.mult, [ps, sg[i]], [o[i]])
            k.dma("sp", t1d[c0:c0 + m, ts_], o[i][:m, :], r=[o[i]], w=[t1d], acc=True)
        nk = c.DI // 128
        segs = []
        for s0 in range(0, nk, 32):
            n_ = min(32, nk - s0)
            segs.append((self.yssdT, s0, n_, self.wview(I["w_so"], s0, n_), I["w_so"]))
        self.gemm(segs, DC, S, epi_a, TB=TB, tag="ya")
        k.phase_end()
        k.phase_begin()
        sg = [k.sb(f"sg{b}", [128, TB]) for b in range(3)]; t1 = [k.sb(f"t1{b}", [128, TB]) for b in range(3)]
        o = [k.sb(f"o5{b}", [128, TB]) for b in range(3)]; ob = [k.sb(f"ob5{b}", [128, TB], BF16) for b in range(3)]
        cnt = [0]
        def epi_b(c0, m, tb, ps):
            i = cnt[0] % 3; cnt[0] += 1
            ts_ = slice(tb * TB, (tb + 1) * TB)
            k.dma("sp", sg[i][:m, :], self.projT[c.ogb + c0: c.ogb + c0 + m, ts_], r=[self.projT], w=[sg[i]])
            k.dma("sp", t1[i][:m, :], t1d[c0:c0 + m, ts_], r=[t1d], w=[t1[i]])
            self.TT("dve", o[i][:m, :], ps[:m, :], sg[i][:m, :], ALU.mult, [ps, sg[i]], [o[i]])
            self.TT("dve", ob[i][:m, :], o[i][:m, :], t1[i][:m, :], ALU.add, [o[i], t1[i]], [ob[i]])
            k.dma("sp", self.merged_b[c0:c0 + m, ts_], ob[i][:m, :], r=[ob[i]], w=[self.merged_b], acc=True)
        self.gemm([(self.yhgT, 0, c.KT, self.wview(I["w_ho"], 0, c.KT), I["w_ho"])], DC, S, epi_b, TB=TB, tag="yb")
        k.phase_end()
        self.ag(self.merged_b, self.mergedT)
        self.ymixT = k.dram("ymixT", [DC, S])
        self.resid_gemm(self.mergedT, c.KT, self.wview(I["w_mo"], 0, c.KT), I["w_mo"], self.ymixT, "mx")
        self.x1T_b = k.dram("x1T_b", [DC, S]); self.x1Tf = k.dram("x1Tf", [c.D, S])
        self.resid_pass(self.ymixT, self.I["xT"], self.Gm, self.x1T_b, "mx")
        self.ag(self.x1T_b, self.x1Tf)

    def resid_gemm(self, actT, nkt, wfn, wbuf, ydst, tag):
        c, k, nc = self.cfg, self.k, self.nc
        S = c.S; DC = c.DC; TB = 512
        ssqb = k.dram(f"{tag}_ssqb", [1, S]); ssqg = k.dram(f"{tag}_ssqg", [8, S])
        k.phase_begin()
        o = [k.sb(f"ro{b}", [128, TB]) for b in range(3)]; sq = [k.sb(f"rsq{b}", [128, TB]) for b in range(2)]
        pss = k.ps("rpss", [128, TB]); row = [k.sb(f"rrow{b}", [1, TB]) for b in range(2)]
        cnt = [0]
        def epi(c0, m, tb, ps):
            i = cnt[0] % 3; j = cnt[0] % 2; cnt[0] += 1
            ts_ = slice(tb * TB, (tb + 1) * TB)
            ct = c0 // 128
            self.AC(o[i][:m, :], ps[:m, :], ACT.Copy, [ps], [o[i]])
            self.AC(sq[j][:m, :], ps[:m, :], ACT.Square, [ps], [sq[j]])
            k.dma("sp", ydst[c0:c0 + m, ts_], o[i][:m, :], r=[o[i]], w=[ydst], acc=True)
            self.MM(pss[:], self.ones_f[:m, :], sq[j][:m, :], ct == 0, ct == c.DCT - 1, [self.ones_f, sq[j]], [pss])
            if ct == c.DCT - 1:
                self.AC(row[tb % 2][:], pss[0:1, :], ACT.Copy, [pss], [row[tb % 2]])
                k.dma("sp", ssqb[0:1, ts_], row[tb % 2][:], r=[row[tb % 2]], w=[ssqb], acc=True)
        self.gemm([(actT, 0, nkt, wfn, wbuf)], DC, S, epi, TB=TB, tag=tag)
        k.phase_end()
        self.ag(ssqb, ssqg)
        if not hasattr(self, "ssq8"):
            self.ssq8 = {}
        self.ssq8[tag] = ssqg

    def resid_pass(self, yT, srcT, G, dst, tag):
        c, k, nc = self.cfg, self.k, self.nc
        S = c.S; DCT = c.DCT; TB = 512
        ssqg = self.ssq8[tag]
        k.phase_begin()
        y = [k.sb(f"py{b}", [128, DCT, TB]) for b in range(2)]; x = [k.sb(f"px{b}", [128, DCT, TB]) for b in range(2)]
        s8 = [k.sb(f"ps8{b}", [8, TB]) for b in range(2)]; rs = [k.sb(f"prs{b}", [128, TB]) for b in range(2)]
        ps = [k.ps(f"pps{b}", [128, TB]) for b in range(2)]
        v = lambda t: t.t.ap().rearrange("(j p) s -> p j s", p=128)
        for tb in range(S // TB):
            ts_ = slice(tb * TB, (tb + 1) * TB); i = tb % 2
            k.dma("sp", y[i][:], v(yT)[:, :, ts_], r=[yT], w=[y[i]])
            k.dma("sp", x[i][:], v(srcT)[:, :, ts_], r=[srcT], w=[x[i]])
            k.dma("sp", s8[i][:], ssqg[:, ts_], r=[ssqg], w=[s8[i]])
            self.MM(ps[i][:], self.ones_f[0:8, :], s8[i][:], True, True, [self.ones_f, s8[i]], [ps[i]])
            self.AC(rs[i][:], ps[i][:], ACT.Sqrt, [ps[i], self.eps], [rs[i]], bias=self.eps[:], scale=1.0 / c.D)
            k.op("dve", (lambda r_=rs[i]: nc.vector.reciprocal(out=r_[:], in_=r_[:])), [rs[i]], [rs[i]])
            self.TT("dve", y[i][:], y[i][:], rs[i][:].unsqueeze(1).broadcast_to([128, DCT, TB]), ALU.mult, [y[i], rs[i]], [y[i]])
            self.TT("pool", y[i][:], y[i][:], G[:, :].unsqueeze(2).broadcast_to([128, DCT, TB]), ALU.mult, [y[i], G], [y[i]])
            self.TT("dve", x[i][:], x[i][:], y[i][:], ALU.add, [x[i], y[i]], [x[i]])
            k.dma("sp", v(dst)[:, :, ts_], x[i][:], r=[x[i]], w=[dst], acc=True)
        k.phase_end()

    def phase7(self):
        c, k, nc, I = self.cfg, self.k, self.nc, self.I
        S = c.S; NT = c.NT; cap = c.cap; D = c.D; F = c.F; KT = c.KT; FT = c.FT; DC = c.DC; DCT = c.DCT
        self.posd = k.dram("posd", [16, S]); self.gmd = k.dram("gmd", [16, S])
        ploc = k.sb("ploc", [128, NT, 2])
        k.phase_begin()
        lg = k.sb("lg", [16, S]); k.dma("sp", lg[:], self.logits[:, :], r=[self.logits], w=[lg])
        aff = lg; junk = k.sb("mjunk", [16, S]); cum = k.sb("cum", [16, S]); onesr = k.sb("onesr", [16, S], BF16)
        self.posm = cum; self.gm = aff
        pst = k.ps("mps", [16, 512]); rcp = k.sb("rcp", [16, 512])
        self.AC(aff[:], lg[:], ACT.Exp, [lg], [aff])
        for tb in range(S // 512):
            ts_ = slice(tb * 512, (tb + 1) * 512)
            self.MM(pst[:], self.ones_f[0:16, 0:16], aff[:, ts_], True, True, [self.ones_f, aff], [pst])
            k.op("dve", (lambda ts_=ts_: nc.vector.reciprocal(out=rcp[:], in_=pst[:])), [pst], [rcp])
            self.TT("dve", aff[:, ts_], aff[:, ts_], rcp[:], ALU.mult, [aff, rcp], [aff], acc=False)
        sc = k.sb("bis", [16, 8])
        self.MEMSET("dve", sc[:, 0:1], 0.0, [sc]); self.MEMSET("dve", sc[:, 1:2], 2.0, [sc], acc=True)
        lo, hi, mid, cn, se, dd = (sc[:, i:i + 1] for i in range(6))
        for it in range(40):
            self.TS("dve", mid, lo, hi, 0.5, ALU.add, ALU.mult, [sc], [sc])
            self.TS("dve", junk[:], aff[:], mid, 0.0, ALU.is_ge, ALU.add, [aff, sc], [junk, sc], accum_out=cn)
            self.TS("dve", se, cn, float(cap), None, ALU.is_ge, None, [sc], [sc])
            self.TT("dve", dd, mid, lo, ALU.subtract, [sc], [sc])
            self.STT(lo, dd, se, lo, ALU.mult, ALU.add, [sc], [sc])
            self.TT("dve", dd, hi, mid, ALU.subtract, [sc], [sc])
            self.STT(hi, dd, se, mid, ALU.mult, ALU.add, [sc], [sc])
        mask = junk
        self.TS("dve", mask[:], aff[:], lo, None, ALU.is_ge, None, [aff, sc], [mask])
        self.MEMSET("pool", onesr[:], 1.0, [onesr])
        k.op("dve", lambda: nc.vector.tensor_tensor_scan(out=cum[:], data0=onesr[:], data1=mask[:], initial=0.0, op0=ALU.mult, op1=ALU.add), [onesr, mask], [cum])
        self.TT("dve", self.posm[:], cum[:], mask[:], ALU.mult, [cum, mask], [self.posm])
        self.TS("dve", self.posm[:], self.posm[:], -1.0, None, ALU.add, None, [self.posm], [self.posm])
        self.TT("dve", self.gm[:], aff[:], mask[:], ALU.mult, [aff, mask], [self.gm])
        es = k.sb("esel", [16, 2]); k.dma("sp", es[:], I["esel"][:, :], r=[I["esel"]], w=[es])
        pp = k.ps("mpp", [128, NT * 2])
        for ti in range(NT):
            self.MM(pp[:, ti * 2:(ti + 1) * 2], self.posm[:, ti * 128:(ti + 1) * 128], es[:], True, True, [self.posm, es], [pp], acc=(ti > 0))
        self.AC(ploc[:].rearrange("p a b -> p (a b)"), pp[:], ACT.Copy, [pp], [ploc])
        k.dma("sp", self.posd[:, :], self.posm[:], r=[self.posm], w=[self.posd])
        k.dma("sp", self.gmd[:, :], self.gm[:], r=[self.gm], w=[self.gmd])
        k.phase_end()
        self.xgT = k.dram("xgT", [D, 2 * cap], BF16)
        CB = min(512, cap)
        k.phase_begin()
        io_i = k.sb("io_i", [128, CB], I32); io_f = k.sb("io_f", [128, CB])
        k.op("pool", lambda: nc.gpsimd.iota(io_i[:], pattern=[[1, CB]], base=0, channel_multiplier=0), (), [io_i])
        k.op("dve", lambda: nc.vector.tensor_copy(out=io_f[:], in_=io_i[:]), [io_i], [io_f])
        sel = k.sb("sel", [128, NT, CB], BF16)
        hl = [k.sb(f"hl{b}", [128, 512], BF16) for b in range(3)]
        gps = [k.ps(f"gps{b}", [128, CB]) for b in range(4)]
        go = [k.sb(f"go{b}", [128, CB], BF16) for b in range(2)]
        DG = min(512, D); ND = DG // 128
        nl = 0; ng = 0
        for j in range(2):
            for cb in range(cap // CB):
                for ti in range(NT):
                    self.TS("dve", sel[:, ti, :], io_f[:], float(cb * CB), ploc[:, ti, j:j + 1], ALU.add, ALU.is_equal, [io_f, ploc], [sel], acc=(ti > 0))
                for dg in range(D // DG):
                    for ti in range(NT):
                        h_ = hl[nl % 3]; nl += 1
                        k.dma("sp", h_[:, :DG], self.h2tok[ti * 128:(ti + 1) * 128, dg * DG:(dg + 1) * DG], r=[self.h2tok], w=[h_])
                        for q in range(ND):
                            self.MM(gps[q][:], h_[:, q * 128:(q + 1) * 128], sel[:, ti, :], ti == 0, ti == NT - 1, [h_, sel], [gps[q]])
                    for q in range(ND):
                        g_ = go[ng % 2]; ng += 1
                        self.AC(g_[:], gps[q][:], ACT.Copy, [gps[q]], [g_])
                        r0 = dg * DG + q * 128
                        k.dma("sp", self.xgT[r0:r0 + 128, j * cap + cb * CB: j * cap + (cb + 1) * CB], g_[:], r=[g_], w=[self.xgT], acc=True)
        k.phase_end()
        TBm = min(512, cap)
        gtmp = k.dram("gtmp", [2 * F, cap]); self.hid_b = k.dram("hid_b", [2 * F, cap], BF16); self.hidT = k.dram("hidT", [16 * F, cap], BF16)
        for j in range(2):
            k.phase_begin()
            o = [k.sb(f"eo{b}", [128, TBm]) for b in range(3)]
            cnt = [0]
            def epi_g(c0, m, tb, ps, j=j, o=o, cnt=cnt):
                i = cnt[0] % 3; cnt[0] += 1
                self.AC(o[i][:m, :], ps[:m, :], ACT.Silu, [ps], [o[i]])
                k.dma("sp", gtmp[j * F + c0: j * F + c0 + m, tb * TBm:(tb + 1) * TBm], o[i][:m, :], r=[o[i]], w=[gtmp], acc=True)
            self.gemm([(self.xgT, 0, KT, self.wview(I["w_g"], 0, KT, r0=j * D), I["w_g"])], F, cap, epi_g, TB=TBm, t0=j * cap, tag=f"eg{j}")
            k.phase_end()
            k.phase_begin()
            gl = [k.sb(f"gl{b}", [128, TBm]) for b in range(3)]; ob = [k.sb(f"eob{b}", [128, TBm], BF16) for b in range(3)]
            cnt = [0]
            def epi_u(c0, m, tb, ps, j=j, gl=gl, ob=ob, cnt=cnt):
                i = cnt[0] % 3; cnt[0] += 1
                k.dma("sp", gl[i][:m, :], gtmp[j * F + c0: j * F + c0 + m, tb * TBm:(tb + 1) * TBm], r=[gtmp], w=[gl[i]])
                self.TT("dve", ob[i][:m, :], ps[:m, :], gl[i][:m, :], ALU.mult, [ps, gl[i]], [ob[i]])
                k.dma("sp", self.hid_b[j * F + c0: j * F + c0 + m, tb * TBm:(tb + 1) * TBm], ob[i][:m, :], r=[ob[i]], w=[self.hid_b], acc=True)
            self.gemm([(self.xgT, 0, KT, self.wview(I["w_u"], 0, KT, r0=j * D), I["w_u"])], F, cap, epi_u, TB=TBm, t0=j * cap, tag=f"eu{j}")
            k.phase_end()
        self.ag(self.hid_b, self.hidT)
        self.ydd = k.dram("ydd", [16 * cap, DC], BF16)
        k.phase_begin()
        hT = [k.sb(f"dh{b}", [128, FT, cap], BF16) for b in range(2)]
        wd = [k.sb(f"dw{b}", [128, FT, DC], BF16) for b in range(2)]
        dps = [k.ps(f"dps{b}", [128, DC]) for b in range(2)]
        yo = [k.sb(f"dyo{b}", [128, DC], BF16) for b in range(3)]
        n = 0
        for e in range(16):
            h_ = hT[e % 2]; w_ = wd[e % 2]
            k.dma("sp", h_[:], self.hidT.t.ap()[e * F:(e + 1) * F, :].rearrange("(ft p) c -> p ft c", p=128), r=[self.hidT], w=[h_])
            k.dma("pool", w_[:], I["w_d"].t.ap()[e * F:(e + 1) * F, :].rearrange("(ft p) c -> p ft c", p=128), r=[I["w_d"]], w=[w_])
            for ct in range(cap // 128):
                ps = dps[n % 2]; y_ = yo[n % 3]; n += 1
                for ft in range(FT):
                    self.MM(ps[:], h_[:, ft, ct * 128:(ct + 1) * 128], w_[:, ft, :], ft == 0, ft == FT - 1, [h_, w_], [ps])
                self.AC(y_[:], ps[:], ACT.Copy, [ps], [y_])
                k.dma("sp", self.ydd[e * cap + ct * 128: e * cap + (ct + 1) * 128, :], y_[:], r=[y_], w=[self.ydd], acc=True)
        k.phase_end()
        self.y2T = k.dram("y2T", [DC, S])
        ssqb = k.dram("mo_ssqb", [1, S]); ssqg = k.dram("mo_ssqg", [8, S])
        k.phase_begin()
        osl = k.sb("osl", [16, 16 * 128]); k.dma("sp", osl[:], I["onesel"][:, :], r=[I["onesel"]], w=[osl])
        cidx = k.sb("cidx", [128, max(1, cap // 128)]); k.dma("sp", cidx[:], I["cidx"][:, :], r=[I["cidx"]], w=[cidx])
        y2 = [k.ps(f"y2ps{b}", [128, 512]) for b in range(DCT)]
        bp = k.ps("bp", [128, 512]); bg = k.ps("bg", [128, 512]); pss = k.ps("cpss", [128, 512])
        bps = [k.sb(f"bps{b}", [128, 512]) for b in range(2)]; bgs = [k.sb(f"bgs{b}", [128, 512]) for b in range(2)]
        Pm = [k.sb(f"Pm{b}", [128, 512], BF16) for b in range(3)]
        yl = [k.sb(f"yl{b}", [128, DC], BF16) for b in range(3)]
        o = [k.sb(f"co{b}", [128, 512]) for b in range(2)]; sq = [k.sb(f"csq{b}", [128, 512]) for b in range(2)]
        row = [k.sb(f"crow{b}", [1, 512]) for b in range(2)]
        pzl = [k.sb(f"pzl{b}", [16, 512]) for b in range(2)]; gzl = [k.sb(f"gzl{b}", [16, 512]) for b in range(2)]
        n = 0
        NCT = cap // 128
        for tb in range(S // 512):
            ts_ = slice(tb * 512, (tb + 1) * 512)
            pz = pzl[tb % 2]; gz = gzl[tb % 2]
            k.dma("sp", pz[:], self.posd[:, ts_], r=[self.posd], w=[pz])
            k.dma("sp", gz[:], self.gmd[:, ts_], r=[self.gmd], w=[gz])
            for e in range(16):
                i2 = e % 2
                self.MM(bp[:], osl[:, e * 128:(e + 1) * 128], pz[:], True, True, [osl, pz], [bp])
                self.MM(bg[:], osl[:, e * 128:(e + 1) * 128], gz[:], True, True, [osl, gz], [bg])
                self.AC(bps[i2][:], bp[:], ACT.Copy, [bp], [bps[i2]])
                self.AC(bgs[i2][:], bg[:], ACT.Copy, [bg], [bgs[i2]])
                for ct in range(NCT):
                    pm = Pm[n % 3]; y_ = yl[n % 3]; n += 1
                    self.STT(pm[:], bps[i2][:], cidx[:, ct:ct + 1], bgs[i2][:], ALU.is_equal, ALU.mult, [bps[i2], cidx, bgs[i2]], [pm])
                    k.dma("sp", y_[:], self.ydd[e * cap + ct * 128: e * cap + (ct + 1) * 128, :], r=[self.ydd], w=[y_])
                    first = (e == 0 and ct == 0); last = (e == 15 and ct == NCT - 1)
                    for q in range(DCT):
                        self.MM(y2[q][:], y_[:, q * 128:(q + 1) * 128], pm[:], first, last, [y_, pm], [y2[q]])
            for q in range(DCT):
                i = q % 2
                self.AC(o[i][:], y2[q][:], ACT.Copy, [y2[q]], [o[i]])
                self.AC(sq[i][:], y2[q][:], ACT.Square, [y2[q]], [sq[i]])
                k.dma("sp", self.y2T[q * 128:(q + 1) * 128, ts_], o[i][:], r=[o[i]], w=[self.y2T], acc=True)
                self.MM(pss[:], self.ones_f[:], sq[i][:], q == 0, q == DCT - 1, [self.ones_f, sq[i]], [pss])
            self.AC(row[tb % 2][:], pss[0:1, :], ACT.Copy, [pss], [row[tb % 2]])
            k.dma("sp", ssqb[0:1, ts_], row[tb % 2][:], r=[row[tb % 2]], w=[ssqb], acc=True)
        k.phase_end()
        self.ag(ssqb, ssqg)
        self.ssq8["mo"] = ssqg

    def build(self):
        import os
        c, k = self.cfg, self.k
        stop = int(os.environ.get("STOP_AFTER", "99"))
        self.declare_inputs()
        self.consts()
        steps = []
        def hT_():
            self.hT = k.dram("hT", [c.D, c.S], BF16)
            self.norm_phase(self.xTf, self.A_m, self.B_m, self.hT)
            self.dbg("hT", self.hT, [c.D, c.S], BF16)
        def p2_():
            self.phase2(); self.dbg("projT", self.projT, [c.NCOL, c.S])
        def p3_():
            self.phase3_prep()
            self.dbg("Qd", self.Qd, [8 * c.R, c.S]); self.dbg("xcT", self.xcT, [c.DIc, c.S])
        def s0_():
            self.ssd_sweep(0); self.dbg("yf", self.yf, [c.S, c.DIc])
        def s1_():
            self.ssd_sweep(1); self.dbg("yssd_b", self.yssd_b, [c.DIc, c.S], BF16)
            self.yssdT = k.dram("yssdT", [c.DI, c.S], BF16)
            self.ag(self.yssd_b, self.yssdT)
        def p4_():
            self.phase4(); self.dbg("ofT", self.ofT, [c.HW, c.S]); self.dbg("obT", self.obT, [c.HW, c.S])
        def p4b_():
            self.phase4b(); self.dbg("yhg_b", self.yhg_b, [c.HW, c.S], BF16)
        def p5_():
            self.phase5(); self.dbg("x1T_b", self.x1T_b, [c.DC, c.S])
        def n2_():
            self.logits = k.dram("logits", [16, c.S])
            self.h2T = k.dram("h2T", [c.D, c.S], BF16); self.h2tok = k.dram("h2tok", [c.S, c.D], BF16)
            self.norm_phase(self.x1Tf, self.A_f, self.B_f, self.h2T, router=True)
            self.dbg("h2T", self.h2T, [c.D, c.S], BF16)
        def p7_():
            self.phase7(); self.dbg("y2T", self.y2T, [c.DC, c.S])
            self.resid_pass(self.y2T, self.x1T_b, self.Gf, self.outT, "mo")
        steps = [self.phase0, hT_, p2_, p3_, s0_, s1_, p4_, p4b_, p5_, n2_, p7_]
        for i, st in enumerate(steps):
            if i > stop:
                break
            st()
        k.finish()
        return self.nc


def _lay(v, nt):
    return np.ascontiguousarray(np.asarray(v).reshape(nt, 128).T)


def shard_inputs(cfg, inp):
    c = cfg
    D, S, R = c.D, c.S, c.R
    x = np.asarray(inp["x"])[0]
    xT = np.ascontiguousarray(x.T)
    w_ada = np.asarray(inp["w_ada"])[0]; b_ada = np.asarray(inp["b_ada"])[0]
    w_in = np.asarray(inp["w_in"])[0]
    conv_w = np.asarray(inp["conv_w"])[0]; conv_b = np.asarray(inp["conv_b"])[0]
    sizes = [c.DI, c.DI + 2 * 8 * c.N, 8 * R, 8 * R, D, D, D, D, D, D, D]
    offs = np.cumsum([0] + sizes)
    o_z, o_xbc, o_dtf, o_dtb, o_q, o_ff, o_fb, o_i, o_g, o_ga, o_gb = offs[:11]
    maps = []
    eye16 = np.eye(16, dtype=np.float32)
    onesel = np.ascontiguousarray(np.repeat(eye16[:, :, None], 128, axis=2).reshape(16, 16 * 128))
    nct = max(1, c.cap // 128)
    cidx = (np.arange(128, dtype=np.float32)[:, None] + 128.0 * np.arange(nct, dtype=np.float32)[None, :]).astype(np.float32)
    rowmask = np.zeros((128, 4), np.float32)
    for j in range(4):
        rowmask[j * 32:(j + 1) * 32, j] = 1.0
    s_ = np.arange(128)[:, None]; t_ = np.arange(128)[None, :]
    same = (s_ // 32) == (t_ // 32)
    bmask = np.concatenate([(same & (s_ <= t_)), (same & (s_ >= t_))], axis=1).astype(np.float32)
    for g in range(8):
        m = {}
        cs_ = slice(g * c.DC, (g + 1) * c.DC)
        m["xT"] = np.ascontiguousarray(xT[cs_, :])
        m["c_l"] = _lay(np.asarray(inp["c"])[0], c.KT)
        n6 = 6 * D // 8
        cols = np.concatenate([np.arange(g * n6, (g + 1) * n6), 2 * D + np.arange(g * c.DC, (g + 1) * c.DC), 5 * D + np.arange(g * c.DC, (g + 1) * c.DC)])
        m["w_ada"] = np.ascontiguousarray(w_ada[:, cols])
        m["b_ada_l"] = _lay(b_ada[cols], c.KT)
        m["npm_l"] = _lay(np.asarray(inp["norm_pre_mix"])[0], c.KT)
        m["npf_l"] = _lay(np.asarray(inp["norm_pre_ffn"])[0], c.KT)
        m["npom_l"] = _lay(np.asarray(inp["norm_post_mix"])[0][cs_], c.DCT)
        m["npof_l"] = _lay(np.asarray(inp["norm_post_ffn"])[0][cs_], c.DCT)
        xch = o_xbc + np.arange(g * c.DIc, (g + 1) * c.DIc)
        bch = o_xbc + c.DI + np.arange(g * c.N, (g + 1) * c.N)
        cch = o_xbc + c.DI + 8 * c.N + np.arange(g * c.N, (g + 1) * c.N)
        hs = np.arange(g * c.HW, (g + 1) * c.HW)
        wcols = np.concatenate([o_z + np.arange(g * c.DIc, (g + 1) * c.DIc), xch, bch, cch,
                                o_q + hs, o_ff + hs, o_fb + hs, o_i + hs, o_g + hs,
                                o_ga + np.arange(g * c.DC, (g + 1) * c.DC), o_gb + np.arange(g * c.DC, (g + 1) * c.DC),
                                o_dtf + np.arange(g * R, (g + 1) * R), o_dtb + np.arange(g * R, (g + 1) * R)])
        assert len(wcols) == c.NCOL
        m["w_in"] = np.ascontiguousarray(w_in[:, wcols])
        cch_all = np.concatenate([xch, bch, cch]) - o_xbc
        cwg = conv_w[:, cch_all]
        m["convw_l"] = np.ascontiguousarray(cwg.T.reshape(c.CT3, 128, 5).transpose(1, 0, 2))
        m["convb_l"] = _lay(conv_b[cch_all], c.CT3)
        hr = slice(g * R, (g + 1) * R)
        dtb = [np.asarray(inp["dt_bias_fwd"])[0][hr], np.asarray(inp["dt_bias_bwd"])[0][hr]]
        alg = [np.asarray(inp["a_log_fwd"])[0][hr], np.asarray(inp["a_log_bwd"])[0][hr]]
        par8 = np.zeros((8 * R, 8), np.float32)
        for blk in range(4):
            for d in range(2):
                rows = slice(blk * 2 * R + d * R, blk * 2 * R + (d + 1) * R)
                par8[rows, 0] = dtb[d]; par8[rows, 1] = alg[d]
                par8[rows, 2 + blk] = 1.0
                par8[rows, 6 + d] = 1.0
        m["par8"] = par8
        m["par2"] = np.zeros((2 * R, 2 + 2 * R), np.float32); m["par2b"] = np.zeros((2 * R, 2), np.float32)
        m["dskip_bc"] = np.ascontiguousarray(np.broadcast_to(np.repeat(np.asarray(inp["d_skip"])[0][hr], 64)[None, :], (128, c.DIc))).astype(np.float32)
        m["ssdw_bc"] = np.ascontiguousarray(np.broadcast_to(np.asarray(inp["ssd_norm_w"])[0][g * c.DIc:(g + 1) * c.DIc][None, :], (128, c.DIc))).astype(np.float32)
        lbt = np.asarray(inp["hg_lower_bound"])[:, g * c.HW:(g + 1) * c.HW]
        m["lb_l"] = np.ascontiguousarray(lbt.reshape(2, c.HHc, 128).transpose(2, 1, 0))
        m["hgw_l"] = _lay(np.asarray(inp["hg_norm_w"])[0][g * c.HW:(g + 1) * c.HW], c.HHc)
        m["w_so"] = np.ascontiguousarray(np.asarray(inp["w_ssd_out"])[0][:, cs_])
        m["w_ho"] = np.ascontiguousarray(np.asarray(inp["w_hg_out"])[0][:, cs_])
        m["w_mo"] = np.ascontiguousarray(np.asarray(inp["w_mix_out"])[0][:, cs_])
        wr = np.asarray(inp["w_router"])[0]
        m["w_r_l"] = np.ascontiguousarray(wr.reshape(c.KT, 128, 16).transpose(1, 0, 2))
        m["w_g"] = np.ascontiguousarray(np.asarray(inp["w_gate"])[0][2 * g:2 * g + 2].reshape(2 * D, c.F))
        m["w_u"] = np.ascontiguousarray(np.asarray(inp["w_up"])[0][2 * g:2 * g + 2].reshape(2 * D, c.F))
        m["w_d"] = np.ascontiguousarray(np.asarray(inp["w_down"])[0][:, :, cs_].reshape(16 * c.F, c.DC))
        es = np.zeros((16, 2), np.float32); es[2 * g, 0] = 1.0; es[2 * g + 1, 1] = 1.0
        m["esel"] = es; m["onesel"] = onesel; m["cidx"] = cidx; m["rowmask"] = rowmask; m["bmask"] = bmask
        maps.append({k_: np.ascontiguousarray(v, dtype=np.float32) for k_, v in m.items()})
    return maps


_CACHE = {}


def run(cfg, inputs, debug=()):
    from concourse.bass_utils import run_bass_kernel_spmd
    key = (cfg.D, cfg.S, tuple(debug))
    if key not in _CACHE:
        p = Prog(cfg, debug)
        _CACHE[key] = p.build()
    nc = _CACHE[key]
    maps = shard_inputs(cfg, inputs)
    res = run_bass_kernel_spmd(nc, maps, core_ids=list(range(8)))
    out = np.empty((1, cfg.S, cfg.D), np.float32)
    for g in range(8):
        out[0][:, g * cfg.DC:(g + 1) * cfg.DC] = res.results[g]["outT"].T
    return out, res


def kernel(**inputs):
    out, _ = run(Cfg(4096, 8192), inputs)
    return out
```
